# Optimizing a Trainium2 kernel written in Bass

```python
import functools
import jax, jax.numpy as jnp
from jax import lax
import numpy as np

D_MODEL = 1024
BATCH = 32
SEQ = 2048
DEPTH = 2

CTX_LEN = 256
GRID_W = 64
RWKV_HEADS = 8
RWKV_HEAD = 64
RWKV_W = RWKV_HEADS * RWKV_HEAD
DECAY_LORA = 64
AAA_LORA = 64
GATE_LORA = 128
LNX_EPS = 64e-5
ATT_HEADS = 8
KV_HEADS = 2
HEAD_DIM = 64
GROUPS = ATT_HEADS // KV_HEADS
ATT_W = ATT_HEADS * HEAD_DIM
KV_W = KV_HEADS * HEAD_DIM
Q_BLOCK = 128
ROPE_THETA = 10000.0
ROPE_AXIS_DIM = HEAD_DIM // 2
N_BRANCH = 2
R_COLS = 3 * RWKV_W + DECAY_LORA + AAA_LORA + GATE_LORA
A_COLS = ATT_W + 2 * KV_W
G_COLS = N_BRANCH * D_MODEL
N_IN = R_COLS + A_COLS + G_COLS
FFN_DIM = 3584
N_EXPERTS = 8
TOP_K = 2
MOE_BLOCK = 512
N_DENSE = (DEPTH + 1) // 2
N_MOE = DEPTH // 2
NORM_EPS = 1e-6

kernel_name = "hybrid_rwkv7_gqa_moe_dit_block"

F32 = jnp.float32


def rmsnorm(x, gain):
    xf = x.astype(F32)
    y = xf * lax.rsqrt(jnp.mean(xf * xf, axis=-1, keepdims=True) + NORM_EPS)
    return (y * gain.astype(F32)).astype(x.dtype)


def centred_shift(p, mu):
    zero = jnp.zeros_like(p[:, :1])
    prev = jnp.concatenate([zero, p[:, :-1]], axis=1)
    nxt = jnp.concatenate([p[:, 1:], zero], axis=1)
    return p + mu[0] * (prev - p) + mu[1] * (nxt - p)


def rwkv_prepare(pr, mu, w0, w2, a0, a2, g2, k_k, k_a):
    B, T, _ = pr.shape
    heads = lambda t: t.reshape(B, T, RWKV_HEADS, RWKV_HEAD)
    pr = centred_shift(pr, mu)
    cuts = [RWKV_W, 2 * RWKV_W, 3 * RWKV_W, 3 * RWKV_W + DECAY_LORA, 3 * RWKV_W + DECAY_LORA + AAA_LORA]
    r, k, v, wd, ad, gd = jnp.split(pr, cuts, axis=-1)
    kkf = heads(k * k_k).astype(F32)
    kk = kkf / jnp.maximum(jnp.sqrt(jnp.sum(kkf * kkf, axis=-1, keepdims=True)), 1e-12)
    tw = jnp.tanh(wd)
    decays, keys, bs = [], [], []
    for d in range(2):
        z = (w0[d] + tw @ w2[d]).astype(F32)
        decay = jnp.exp(-jnp.exp(-jax.nn.softplus(-z) - 0.5))
        a = jax.nn.sigmoid(a0[d] + ad @ a2[d])
        decays.append(heads(decay))
        keys.append(heads(k * (1 + (a - 1) * k_a)))
        bs.append(kk * heads(a).astype(F32))
    g = jax.nn.sigmoid(gd) @ g2
    return heads(r), heads(v), kk, g, decays, keys, bs


def wkv_scan(state, r, decay, k, v, kk, b, reverse):
    xs = tuple(jnp.moveaxis(t.astype(F32), 1, 0) for t in (r, decay, k, v, -kk, b))

    def step(S, inp):
        r_t, w_t, k_t, v_t, a_t, b_t = inp
        Sa = jnp.einsum('bhvk,bhk->bhv', S, a_t)
        S = S * w_t[:, :, None, :] + Sa[..., None] * b_t[:, :, None, :] + v_t[..., None] * k_t[:, :, None, :]
        return S, jnp.einsum('bhvk,bhk->bhv', S, r_t)

    S, ys = lax.scan(step, state, xs, reverse=reverse)
    return S, jnp.moveaxis(ys, 0, 1)


def rwkv_output(y, r, keys, v, g, r_k, lnx_w, lnx_b):
    B, T = y.shape[:2]
    mean = jnp.mean(y, axis=-1, keepdims=True)
    var = jnp.mean(jnp.square(y - mean), axis=-1, keepdims=True)
    yn = ((y - mean) * lax.rsqrt(var + LNX_EPS)).reshape(B, T, RWKV_W)
    yn = (yn * lnx_w.astype(F32) + lnx_b.astype(F32)).astype(v.dtype)
    kbar = 0.5 * (keys[0] + keys[1])
    bonus = jnp.sum(r * kbar * r_k, axis=-1, keepdims=True) * v
    return (yn + bonus.reshape(B, T, RWKV_W)) * g


def head_rms(t, gain):
    tf = t.astype(F32)
    return (tf * lax.rsqrt(jnp.mean(tf * tf, axis=-1, keepdims=True) + NORM_EPS) * gain.astype(F32)).astype(t.dtype)


def attn_qkv(pa, q_norm, k_norm):
    B, T, _ = pa.shape
    q = pa[..., :ATT_W].reshape(B, T, KV_HEADS, GROUPS, HEAD_DIM)
    k = pa[..., ATT_W:ATT_W + KV_W].reshape(B, T, KV_HEADS, HEAD_DIM)
    v = pa[..., ATT_W + KV_W:].reshape(B, T, KV_HEADS, HEAD_DIM)
    return head_rms(q, q_norm), head_rms(k, k_norm), v


def rope_2d_tables(n_tokens):
    rows = n_tokens // GRID_W
    row, col = jnp.meshgrid(jnp.arange(rows), jnp.arange(GRID_W), indexing='ij')
    inv = ROPE_THETA ** (-jnp.arange(0, ROPE_AXIS_DIM, 2, dtype=F32) / ROPE_AXIS_DIM)
    ang = jnp.concatenate([row.reshape(-1, 1).astype(F32) * inv, col.reshape(-1, 1).astype(F32) * inv], axis=-1)
    return jnp.cos(ang), jnp.sin(ang)


def apply_rope(t, cos, sin):
    shp = t.shape
    tp = t.astype(F32).reshape(shp[:-1] + (HEAD_DIM // 2, 2))
    bshape = (1, shp[1]) + (1,) * (len(shp) - 3) + (HEAD_DIM // 2,)
    c, s = cos.reshape(bshape), sin.reshape(bshape)
    x0, x1 = tp[..., 0], tp[..., 1]
    return jnp.stack([x0 * c - x1 * s, x0 * s + x1 * c], axis=-1).reshape(shp).astype(t.dtype)


def block_attention(q, k, v):
    B, T = q.shape[:2]
    nb = T // Q_BLOCK
    qb = jnp.moveaxis(q.reshape(B, nb, Q_BLOCK, KV_HEADS, GROUPS, HEAD_DIM), 1, 0)
    scale = HEAD_DIM ** -0.5

    def one(qblk):
        s = jnp.einsum('bqhgd,bkhd->bhgqk', qblk, k).astype(F32) * scale
        p = jax.nn.softmax(s, axis=-1).astype(v.dtype)
        return jnp.einsum('bhgqk,bkhd->bqhgd', p, v)

    o = lax.map(one, qb)
    return jnp.moveaxis(o, 0, 1).reshape(B, T, ATT_W)


def merge_branches(pg, y_rwkv, y_att, w_pa, w_pb, w_o):
    gate_a = jax.nn.sigmoid(pg[..., :D_MODEL])
    gate_b = jax.nn.sigmoid(pg[..., D_MODEL:])
    return (gate_a * (y_rwkv @ w_pa) + gate_b * (y_att @ w_pb)) @ w_o


def swiglu(h, wg, wu, wd):
    return (jax.nn.silu(h @ wg) * (h @ wu)) @ wd


def moe_swiglu(h, router, wg, wu, wd):
    shp = h.shape
    hf = h.reshape(-1, D_MODEL)
    T = hf.shape[0]
    logits = (hf @ router).astype(F32)
    top_logit, top_idx = lax.top_k(logits, TOP_K)
    top_w = jax.nn.softmax(top_logit, axis=-1)
    n_assign = T * TOP_K
    e_flat = top_idx.reshape(-1).astype(jnp.int32)
    tok_flat = jnp.repeat(jnp.arange(T, dtype=jnp.int32), TOP_K)
    w_flat = top_w.reshape(-1)
    order = jnp.argsort(e_flat)
    e_sorted = e_flat[order]
    counts = jnp.zeros((N_EXPERTS,), jnp.int32).at[e_flat].add(1)
    starts = jnp.cumsum(counts) - counts
    padded = (counts + MOE_BLOCK - 1) // MOE_BLOCK * MOE_BLOCK
    pends = jnp.cumsum(padded)
    pstarts = pends - padded
    dest = pstarts[e_sorted] + jnp.arange(n_assign, dtype=jnp.int32) - starts[e_sorted]
    n_pad = (-(-n_assign // MOE_BLOCK) + N_EXPERTS) * MOE_BLOCK
    tok_buf = jnp.zeros((n_pad,), jnp.int32).at[dest].set(tok_flat[order])
    w_buf = jnp.zeros((n_pad,), F32).at[dest].set(w_flat[order])
    nblk = n_pad // MOE_BLOCK
    blk_start = jnp.arange(nblk, dtype=jnp.int32) * MOE_BLOCK
    blk_e = jnp.minimum(jnp.searchsorted(pends, blk_start, side='right'), N_EXPERTS - 1).astype(jnp.int32)
    xb = hf[tok_buf].reshape(nblk, MOE_BLOCK, D_MODEL)

    def expert_block(args):
        xblk, e = args
        return swiglu(xblk, wg[e], wu[e], wd[e])

    yb = lax.map(expert_block, (xb, blk_e)).reshape(n_pad, D_MODEL)
    out = jnp.zeros_like(hf).at[tok_buf].add(yb * w_buf[:, None].astype(hf.dtype))
    return out.reshape(shp)


def setup_inputs(seed: int = 0) -> dict:
    key = jax.random.key(seed)
    ks = iter(jax.random.split(key, 48))
    nrm = lambda shape, scale: jax.random.normal(next(ks), shape, F32) * scale
    uni = lambda shape, lo, hi: jax.random.uniform(next(ks), shape, F32, lo, hi)
    D = D_MODEL
    return {
        "x": nrm((BATCH, SEQ, D), 1.0),
        "c": nrm((BATCH, D), 1.0),
        "ctx": nrm((BATCH, CTX_LEN, D), 1.0),
        "c_ctx": nrm((D,), 1.0),
        "ada_w": nrm((DEPTH, D, 6 * D), 0.5 * D ** -0.5),
        "ada_b": nrm((DEPTH, 6 * D), 0.02),
        "norm1": 1.0 + nrm((DEPTH, D), 0.05),
        "norm2": 1.0 + nrm((DEPTH, D), 0.05),
        "w_in": nrm((DEPTH, D, N_IN), D ** -0.5),
        "shift_mu": uni((DEPTH, 2, R_COLS), 0.0, 0.5),
        "rwkv_w0": uni((DEPTH, 2, RWKV_W), -6.0, 1.0),
        "rwkv_w2": nrm((DEPTH, 2, DECAY_LORA, RWKV_W), 0.1),
        "rwkv_a0": nrm((DEPTH, 2, RWKV_W), 0.1),
        "rwkv_a2": nrm((DEPTH, 2, AAA_LORA, RWKV_W), AAA_LORA ** -0.5),
        "rwkv_g2": nrm((DEPTH, GATE_LORA, RWKV_W), GATE_LORA ** -0.5),
        "rwkv_kk": 0.85 + nrm((DEPTH, RWKV_W), 0.05),
        "rwkv_ka": 1.0 + nrm((DEPTH, RWKV_W), 0.05),
        "rwkv_rk": nrm((DEPTH, RWKV_HEADS, RWKV_HEAD), 0.1),
        "lnx_w": 1.0 + nrm((DEPTH, RWKV_W), 0.05),
        "lnx_b": nrm((DEPTH, RWKV_W), 0.02),
        "q_norm": 1.0 + nrm((DEPTH, HEAD_DIM), 0.05),
        "k_norm": 1.0 + nrm((DEPTH, HEAD_DIM), 0.05),
        "w_pa": nrm((DEPTH, RWKV_W, D), RWKV_W ** -0.5),
        "w_pb": nrm((DEPTH, ATT_W, D), ATT_W ** -0.5),
        "w_o": nrm((DEPTH, D, D), D ** -0.5),
        "ffn_wg": nrm((N_DENSE, D, FFN_DIM), D ** -0.5),
        "ffn_wu": nrm((N_DENSE, D, FFN_DIM), D ** -0.5),
        "ffn_wd": nrm((N_DENSE, FFN_DIM, D), FFN_DIM ** -0.5),
        "router": nrm((N_MOE, D, N_EXPERTS), D ** -0.5),
        "moe_wg": nrm((N_MOE, N_EXPERTS, D, FFN_DIM), D ** -0.5),
        "moe_wu": nrm((N_MOE, N_EXPERTS, D, FFN_DIM), D ** -0.5),
        "moe_wd": nrm((N_MOE, N_EXPERTS, FFN_DIM, D), FFN_DIM ** -0.5),
        "final_norm": 1.0 + nrm((D,), 0.05),
    }


def reference(x, c, ctx, c_ctx, ada_w, ada_b, norm1, norm2, w_in, shift_mu, rwkv_w0, rwkv_w2, rwkv_a0, rwkv_a2,
              rwkv_g2, rwkv_kk, rwkv_ka, rwkv_rk, lnx_w, lnx_b, q_norm, k_norm, w_pa, w_pb, w_o,
              ffn_wg, ffn_wu, ffn_wd, router, moe_wg, moe_wu, moe_wd, final_norm):
    B, T, _ = x.shape
    cos, sin = rope_2d_tables(T)
    c_act = jax.nn.silu(c)
    cc_act = jax.nn.silu(c_ctx)
    xc = ctx
    zero_state = jnp.zeros((B, RWKV_HEADS, RWKV_HEAD, RWKV_HEAD), F32)
    for l in range(DEPTH):
        last = l == DEPTH - 1
        mod = (c_act @ ada_w[l] + ada_b[l])[:, None, :]
        modc = cc_act @ ada_w[l] + ada_b[l]
        sh1, sc1, gt1, sh2, sc2, gt2 = jnp.split(mod, 6, axis=-1)
        csh1, csc1, cgt1, csh2, csc2, cgt2 = jnp.split(modc, 6, axis=-1)

        h = rmsnorm(x, norm1[l]) * (1 + sc1) + sh1
        hc = rmsnorm(xc, norm1[l]) * (1 + csc1) + csh1
        p = h @ w_in[l]
        pc = hc @ w_in[l]

        rw = (shift_mu[l], rwkv_w0[l], rwkv_w2[l], rwkv_a0[l], rwkv_a2[l], rwkv_g2[l], rwkv_kk[l], rwkv_ka[l])
        lr, lv, lkk, lg, ldec, lkeys, lbs = rwkv_prepare(p[..., :R_COLS], *rw)
        cr, cv, ckk, cg, cdec, ckeys, cbs = rwkv_prepare(pc[..., :R_COLS], *rw)
        y_lat_dirs, y_ctx_dirs = [], []
        for d in range(2):
            rev = d == 1
            s_ctx, yc_d = wkv_scan(zero_state, cr, cdec[d], ckeys[d], cv, ckk, cbs[d], rev)
            _, yl_d = wkv_scan(s_ctx, lr, ldec[d], lkeys[d], lv, lkk, lbs[d], rev)
            y_lat_dirs.append(yl_d)
            y_ctx_dirs.append(yc_d)
        y_rwkv = rwkv_output(y_lat_dirs[0] + y_lat_dirs[1], lr, lkeys, lv, lg, rwkv_rk[l], lnx_w[l], lnx_b[l])

        q, k, v = attn_qkv(p[..., R_COLS:R_COLS + A_COLS], q_norm[l], k_norm[l])
        qc, kc, vc = attn_qkv(pc[..., R_COLS:R_COLS + A_COLS], q_norm[l], k_norm[l])
        q = apply_rope(q, cos, sin)
        k = apply_rope(k, cos, sin)
        y_att = block_attention(q, jnp.concatenate([k, kc], axis=1), jnp.concatenate([v, vc], axis=1))

        x = x + gt1 * merge_branches(p[..., R_COLS + A_COLS:], y_rwkv, y_att, w_pa[l], w_pb[l], w_o[l])
        if not last:
            yc_rwkv = rwkv_output(y_ctx_dirs[0] + y_ctx_dirs[1], cr, ckeys, cv, cg, rwkv_rk[l], lnx_w[l], lnx_b[l])
            yc_att = block_attention(qc, kc, vc)
            xc = xc + cgt1 * merge_branches(pc[..., R_COLS + A_COLS:], yc_rwkv, yc_att, w_pa[l], w_pb[l], w_o[l])

        if l % 2 == 0:
            ffn = functools.partial(swiglu, wg=ffn_wg[l // 2], wu=ffn_wu[l // 2], wd=ffn_wd[l // 2])
        else:
            ffn = functools.partial(moe_swiglu, router=router[l // 2], wg=moe_wg[l // 2], wu=moe_wu[l // 2], wd=moe_wd[l // 2])
        h2 = rmsnorm(x, norm2[l]) * (1 + sc2) + sh2
        x = x + gt2 * ffn(h2)
        if not last:
            hc2 = rmsnorm(xc, norm2[l]) * (1 + csc2) + csh2
            xc = xc + cgt2 * ffn(hc2)
    return rmsnorm(x, final_norm)
```

```python
import contextlib
import numpy as np
import concourse.bass as bass
import concourse.mybir as mybir
from concourse.bass_utils import run_bass_kernel_spmd

F32 = mybir.dt.float32
BF16 = mybir.dt.bfloat16
AF = mybir.ActivationFunctionType
ALU = mybir.AluOpType
AX = mybir.AxisListType

ENGS = ("pe", "act", "dve", "pool", "sp")
EPOCH = 30000
NDMASEM = 48

D = 1024
LAT = 2048
CTX = 256
NT = LAT + CTX
NIN = 4608
RC = 1792
FF = 3584
NE = 8
LW = -0.6065306597126334
TBS = [(0, 512), (512, 512), (1024, 512), (1536, 512), (2048, 256)]
NPL = 130


class Res:
    __slots__ = ("last_w", "readers")

    def __init__(self):
        self.last_w = None
        self.readers = []


class Prog:
    def __init__(self, nc):
        self.nc = nc
        self.es = contextlib.ExitStack()
        self.seq = {e: 0 for e in ENGS}
        self.sems = []
        self.engsem = {}
        self.known = {e: {} for e in ENGS}
        self.dmasem = []
        self.ndma = 0
        self.dma_last = {}
        self.ninstr = 0
        self.eobj = {"pe": nc.tensor, "act": nc.scalar, "dve": nc.vector, "pool": nc.gpsimd, "sp": nc.sync}
        self.scope = [self.es]

    def new_sem(self, name):
        h = self.es.enter_context(self.nc.semaphore(name))
        self.sems.append(h)
        return len(self.sems) - 1

    def push_scope(self):
        st = contextlib.ExitStack()
        self.scope.append(st)

    def pop_scope(self):
        self.barrier()
        self.scope.pop().close()

    def sb(self, name, shape, dt):
        self.ntile = getattr(self, "ntile", 0) + 1
        return self.scope[-1].enter_context(self.nc.sbuf_tensor(f"{name}_{self.ntile}", list(shape), dt))

    def ps(self, name, shape, dt):
        return self.es.enter_context(self.nc.psum_tensor(name, list(shape), dt))

    def _waits_for(self, eng, reads, writes):
        need = {}
        known = self.known[eng]

        def add(key):
            if key is None:
                return
            s, v, ke = key
            if ke == eng and eng in ("pe", "sp"):
                return
            if known.get(s, 0) >= v:
                return
            if need.get(s, 0) < v:
                need[s] = v

        for r in reads:
            add(r.last_w)
        for w in writes:
            add(w.last_w)
            for k in w.readers:
                if k[2] == eng:
                    continue
                add(k)
        for s, v in need.items():
            known[s] = v
        return list(need.items())

    def _emit(self, eng, waits, fn, s, inc):
        e = self.eobj[eng]
        for ws, wv in waits:
            e.wait_ge(self.sems[ws], wv)
        if fn is not None:
            fn(e).then_inc(self.sems[s], inc)

    def op(self, eng, fn, reads=(), writes=()):
        waits = self._waits_for(eng, reads, writes)
        self.seq[eng] += 1
        n = self.seq[eng]
        ep = (n - 1) // EPOCH
        if (eng, ep) not in self.engsem:
            self.engsem[(eng, ep)] = self.new_sem(f"s_{eng}_{ep}")
        s = self.engsem[(eng, ep)]
        v = (n - 1) % EPOCH + 1
        key = (s, v, eng)
        self._emit(eng, waits, fn, s, 1)
        for r in reads:
            r.readers.append(key)
        for w in writes:
            w.last_w = key
            w.readers = []
        self.ninstr += 1
        return key

    def dma(self, eng, out_ap, in_ap, reads=(), writes=()):
        if not self.dmasem:
            self.dmasem = [self.new_sem(f"s_dma_{i}") for i in range(NDMASEM)]
        j = self.ndma
        self.ndma += 1
        slot = j % NDMASEM
        s = self.dmasem[slot]
        v = 16 * (j // NDMASEM + 1)
        waits = self._waits_for(eng, reads, writes)
        prev = self.dma_last.get(slot)
        if prev is not None and self.known[eng].get(prev[0], 0) < prev[1]:
            d = dict(waits)
            d[prev[0]] = max(d.get(prev[0], 0), prev[1])
            self.known[eng][prev[0]] = prev[1]
            waits = list(d.items())
        key = (s, v, "dma")
        self.dma_last[slot] = key
        self._emit(eng, waits, lambda e: e.dma_start(out=out_ap, in_=in_ap), s, 16)
        for r in reads:
            r.readers.append(key)
        for w in writes:
            w.last_w = key
            w.readers = []
        self.ninstr += 1
        return key

    def barrier(self):
        keys = []
        for (eng, ep), s in self.engsem.items():
            if self.seq[eng] > 0 and ep == (self.seq[eng] - 1) // EPOCH:
                keys.append((s, (self.seq[eng] - 1) % EPOCH + 1))
        for slot, k in self.dma_last.items():
            keys.append((k[0], k[1]))
        for eng in ENGS:
            for s, v in keys:
                if self.known[eng].get(s, 0) < v:
                    self.known[eng][s] = v
                    self.eobj[eng].wait_ge(self.sems[s], v)

    def close(self):
        while len(self.scope) > 1:
            self.scope.pop().close()
        self.es.close()


def _colz(v):
    return np.ascontiguousarray(np.asarray(v, np.float32).reshape(-1, 128).T)


def host_consts():
    c = {}
    c["ident"] = np.eye(128, dtype=np.float32)
    bd = np.zeros((128, 128), np.float32)
    bd[:64, :64] = 1.0
    bd[64:, 64:] = 1.0
    c["onesbd"] = bd
    p = np.arange(128)[:, None]
    f = np.arange(128)[None, :]
    lt = (p < f).astype(np.float32)
    le = (p <= f).astype(np.float32)
    gt = (p > f).astype(np.float32)
    ge = (p >= f).astype(np.float32)
    c["mask"] = np.concatenate([
        np.tile(lt, (1, 4)), np.tile(le, (1, 4)), np.tile(gt, (1, 2)),
        np.tile(gt, (1, 4)), np.tile(ge, (1, 4)), np.tile(lt, (1, 2)),
    ], axis=1).astype(np.float32)
    R = np.zeros((128, 128), np.float32)
    for i in range(64):
        R[2 * i + 1, 2 * i] = -1.0
        R[2 * i, 2 * i + 1] = 1.0
    c["rot"] = R
    t = np.arange(LAT)
    row = (t // 64).astype(np.float32)
    col = (t % 64).astype(np.float32)
    inv = (10000.0 ** (-np.arange(0, 32, 2, dtype=np.float32) / 32.0)).astype(np.float32)
    ang = np.concatenate([row[:, None] * inv[None, :], col[:, None] * inv[None, :]], axis=1).astype(np.float32)
    cos = np.cos(ang).astype(np.float32)
    sin = np.sin(ang).astype(np.float32)
    cosf = np.repeat(cos, 2, axis=1).T
    sinf = np.repeat(sin, 2, axis=1).T
    c["cos"] = np.ascontiguousarray(np.concatenate([cosf, cosf], axis=0))
    c["sin"] = np.ascontiguousarray(np.concatenate([sinf, sinf], axis=0))
    rm = np.ones((128, 512), np.float32)
    rm[:, ::128] = 0.0
    c["rmask"] = rm
    return c


def host_params(inp):
    cols = []
    for l in range(2):
        cols += [_colz(inp["norm1"][l]), _colz(inp["norm2"][l]), _colz(inp["ada_b"][l]),
                 _colz(inp["shift_mu"][l, 0]), _colz(inp["shift_mu"][l, 1]),
                 _colz(inp["rwkv_w0"][l, 0]), _colz(inp["rwkv_w0"][l, 1]),
                 _colz(inp["rwkv_a0"][l, 0]), _colz(inp["rwkv_a0"][l, 1]),
                 _colz(inp["rwkv_kk"][l]), _colz(inp["rwkv_ka"][l]), _colz(inp["rwkv_rk"][l]),
                 _colz(inp["lnx_w"][l]), _colz(inp["lnx_b"][l]),
                 _colz(np.tile(inp["q_norm"][l], 2)), _colz(np.tile(inp["k_norm"][l], 2))]
    cols.append(_colz(inp["final_norm"]))
    return np.ascontiguousarray(np.concatenate(cols, axis=1))


PO = {}
_o = 0
for _n, _w in [("norm1", 8), ("norm2", 8), ("adab", 48), ("mu0", 14), ("mu1", 14), ("w0_0", 4), ("w0_1", 4),
               ("a0_0", 4), ("a0_1", 4), ("kk", 4), ("ka", 4), ("rk", 4), ("lnxw", 4), ("lnxb", 4), ("qn", 1), ("kn", 1)]:
    PO[_n] = _o
    _o += _w
assert _o == NPL


def build(nb=4, nlayers=2, dbg=None, stop_after=None):
    dbg = dbg or set()
    nc = bass.Bass("TRN2", target_bir_lowering=False)
    P = Prog(nc)
    dt = nc.dram_tensor

    def din(name, shape, dtype=F32):
        return dt(name, list(shape), dtype, kind="ExternalInput").ap()

    def dscr(name, shape, dtype):
        return dt(name, list(shape), dtype, kind="Internal").ap()

    xT_d = din("xT", [nb, D, LAT])
    cxT_d = din("cxT", [nb, D, CTX])
    cT_d = din("cT", [128, 8, 5])
    par_d = din("params", [128, 2 * NPL + 8])
    ident_d = din("ident", [128, 128])
    onesbd_d = din("onesbd", [128, 128])
    mask_d = din("mask", [128, 2560])
    rot_d = din("rot", [128, 128])
    cos_d = din("cos", [128, LAT])
    sin_d = din("sin", [128, LAT])
    rmask_d = din("rmask", [128, 512])
    ada_w_d = din("ada_w", [2, D, 6 * D])
    w_in_d = din("w_in", [2, D, NIN])
    w2_d = din("rwkv_w2", [2, 2, 64, 512])
    a2_d = din("rwkv_a2", [2, 2, 64, 512])
    g2_d = din("rwkv_g2", [2, 128, 512])
    w_pa_d = din("w_pa", [2, 512, D])
    w_pb_d = din("w_pb", [2, 512, D])
    w_o_d = din("w_o", [2, D, D])
    ffn_wg_d = din("ffn_wg", [1, D, FF])
    ffn_wu_d = din("ffn_wu", [1, D, FF])
    ffn_wd_d = din("ffn_wd", [1, FF, D])
    router_d = din("router", [1, D, NE])
    if nlayers > 1:
        moe_wg_d = din("moe_wg", [1, NE, D, FF])
        moe_wu_d = din("moe_wu", [1, NE, D, FF])
        moe_wd_d = din("moe_wd", [1, NE, FF, D])
    outT_d = dt("outT", [nb, D, LAT], F32, kind="ExternalOutput").ap()

    PR_d = dscr("PR", [RC, NT], F32)
    PA_d = dscr("PA", [768, NT], F32)
    PG_d = dscr("PG", [2048, NT], BF16)
    YR_d = dscr("YR", [512, NT], BF16)
    YA_d = dscr("YA", [512, NT], BF16)
    rPR = [Res() for _ in range(14)]
    rPA = [Res() for _ in range(6)]
    rPG = [Res() for _ in range(16)]
    rYR = [Res() for _ in range(4)]
    rYA = [Res() for _ in range(4)]
    dbg_out = {}

    def dbg_tensor(name, shape, dtype=F32):
        dbg_out[name] = dt(name, list(shape), dtype, kind="ExternalOutput").ap()
        return dbg_out[name]

    xT = P.sb("xT_sb", [128, 8, NT], F32)
    rx = [Res() for _ in TBS]
    par = P.sb("par", [128, 2 * NPL + 8], F32)
    r_par = Res()
    dpar = P.sb("dpar", [128, 64], F32)
    r_dpar = Res()
    cst = P.sb("cst", [128, 8], F32)
    r_cst = Res()
    modT = P.sb("modT", [128, 2, 48, 5], F32)
    r_mod = Res()
    mdv = P.sb("mdv", [128, 2, 6, 8, 5], F32)
    r_mdv = Res()
    ident_f = P.sb("ident_f", [128, 128], F32)
    ident_b = P.sb("ident_b", [128, 128], BF16)
    ident2_f = P.sb("ident2_f", [128, 256], F32)
    onesbd_f = P.sb("onesbd_f", [128, 128], F32)
    onesbd_b = P.sb("onesbd_b", [128, 128], BF16)
    ones_f = P.sb("ones_f", [128, 128], F32)
    rot_b = P.sb("rot_b", [128, 128], BF16)
    r_const = Res()

    banks = [P.ps(f"bank{i}", [128, 512], F32) for i in range(8)]
    rbank = [Res() for _ in range(8)]
    bank_ctr = [0]

    def nbank(lo=0, hi=8):
        i = lo + bank_ctr[0] % (hi - lo)
        bank_ctr[0] += 1
        return banks[i], rbank[i]

    def pcol(l, name, j=0):
        o = l * NPL + PO[name] + j
        return par[:, o:o + 1]

    def prologue():
        P.push_scope()
        stg = P.sb("pl_stg", [128, 128], F32)
        r_stg = Res()
        P.dma("sp", par[:], par_d, writes=[r_par])
        P.dma("sp", ident_f[:], ident_d, writes=[r_const])
        P.dma("sp", onesbd_f[:], onesbd_d, writes=[r_const])
        P.dma("sp", stg[:], rot_d, writes=[r_stg])
        P.op("dve", lambda e: e.tensor_copy(out=rot_b[:], in_=stg[:]), reads=[r_stg], writes=[r_const])
        P.op("dve", lambda e: e.tensor_copy(out=ident_b[:], in_=ident_f[:]), reads=[r_const], writes=[r_const])
        P.op("dve", lambda e: e.tensor_copy(out=ident2_f[:, 0:128], in_=ident_f[:]), reads=[r_const], writes=[r_const])
        P.op("dve", lambda e: e.tensor_copy(out=ident2_f[:, 128:256], in_=ident_f[:]), reads=[r_const], writes=[r_const])
        P.op("dve", lambda e: e.tensor_copy(out=onesbd_b[:], in_=onesbd_f[:]), reads=[r_const], writes=[r_const])
        P.op("dve", lambda e: e.memset(ones_f[:], 1.0), writes=[r_const])
        P.op("dve", lambda e: e.memset(cst[:, 0:1], 1e-6), writes=[r_cst])
        P.op("dve", lambda e: e.memset(cst[:, 1:2], 64e-5), writes=[r_cst])
        P.op("dve", lambda e: e.memset(cst[:, 2:3], 1.0), writes=[r_cst])
        P.op("dve", lambda e: e.memset(cst[:, 3:4], 0.0), writes=[r_cst])
        P.op("dve", lambda e: e.memset(cst[:, 4:5], 1e-24), writes=[r_cst])
        for l in range(2):
            b0 = l * 32
            m0 = par[:, l * NPL + PO["mu0"]: l * NPL + PO["mu0"] + 14]
            m1 = par[:, l * NPL + PO["mu1"]: l * NPL + PO["mu1"] + 14]
            P.op("dve", lambda e, m0=m0, m1=m1, b0=b0: e.tensor_tensor(out=dpar[:, b0:b0 + 14], in0=m0, in1=m1, op=ALU.add),
                 reads=[r_par], writes=[r_dpar])
            P.op("dve", lambda e, b0=b0: e.tensor_scalar(out=dpar[:, b0:b0 + 14], in0=dpar[:, b0:b0 + 14], scalar1=-1.0, scalar2=1.0,
                                                        op0=ALU.mult, op1=ALU.add), reads=[r_dpar], writes=[r_dpar])
            ka = par[:, l * NPL + PO["ka"]: l * NPL + PO["ka"] + 4]
            P.op("dve", lambda e, ka=ka, b0=b0: e.tensor_scalar(out=dpar[:, b0 + 14:b0 + 18], in0=ka, scalar1=-1.0, scalar2=1.0,
                                                               op0=ALU.mult, op1=ALU.add), reads=[r_par], writes=[r_dpar])
            qn = pcol(l, "qn")
            P.op("dve", lambda e, qn=qn, b0=b0: e.tensor_scalar(out=dpar[:, b0 + 18:b0 + 19], in0=qn, scalar1=0.125, scalar2=None,
                                                               op0=ALU.mult), reads=[r_par], writes=[r_dpar])
        cact = P.sb("pl_cact", [128, 8, 5], F32)
        r_cact = Res()
        P.dma("sp", cact[:], cT_d, writes=[r_cact])
        P.op("act", lambda e: e.activation(out=cact[:], in_=cact[:], func=AF.Silu), reads=[r_cact], writes=[r_cact])
        aw = [P.sb(f"pl_aw{i}", [128, 8, 512], F32) for i in range(2)]
        r_aw = [Res(), Res()]
        for l in range(nlayers):
            for cb in range(12):
                i = cb % 2
                src = ada_w_d[l].rearrange("(kc p) n -> p kc n", p=128)[:, :, cb * 512:(cb + 1) * 512]
                P.dma("sp", aw[i][:], src, writes=[r_aw[i]])
                for cc in range(4):
                    ch = cb * 4 + cc
                    bk, rb = nbank()
                    for kc in range(8):
                        P.op("pe", lambda e, bk=bk, i=i, kc=kc, cc=cc: e.matmul(
                            bk[:, 0:5], aw[i][:, kc, cc * 128:(cc + 1) * 128], cact[:, kc, :], start=(kc == 0), stop=(kc == 7)),
                            reads=[r_aw[i], r_cact], writes=[rb])
                    bcol = pcol(l, "adab", ch)
                    P.op("dve", lambda e, bk=bk, l=l, ch=ch, bcol=bcol: e.tensor_scalar(
                        out=modT[:, l, ch, :], in0=bk[:, 0:5], scalar1=bcol, scalar2=None, op0=ALU.add),
                        reads=[rb, r_par], writes=[r_mod])
            for half, nm in ((0, "norm1"), (1, "norm2")):
                base = half * 24
                for c in range(8):
                    nrm = pcol(l, nm, c)
                    P.op("dve", lambda e, l=l, half=half, c=c, base=base, nrm=nrm: e.tensor_scalar(
                        out=mdv[:, l, half * 3 + 0, c, :], in0=modT[:, l, base + 8 + c, :], scalar1=1.0, scalar2=nrm,
                        op0=ALU.add, op1=ALU.mult), reads=[r_mod, r_par], writes=[r_mdv])
                    P.op("dve", lambda e, l=l, half=half, c=c, base=base: e.tensor_copy(
                        out=mdv[:, l, half * 3 + 1, c, :], in_=modT[:, l, base + c, :]), reads=[r_mod], writes=[r_mdv])
                    P.op("dve", lambda e, l=l, half=half, c=c, base=base: e.tensor_copy(
                        out=mdv[:, l, half * 3 + 2, c, :], in_=modT[:, l, base + 16 + c, :]), reads=[r_mod], writes=[r_mdv])
        P.pop_scope()

    def norm_block(l, half, jb, tb, hT, rh, sqb, r_sqb, tmpb, r_tmpb, rstd, r_rstd, hT32=None, rh32=None):
        s, n = TBS[tb]
        j = 4 if tb == 4 else jb
        bk, rb = nbank()
        for c in range(8):
            i = c % 2
            P.op("act", lambda e, c=c, i=i: e.activation(out=sqb[i][:, 0:n], in_=xT[:, c, s:s + n], func=AF.Square),
                 reads=[rx[tb]], writes=[r_sqb[i]])
            P.op("pe", lambda e, c=c, i=i, bk=bk: e.matmul(bk[:, 0:n], ones_f[:], sqb[i][:, 0:n], start=(c == 0), stop=(c == 7)),
                 reads=[r_sqb[i], r_const], writes=[rb])
        P.op("act", lambda e, bk=bk: e.activation(out=rstd[:, 0:n], in_=bk[:, 0:n], func=AF.Sqrt, scale=1.0 / D, bias=cst[:, 0:1]),
             reads=[rb, r_cst], writes=[r_rstd])
        P.op("dve", lambda e: e.reciprocal(out=rstd[:, 0:n], in_=rstd[:, 0:n]), reads=[r_rstd], writes=[r_rstd])
        for c in range(8):
            i = c % 2
            g = mdv[:, l, half * 3 + 0, c, j:j + 1]
            sh = mdv[:, l, half * 3 + 1, c, j:j + 1]
            P.op("dve", lambda e, c=c, i=i, g=g: e.scalar_tensor_tensor(out=tmpb[i][:, 0:n], in0=xT[:, c, s:s + n], scalar=g,
                                                                      in1=rstd[:, 0:n], op0=ALU.mult, op1=ALU.mult),
                 reads=[rx[tb], r_mdv, r_rstd], writes=[r_tmpb[i]])
            P.op("act", lambda e, c=c, i=i, sh=sh: e.activation(out=hT[:, c, s:s + n], in_=tmpb[i][:, 0:n], func=AF.Identity,
                                                                scale=1.0, bias=sh),
                 reads=[r_tmpb[i], r_mdv], writes=[rh[tb]])
            if hT32 is not None:
                P.op("pool", lambda e, c=c, i=i, sh=sh: e.tensor_scalar(out=hT32[:, c, 0:n], in0=tmpb[i][:, 0:n], scalar1=sh, scalar2=None,
                                                                       op0=ALU.add),
                     reads=[r_tmpb[i], r_mdv], writes=[rh32])

    def phase_proj(l, jb, tbs):
        P.push_scope()
        hT = P.sb("hT", [128, 8, NT], BF16)
        rh = [Res() for _ in TBS]
        sqb = [P.sb(f"p1_sq{i}", [128, 512], F32) for i in range(2)]
        r_sqb = [Res(), Res()]
        tmpb = [P.sb(f"p1_tmp{i}", [128, 512], F32) for i in range(2)]
        r_tmpb = [Res(), Res()]
        rstd = P.sb("p1_rstd", [128, 512], F32)
        r_rstd = Res()
        for tb in tbs:
            norm_block(l, 0, jb, tb, hT, rh, sqb, r_sqb, tmpb, r_tmpb, rstd, r_rstd)
        if "h" in dbg and l == 0:
            hd = dbg_tensor("dbg_h", [D, NT], BF16)
            for c in range(8):
                P.dma("sp", hd[c * 128:(c + 1) * 128, :], hT[:, c, :], reads=rh)
        wst = [P.sb(f"p2_wst{i}", [128, 8, 256], F32) for i in range(2)]
        r_wst = [Res(), Res()]
        wbf = [P.sb(f"p2_wbf{i}", [128, 8, 256], BF16) for i in range(2)]
        r_wbf = [Res(), Res()]
        pf = P.sb("p2_pf", [128, NT + 3], F32)
        r_pf = Res()
        po = [P.sb(f"p2_po{i}", [128, NT], F32) for i in range(2)]
        r_po = [Res(), Res()]
        pg = [P.sb(f"p2_pg{i}", [128, NT], BF16) for i in range(2)]
        r_pg = [Res(), Res()]
        P.op("pool", lambda e: e.memset(pf[:], 0.0), writes=[r_pf])
        wsrc = w_in_d[l].rearrange("(kc p) n -> p kc n", p=128)
        nctx = 4 in tbs
        for cg in range(18):
            i = cg % 2
            P.dma("sp", wst[i][:], wsrc[:, :, cg * 256:(cg + 1) * 256], writes=[r_wst[i]])
            P.op("pool", lambda e, i=i: e.tensor_copy(out=wbf[i][:], in_=wst[i][:]), reads=[r_wst[i]], writes=[r_wbf[i]])
            for cc in range(2):
                gc = cg * 2 + cc
                o = gc % 2
                for tb in tbs:
                    s, n = TBS[tb]
                    bk, rb = nbank()
                    for kc in range(8):
                        P.op("pe", lambda e, bk=bk, i=i, kc=kc, cc=cc, s=s, n=n: e.matmul(
                            bk[:, 0:n], wbf[i][:, kc, cc * 128:(cc + 1) * 128], hT[:, kc, s:s + n], start=(kc == 0), stop=(kc == 7)),
                            reads=[r_wbf[i], rh[tb]], writes=[rb])
                    if gc < 14:
                        off = 1 + s if tb < 4 else 2 + s
                        P.op("act", lambda e, bk=bk, off=off, n=n: e.copy(out=pf[:, off:off + n], in_=bk[:, 0:n]),
                             reads=[rb], writes=[r_pf])
                    elif gc < 20:
                        P.op("act", lambda e, bk=bk, o=o, s=s, n=n: e.copy(out=po[o][:, s:s + n], in_=bk[:, 0:n]),
                             reads=[rb], writes=[r_po[o]])
                    else:
                        P.op("act", lambda e, bk=bk, o=o, s=s, n=n: e.activation(out=pg[o][:, s:s + n], in_=bk[:, 0:n], func=AF.Sigmoid),
                             reads=[rb], writes=[r_pg[o]])
                if gc < 14:
                    muc = dpar[:, l * 32 + gc: l * 32 + gc + 1]
                    mu0 = pcol(l, "mu0", gc)
                    mu1 = pcol(l, "mu1", gc)
                    segs = [(0, LAT, 1)] + ([(LAT, CTX, 2 + LAT)] if nctx else [])
                    for (os_, n, ps_) in segs:
                        P.op("act", lambda e, o=o, os_=os_, n=n, ps_=ps_, muc=muc: e.activation(
                            out=po[o][:, os_:os_ + n], in_=pf[:, ps_:ps_ + n], func=AF.Identity, scale=muc, bias=cst[:, 3:4]),
                            reads=[r_pf, r_dpar, r_cst], writes=[r_po[o]])
                        P.op("dve", lambda e, o=o, os_=os_, n=n, ps_=ps_, mu0=mu0: e.scalar_tensor_tensor(
                            out=po[o][:, os_:os_ + n], in0=pf[:, ps_ - 1:ps_ - 1 + n], scalar=mu0, in1=po[o][:, os_:os_ + n],
                            op0=ALU.mult, op1=ALU.add), reads=[r_pf, r_par, r_po[o]], writes=[r_po[o]])
                        P.op("dve", lambda e, o=o, os_=os_, n=n, ps_=ps_, mu1=mu1: e.scalar_tensor_tensor(
                            out=po[o][:, os_:os_ + n], in0=pf[:, ps_ + 1:ps_ + 1 + n], scalar=mu1, in1=po[o][:, os_:os_ + n],
                            op0=ALU.mult, op1=ALU.add), reads=[r_pf, r_par, r_po[o]], writes=[r_po[o]])
                    P.dma("sp", PR_d[gc * 128:(gc + 1) * 128, :], po[o][:], reads=[r_po[o]], writes=[rPR[gc]])
                elif gc < 20:
                    P.dma("sp", PA_d[(gc - 14) * 128:(gc - 13) * 128, :], po[o][:], reads=[r_po[o]], writes=[rPA[gc - 14]])
                else:
                    P.dma("sp", PG_d[(gc - 20) * 128:(gc - 19) * 128, :], pg[o][:], reads=[r_pg[o]], writes=[rPG[gc - 20]])
        P.pop_scope()

    KR_d = dscr("KR", [128, NT], BF16)
    rKR = Res()

    def phase_attn(l, do_ctx):
        P.push_scope()
        cos_t = P.sb("at_cos", [128, LAT], F32)
        sin_t = P.sb("at_sin", [128, LAT], F32)
        r_cs = Res()
        P.dma("sp", cos_t[:], cos_d, writes=[r_cs])
        P.dma("sp", sin_t[:], sin_d, writes=[r_cs])
        ones_b = P.sb("at_ones", [128, 64], BF16)
        P.op("pool", lambda e: e.memset(ones_b[:], 1.0), writes=[r_cs])
        raw = P.sb("at_raw", [128, NT], F32)
        r_raw = Res()
        QR = [P.sb(f"at_qr{c}", [128, NT], BF16) for c in range(4)]
        r_QR = [Res() for _ in range(4)]
        krp = P.sb("at_krp", [128, NT], BF16)
        r_krp = Res()
        KX = [[P.sb(f"at_kx{g}{hf}", [128, NT], BF16) for hf in range(2)] for g in range(2)]
        r_KX = [[Res(), Res()], [Res(), Res()]]
        Vt = P.sb("at_vt", [128, 18, 128], BF16)
        r_Vt = Res()
        sq = P.sb("at_sq", [128, 512], BF16)
        r_sq = Res()
        rs = P.sb("at_rs", [128, 512], F32)
        r_rs = Res()
        qn = P.sb("at_qn", [128, 512], BF16)
        r_qn = Res()
        t1 = P.sb("at_t1", [128, 512], F32)
        r_t1 = Res()
        t2 = P.sb("at_t2", [128, 512], F32)
        r_t2 = Res()

        def proc_chunk(src_rows, gain, dst, r_dst):
            P.dma("sp", raw[:], PA_d[src_rows * 128:(src_rows + 1) * 128, :], reads=[rPA[src_rows]], writes=[r_raw])
            for tb in range(5):
                s, n = TBS[tb]
                P.op("act", lambda e: e.activation(out=sq[:, 0:n], in_=raw[:, s:s + n], func=AF.Square), reads=[r_raw], writes=[r_sq])
                bk, rb = nbank()
                P.op("pe", lambda e: e.matmul(bk[:, 0:n], onesbd_b[:], sq[:, 0:n], start=True, stop=True), reads=[r_sq, r_const], writes=[rb])
                P.op("act", lambda e: e.activation(out=rs[:, 0:n], in_=bk[:, 0:n], func=AF.Sqrt, scale=1.0 / 64, bias=cst[:, 0:1]),
                     reads=[rb, r_cst], writes=[r_rs])
                P.op("dve", lambda e: e.reciprocal(out=rs[:, 0:n], in_=rs[:, 0:n]), reads=[r_rs], writes=[r_rs])
                if tb < 4:
                    P.op("dve", lambda e: e.scalar_tensor_tensor(out=qn[:, 0:n], in0=raw[:, s:s + n], scalar=gain, in1=rs[:, 0:n],
                                                                op0=ALU.mult, op1=ALU.mult), reads=[r_raw, r_rs, r_par, r_dpar], writes=[r_qn])
                    bk2, rb2 = nbank()
                    P.op("pe", lambda e: e.matmul(bk2[:, 0:n], rot_b[:], qn[:, 0:n], start=True, stop=True), reads=[r_qn, r_const], writes=[rb2])
                    P.op("pool", lambda e: e.tensor_tensor(out=t1[:, 0:n], in0=qn[:, 0:n], in1=cos_t[:, s:s + n], op=ALU.mult),
                         reads=[r_qn, r_cs], writes=[r_t1])
                    P.op("dve", lambda e: e.tensor_tensor(out=t2[:, 0:n], in0=bk2[:, 0:n], in1=sin_t[:, s:s + n], op=ALU.mult),
                         reads=[rb2, r_cs], writes=[r_t2])
                    P.op("pool", lambda e: e.tensor_tensor(out=dst[:, s:s + n], in0=t1[:, 0:n], in1=t2[:, 0:n], op=ALU.add),
                         reads=[r_t1, r_t2], writes=[r_dst])
                else:
                    P.op("dve", lambda e: e.scalar_tensor_tensor(out=dst[:, s:s + n], in0=raw[:, s:s + n], scalar=gain, in1=rs[:, 0:n],
                                                                op0=ALU.mult, op1=ALU.mult), reads=[r_raw, r_rs, r_par, r_dpar], writes=[r_dst])

        qg = dpar[:, l * 32 + 18:l * 32 + 19]
        for c in range(4):
            proc_chunk(c, qg, QR[c], r_QR[c])
        proc_chunk(4, pcol(l, "kn"), krp, r_krp)
        P.dma("sp", KR_d, krp[:], reads=[r_krp], writes=[rKR])
        for g in range(2):
            for hf in range(2):
                P.op("pool", lambda e, g=g, hf=hf: e.memset(KX[g][hf][:], 0.0), writes=[r_KX[g][hf]])
                P.dma("sp", KX[g][hf][hf * 64:(hf + 1) * 64, :], KR_d[g * 64:(g + 1) * 64, :], reads=[rKR], writes=[r_KX[g][hf]])
        P.dma("sp", raw[:], PA_d[5 * 128:6 * 128, :], reads=[rPA[5]], writes=[r_raw])
        for k4 in range(0, 18, 4):
            nk = min(4, 18 - k4)
            bk, rb = nbank()
            for i in range(nk):
                kc = k4 + i
                P.op("pe", lambda e, i=i, kc=kc: e.transpose(bk[:, i * 128:(i + 1) * 128], raw[:, kc * 128:(kc + 1) * 128], ident_f[:]),
                     reads=[r_raw, r_const], writes=[rb])
            P.op("act", lambda e, k4=k4, nk=nk: e.copy(out=Vt[:, k4:k4 + nk, :].rearrange("p a b -> p (a b)"), in_=bk[:, 0:nk * 128]),
                 reads=[rb], writes=[r_Vt])
        if "attn" in dbg and l == 0:
            dq = dbg_tensor("dbg_qr", [512, NT], BF16)
            for c in range(4):
                P.dma("sp", dq[c * 128:(c + 1) * 128, :], QR[c][:], reads=[r_QR[c]])
            dk = dbg_tensor("dbg_kr", [128, NT], BF16)
            P.dma("sp", dk, krp[:], reads=[r_krp])
        pT = [P.sb(f"at_pT{i}", [128, 512], BF16) for i in range(3)]
        r_pT = [Res() for _ in range(3)]
        rec = P.sb("at_rec", [64, 512], F32)
        r_rec = Res()
        yo = [P.sb(f"at_yo{i}", [64, 512], BF16) for i in range(2)]
        r_yo = [Res(), Res()]
        cnt = 0
        for h in range(8):
            c, hf, g = h // 2, h % 2, h // 4
            for tb in range(5 if do_ctx else 4):
                s, n = TBS[tb]
                kcs = list(range(18)) if tb < 4 else [16, 17]
                io = cnt % 2
                bkO, rbO = banks[io * 2], rbank[io * 2]
                bkD, rbD = banks[io * 2 + 1], rbank[io * 2 + 1]
                for ki, kc in enumerate(kcs):
                    bkS, rbS = nbank(4, 8)
                    ip = (cnt * 18 + ki) % 3
                    P.op("pe", lambda e, bkS=bkS, kc=kc: e.matmul(bkS[:, 0:n], KX[g][hf][:, kc * 128:(kc + 1) * 128], QR[c][:, s:s + n],
                                                               start=True, stop=True), reads=[r_KX[g][hf], r_QR[c]], writes=[rbS])
                    P.op("act", lambda e, bkS=bkS, ip=ip: e.activation(out=pT[ip][:, 0:n], in_=bkS[:, 0:n], func=AF.Exp),
                         reads=[rbS], writes=[r_pT[ip]])
                    P.op("pe", lambda e, kc=kc, ip=ip, ki=ki: e.matmul(bkO[0:64, 0:n], Vt[:, kc, g * 64:(g + 1) * 64], pT[ip][:, 0:n],
                                                                      start=(ki == 0), stop=(ki == len(kcs) - 1)),
                         reads=[r_Vt, r_pT[ip]], writes=[rbO])
                    P.op("pe", lambda e, ip=ip, ki=ki: e.matmul(bkD[0:64, 0:n], ones_b[:], pT[ip][:, 0:n],
                                                               start=(ki == 0), stop=(ki == len(kcs) - 1)),
                         reads=[r_cs, r_pT[ip]], writes=[rbD])
                P.op("dve", lambda e: e.reciprocal(out=rec[:, 0:n], in_=bkD[0:64, 0:n]), reads=[rbD], writes=[r_rec])
                P.op("dve", lambda e, io=io: e.tensor_tensor(out=yo[io][:, 0:n], in0=bkO[0:64, 0:n], in1=rec[:, 0:n], op=ALU.mult),
                     reads=[rbO, r_rec], writes=[r_yo[io]])
                P.dma("sp", YA_d[h * 64:(h + 1) * 64, s:s + n], yo[io][:, 0:n], reads=[r_yo[io]], writes=[rYA[h // 2]])
                cnt += 1
        P.pop_scope()

    def load_cast(dst_ap, src_ap, stg_list, r_stg_list, ctr, r_dst, shape_slice):
        i = ctr[0] % len(stg_list)
        ctr[0] += 1
        st = shape_slice(stg_list[i])
        P.dma("sp", st, src_ap, writes=[r_stg_list[i]])
        P.op("pool", lambda e: e.tensor_copy(out=dst_ap, in_=st), reads=[r_stg_list[i]], writes=[r_dst])

    def phase_merge(l, jb, tbs):
        P.push_scope()
        stg = [P.sb(f"mg_stg{i}", [128, 1024], F32) for i in range(2)]
        r_stg = [Res(), Res()]
        ctr = [0]
        wpa = P.sb("mg_wpa", [128, 4, 1024], BF16)
        wpb = P.sb("mg_wpb", [128, 4, 1024], BF16)
        wo = P.sb("mg_wo", [128, 8, 1024], BF16)
        r_w = Res()
        for kc in range(4):
            load_cast(wpa[:, kc, :], w_pa_d[l, kc * 128:(kc + 1) * 128, :], stg, r_stg, ctr, r_w, lambda t: t[:])
            load_cast(wpb[:, kc, :], w_pb_d[l, kc * 128:(kc + 1) * 128, :], stg, r_stg, ctr, r_w, lambda t: t[:])
        for kc in range(8):
            load_cast(wo[:, kc, :], w_o_d[l, kc * 128:(kc + 1) * 128, :], stg, r_stg, ctr, r_w, lambda t: t[:])
        yr = P.sb("mg_yr", [128, 4, 512], BF16)
        ya = P.sb("mg_ya", [128, 4, 512], BF16)
        ga = P.sb("mg_ga", [128, 8, 512], BF16)
        gb = P.sb("mg_gb", [128, 8, 512], BF16)
        r_in = Res()
        z = P.sb("mg_z", [128, 8, 512], BF16)
        r_z = Res()
        ta = [P.sb(f"mg_ta{i}", [128, 512], F32) for i in range(2)]
        r_ta = [Res(), Res()]
        tb_ = [P.sb(f"mg_tb{i}", [128, 512], F32) for i in range(2)]
        r_tb = [Res(), Res()]
        for tb in tbs:
            s, n = TBS[tb]
            j = 4 if tb == 4 else jb
            P.dma("sp", yr[:, :, 0:n], YR_d.rearrange("(c p) t -> p c t", p=128)[:, :, s:s + n], reads=rYR, writes=[r_in])
            P.dma("sp", ya[:, :, 0:n], YA_d.rearrange("(c p) t -> p c t", p=128)[:, :, s:s + n], reads=rYA, writes=[r_in])
            P.dma("sp", ga[:, :, 0:n], PG_d[0:1024, :].rearrange("(c p) t -> p c t", p=128)[:, :, s:s + n], reads=rPG, writes=[r_in])
            P.dma("sp", gb[:, :, 0:n], PG_d[1024:2048, :].rearrange("(c p) t -> p c t", p=128)[:, :, s:s + n], reads=rPG, writes=[r_in])
            for dc in range(8):
                i = dc % 2
                bkA, rbA = nbank()
                for kc in range(4):
                    P.op("pe", lambda e, kc=kc: e.matmul(bkA[:, 0:n], wpa[:, kc, dc * 128:(dc + 1) * 128], yr[:, kc, 0:n],
                                                        start=(kc == 0), stop=(kc == 3)), reads=[r_w, r_in], writes=[rbA])
                bkB, rbB = nbank()
                for kc in range(4):
                    P.op("pe", lambda e, kc=kc: e.matmul(bkB[:, 0:n], wpb[:, kc, dc * 128:(dc + 1) * 128], ya[:, kc, 0:n],
                                                        start=(kc == 0), stop=(kc == 3)), reads=[r_w, r_in], writes=[rbB])
                P.op("dve", lambda e: e.tensor_tensor(out=ta[i][:, 0:n], in0=bkA[:, 0:n], in1=ga[:, dc, 0:n], op=ALU.mult),
                     reads=[rbA, r_in], writes=[r_ta[i]])
                P.op("dve", lambda e: e.tensor_tensor(out=tb_[i][:, 0:n], in0=bkB[:, 0:n], in1=gb[:, dc, 0:n], op=ALU.mult),
                     reads=[rbB, r_in], writes=[r_tb[i]])
                P.op("pool", lambda e: e.tensor_tensor(out=z[:, dc, 0:n], in0=ta[i][:, 0:n], in1=tb_[i][:, 0:n], op=ALU.add),
                     reads=[r_ta[i], r_tb[i]], writes=[r_z])
            for dc in range(8):
                bkO, rbO = nbank()
                for kc in range(8):
                    P.op("pe", lambda e, kc=kc: e.matmul(bkO[:, 0:n], wo[:, kc, dc * 128:(dc + 1) * 128], z[:, kc, 0:n],
                                                        start=(kc == 0), stop=(kc == 7)), reads=[r_w, r_z], writes=[rbO])
                gt = mdv[:, l, 2, dc, j:j + 1]
                P.op("dve", lambda e, gt=gt: e.scalar_tensor_tensor(out=xT[:, dc, s:s + n], in0=bkO[:, 0:n], scalar=gt, in1=xT[:, dc, s:s + n],
                                                                   op0=ALU.mult, op1=ALU.add), reads=[rbO, r_mdv, rx[tb]], writes=[rx[tb]])
        P.pop_scope()

    def phase_ffn(l, jb, tbs, moe):
        P.push_scope()
        hT = P.sb("hT2", [128, 8, NT], BF16)
        rh = [Res() for _ in TBS]
        if moe:
            wgT = P.sb("f1_wgT", [8, LAT], F32)
            r_wgT = Res()
            sel_all = P.sb("f1_sel", [8, NE, 128], F32)
            r_sel = Res()
        P.push_scope()
        sqb = [P.sb(f"f1_sq{i}", [128, 512], F32) for i in range(2)]
        r_sqb = [Res(), Res()]
        tmpb = [P.sb(f"f1_tmp{i}", [128, 512], F32) for i in range(2)]
        r_tmpb = [Res(), Res()]
        rstd = P.sb("f1_rstd", [128, 512], F32)
        r_rstd = Res()
        if moe:
            h32 = P.sb("f1_h32", [128, 8, 512], F32)
            r_h32 = Res()
            rt_f = P.sb("f1_rt", [128, 8, NE], F32)
            r_rt = Res()
            P.dma("sp", rt_f[:], router_d[0].rearrange("(kc p) e -> p kc e", p=128), writes=[r_rt])
            lgT = P.sb("f1_lgT", [8, 512], F32)
            r_lgT = Res()
            sm = P.sb("f1_sm", [128, 64], F32)
            r_sm = Res()
            for e_ in range(NE):
                P.op("dve", lambda e, e_=e_: e.tensor_copy(out=sel_all[:, e_, :], in_=bass.AP(ident_f, e_, [[128, 8], [0, 128]])),
                     reads=[r_const], writes=[r_sel])
        for tb in tbs:
            s, n = TBS[tb]
            if moe:
                norm_block(l, 1, jb, tb, hT, rh, sqb, r_sqb, tmpb, r_tmpb, rstd, r_rstd, hT32=h32, rh32=r_h32)
                bk, rb = nbank()
                for kc in range(8):
                    P.op("pe", lambda e, kc=kc: e.matmul(bk[0:8, 0:n], rt_f[:, kc, :], h32[:, kc, 0:n], start=(kc == 0), stop=(kc == 7)),
                         reads=[r_rt, r_h32], writes=[rb])
                P.op("act", lambda e: e.copy(out=lgT[:, 0:n], in_=bk[0:8, 0:n]), reads=[rb], writes=[r_lgT])
                bk2, rb2 = nbank()
                for sb_ in range(n // 128):
                    bk1, rb1 = nbank()
                    P.op("pe", lambda e: e.transpose(bk1[:, 0:8], lgT[:, sb_ * 128:(sb_ + 1) * 128], ident_f[0:8, 0:8]),
                         reads=[r_lgT, r_const], writes=[rb1])
                    lg, m1, eq, lg2, m2, sel, ex, den = (sm[:, 0:8], sm[:, 8:9], sm[:, 16:24], sm[:, 24:32], sm[:, 9:10], sm[:, 32:40],
                                                         sm[:, 40:48], sm[:, 10:11])
                    nm1 = sm[:, 11:12]
                    wg_ = sm[:, 48:56]
                    P.op("dve", lambda e: e.tensor_copy(out=lg, in_=bk1[:, 0:8]), reads=[rb1], writes=[r_sm])
                    P.op("dve", lambda e: e.tensor_reduce(out=m1, in_=lg, axis=AX.X, op=ALU.max), reads=[r_sm], writes=[r_sm])
                    P.op("dve", lambda e: e.tensor_scalar(out=eq, in0=lg, scalar1=m1, scalar2=-1e30, op0=ALU.is_equal, op1=ALU.mult),
                         reads=[r_sm], writes=[r_sm])
                    P.op("dve", lambda e: e.tensor_tensor(out=lg2, in0=lg, in1=eq, op=ALU.add), reads=[r_sm], writes=[r_sm])
                    P.op("dve", lambda e: e.tensor_reduce(out=m2, in_=lg2, axis=AX.X, op=ALU.max), reads=[r_sm], writes=[r_sm])
                    P.op("dve", lambda e: e.tensor_scalar(out=sel, in0=lg, scalar1=m2, scalar2=None, op0=ALU.is_ge), reads=[r_sm], writes=[r_sm])
                    P.op("dve", lambda e: e.tensor_scalar(out=nm1, in0=m1, scalar1=-1.0, scalar2=None, op0=ALU.mult), reads=[r_sm], writes=[r_sm])
                    P.op("act", lambda e: e.activation(out=ex, in_=lg, func=AF.Exp, scale=1.0, bias=nm1), reads=[r_sm], writes=[r_sm])
                    P.op("dve", lambda e: e.tensor_tensor(out=ex, in0=ex, in1=sel, op=ALU.mult), reads=[r_sm], writes=[r_sm])
                    P.op("dve", lambda e: e.tensor_reduce(out=den, in_=ex, axis=AX.X, op=ALU.add), reads=[r_sm], writes=[r_sm])
                    P.op("dve", lambda e: e.reciprocal(out=den, in_=den), reads=[r_sm], writes=[r_sm])
                    P.op("dve", lambda e: e.tensor_scalar(out=wg_, in0=ex, scalar1=den, scalar2=None, op0=ALU.mult), reads=[r_sm], writes=[r_sm])
                    P.op("pe", lambda e: e.transpose(bk2[0:8, sb_ * 128:(sb_ + 1) * 128], wg_, ident_f[:]), reads=[r_sm, r_const], writes=[rb2])
                P.op("act", lambda e: e.copy(out=wgT[:, s:s + n], in_=bk2[0:8, 0:n]), reads=[rb2], writes=[r_wgT])
            else:
                norm_block(l, 1, jb, tb, hT, rh, sqb, r_sqb, tmpb, r_tmpb, rstd, r_rstd)
        if "moe" in dbg and moe:
            dw = dbg_tensor("dbg_wgT", [8, LAT], F32)
            P.dma("sp", dw, wgT[:], reads=[r_wgT])
        P.pop_scope()
        NFI = 2
        stg = [P.sb(f"f2_stg{i}", [128, 1024], F32) for i in range(2)]
        r_stg = [Res() for _ in range(2)]
        ctr = [0]
        wgb = [P.sb(f"f2_wgb{i}", [128, 8, 128], BF16) for i in range(2)]
        wub = [P.sb(f"f2_wub{i}", [128, 8, 128], BF16) for i in range(2)]
        r_wgu = [Res(), Res()]
        wdb = [P.sb(f"f2_wdb{i}", [128, NFI, 1024], BF16) for i in range(2)]
        r_wdb = [Res(), Res()]
        actT = P.sb("f2_act", [128, NFI, NT], BF16)
        r_act = [Res() for _ in TBS]
        sl = [P.sb(f"f2_sl{i}", [128, 512], F32) for i in range(2)]
        r_sl = [Res(), Res()]
        sl2 = [P.sb(f"f2_sl2{i}", [128, 512], F32) for i in range(2)]
        r_sl2 = [Res(), Res()]
        if moe:
            wb = P.sb("f2_wb", [128, LAT], BF16)
            r_wb = Res()
        gi = 0
        ii = 0
        for ex_ in range(NE if moe else 1):
            if moe:
                wgs, wus, wds = moe_wg_d[0, ex_], moe_wu_d[0, ex_], moe_wd_d[0, ex_]
                for tb in tbs:
                    s, n = TBS[tb]
                    bk, rb = nbank()
                    P.op("pe", lambda e: e.matmul(bk[:, 0:n], sel_all[:, ex_, :], wgT[:, s:s + n], start=True, stop=True),
                         reads=[r_sel, r_wgT], writes=[rb])
                    P.op("act", lambda e: e.copy(out=wb[:, s:s + n], in_=bk[:, 0:n]), reads=[rb], writes=[r_wb])
            else:
                wgs, wus, wds = ffn_wg_d[0], ffn_wu_d[0], ffn_wd_d[0]
            wgs_r = wgs.rearrange("(kc p) f -> p kc f", p=128)
            wus_r = wus.rearrange("(kc p) f -> p kc f", p=128)
            for fg in range(FF // 128 // NFI):
                g2 = gi % 2
                gi += 1
                for fi in range(NFI):
                    fc = fg * NFI + fi
                    w2i = ii % 2
                    ii += 1
                    load_cast(wgb[w2i][:], wgs_r[:, :, fc * 128:(fc + 1) * 128], stg, r_stg, ctr, r_wgu[w2i],
                              lambda t: t[:].rearrange("p (a b) -> p a b", a=8))
                    load_cast(wub[w2i][:], wus_r[:, :, fc * 128:(fc + 1) * 128], stg, r_stg, ctr, r_wgu[w2i],
                              lambda t: t[:].rearrange("p (a b) -> p a b", a=8))
                    load_cast(wdb[g2][:, fi, :], wds[fc * 128:(fc + 1) * 128, :], stg, r_stg, ctr, r_wdb[g2], lambda t: t[:])
                    for tb in tbs:
                        s, n = TBS[tb]
                        si = (fi + tb) % 2
                        bkG, rbG = nbank()
                        for kc in range(8):
                            P.op("pe", lambda e, kc=kc: e.matmul(bkG[:, 0:n], wgb[w2i][:, kc, :], hT[:, kc, s:s + n], start=(kc == 0), stop=(kc == 7)),
                                 reads=[r_wgu[w2i], rh[tb]], writes=[rbG])
                        bkU, rbU = nbank()
                        for kc in range(8):
                            P.op("pe", lambda e, kc=kc: e.matmul(bkU[:, 0:n], wub[w2i][:, kc, :], hT[:, kc, s:s + n], start=(kc == 0), stop=(kc == 7)),
                                 reads=[r_wgu[w2i], rh[tb]], writes=[rbU])
                        P.op("act", lambda e: e.activation(out=sl[si][:, 0:n], in_=bkG[:, 0:n], func=AF.Silu), reads=[rbG], writes=[r_sl[si]])
                        if moe:
                            P.op("dve", lambda e: e.tensor_tensor(out=sl2[si][:, 0:n], in0=bkU[:, 0:n], in1=sl[si][:, 0:n], op=ALU.mult),
                                 reads=[rbU, r_sl[si]], writes=[r_sl2[si]])
                            P.op("pool", lambda e: e.tensor_tensor(out=actT[:, fi, s:s + n], in0=sl2[si][:, 0:n], in1=wb[:, s:s + n], op=ALU.mult),
                                 reads=[r_sl2[si], r_wb], writes=[r_act[tb]])
                        else:
                            P.op("dve", lambda e: e.tensor_tensor(out=actT[:, fi, s:s + n], in0=bkU[:, 0:n], in1=sl[si][:, 0:n], op=ALU.mult),
                                 reads=[rbU, r_sl[si]], writes=[r_act[tb]])
                for tb in tbs:
                    s, n = TBS[tb]
                    j = 4 if tb == 4 else jb
                    for dc in range(8):
                        bkO, rbO = nbank()
                        for fi in range(NFI):
                            P.op("pe", lambda e, fi=fi: e.matmul(bkO[:, 0:n], wdb[g2][:, fi, dc * 128:(dc + 1) * 128], actT[:, fi, s:s + n],
                                                                start=(fi == 0), stop=(fi == NFI - 1)), reads=[r_wdb[g2], r_act[tb]], writes=[rbO])
                        gt = mdv[:, l, 5, dc, j:j + 1]
                        P.op("dve", lambda e, gt=gt: e.scalar_tensor_tensor(out=xT[:, dc, s:s + n], in0=bkO[:, 0:n], scalar=gt, in1=xT[:, dc, s:s + n],
                                                                           op0=ALU.mult, op1=ALU.add), reads=[rbO, r_mdv, rx[tb]], writes=[rx[tb]])
        P.pop_scope()

    def phase_final(jb):
        P.push_scope()
        sqb = [P.sb(f"fn_sq{i}", [128, 512], F32) for i in range(2)]
        r_sqb = [Res(), Res()]
        ob = [P.sb(f"fn_o{i}", [128, 512], F32) for i in range(2)]
        r_ob = [Res(), Res()]
        rstd = P.sb("fn_rstd", [128, 512], F32)
        r_rstd = Res()
        for tb in range(4):
            s, n = TBS[tb]
            bk, rb = nbank()
            for c in range(8):
                i = c % 2
                P.op("act", lambda e: e.activation(out=sqb[i][:, 0:n], in_=xT[:, c, s:s + n], func=AF.Square), reads=[rx[tb]], writes=[r_sqb[i]])
                P.op("pe", lambda e: e.matmul(bk[:, 0:n], ones_f[:], sqb[i][:, 0:n], start=(c == 0), stop=(c == 7)),
                     reads=[r_sqb[i], r_const], writes=[rb])
            P.op("act", lambda e: e.activation(out=rstd[:, 0:n], in_=bk[:, 0:n], func=AF.Sqrt, scale=1.0 / D, bias=cst[:, 0:1]),
                 reads=[rb, r_cst], writes=[r_rstd])
            P.op("dve", lambda e: e.reciprocal(out=rstd[:, 0:n], in_=rstd[:, 0:n]), reads=[r_rstd], writes=[r_rstd])
            for c in range(8):
                i = c % 2
                fnc = par[:, 2 * NPL + c:2 * NPL + c + 1]
                P.op("dve", lambda e: e.scalar_tensor_tensor(out=ob[i][:, 0:n], in0=xT[:, c, s:s + n], scalar=fnc, in1=rstd[:, 0:n],
                                                            op0=ALU.mult, op1=ALU.mult), reads=[rx[tb], r_par, r_rstd], writes=[r_ob[i]])
                P.dma("sp", outT_d[jb, c * 128:(c + 1) * 128, s:s + n], ob[i][:, 0:n], reads=[r_ob[i]])
        P.pop_scope()

    RB = 256

    def phase_rwkv(l):
        P.push_scope()
        twT = P.sb("rw_tw", [64, NT], BF16)
        adT = P.sb("rw_ad", [64, NT], BF16)
        gsT = P.sb("rw_gs", [128, NT], BF16)
        r_lora = Res()
        w2b = [P.sb(f"rw_w2b{d}", [64, 512], BF16) for d in range(2)]
        a2b = [P.sb(f"rw_a2b{d}", [64, 512], BF16) for d in range(2)]
        g2b = P.sb("rw_g2b", [128, 512], BF16)
        mk = P.sb("rw_mk", [128, 2560], BF16)
        rmask = P.sb("rw_rmask", [128, 512], F32)
        r_w = Res()
        P.push_scope()
        stg = P.sb("rw_stg", [128, NT], F32)
        r_stg = Res()
        P.dma("sp", stg[0:64, :], PR_d[1536:1600, :], reads=[rPR[12]], writes=[r_stg])
        P.op("act", lambda e: e.activation(out=twT[:], in_=stg[0:64, :], func=AF.Tanh), reads=[r_stg], writes=[r_lora])
        P.dma("sp", stg[0:64, :], PR_d[1600:1664, :], reads=[rPR[12]], writes=[r_stg])
        P.op("act", lambda e: e.copy(out=adT[:], in_=stg[0:64, :]), reads=[r_stg], writes=[r_lora])
        P.dma("sp", stg[:], PR_d[1664:1792, :], reads=[rPR[13]], writes=[r_stg])
        P.op("act", lambda e: e.activation(out=gsT[:], in_=stg[:], func=AF.Sigmoid), reads=[r_stg], writes=[r_lora])
        for d in range(2):
            P.dma("sp", stg[0:64, 0:512], w2_d[l, d], writes=[r_stg])
            P.op("dve", lambda e: e.tensor_copy(out=w2b[d][:], in_=stg[0:64, 0:512]), reads=[r_stg], writes=[r_w])
            P.dma("sp", stg[0:64, 0:512], a2_d[l, d], writes=[r_stg])
            P.op("dve", lambda e: e.tensor_copy(out=a2b[d][:], in_=stg[0:64, 0:512]), reads=[r_stg], writes=[r_w])
        P.dma("sp", stg[:, 0:512], g2_d[l], writes=[r_stg])
        P.op("dve", lambda e: e.tensor_copy(out=g2b[:], in_=stg[:, 0:512]), reads=[r_stg], writes=[r_w])
        for i in range(5):
            P.dma("sp", stg[:, 0:512], mask_d[:, i * 512:(i + 1) * 512], writes=[r_stg])
            P.op("dve", lambda e: e.tensor_copy(out=mk[:, i * 512:(i + 1) * 512], in_=stg[:, 0:512]), reads=[r_stg], writes=[r_w])
        P.dma("sp", rmask[:], rmask_d, writes=[r_w])
        P.pop_scope()

        rT = P.sb("rw_r", [128, NT], F32)
        kT = P.sb("rw_k", [128, NT], F32)
        vT = P.sb("rw_v", [128, NT], F32)
        kkT = P.sb("rw_kk", [128, NT], F32)
        r_rkv = Res()
        r_kk = Res()
        Yacc = P.sb("rw_yacc", [128, NT], F32)
        r_Y = Res()
        yob = [(P.sb(f"rw_yob{i}", [128, RB], BF16), Res()) for i in range(2)]
        Vp = P.sb("rw_vp", [128, 18, 256], BF16)
        r_Vp = Res()
        P.op("pool", lambda e: e.memset(Vp[:], 0.0), writes=[r_Vp])

        def f32t(nm, w=RB):
            return P.sb(nm, [128, w], F32), Res()

        def b16t(nm, w=RB):
            return P.sb(nm, [128, w], BF16), Res()
        sg, r_sg = f32t("rw_sg")
        a_, r_a = f32t("rw_a")
        cs, r_cs_ = f32t("rw_cs")
        s1, r_s1 = f32t("rw_s1")
        s0, r_s0 = f32t("rw_s0")
        e0, r_e0 = f32t("rw_e0")
        e1, r_e1 = f32t("rw_e1")
        e2, r_e2 = f32t("rw_e2")
        e3, r_e3 = f32t("rw_e3")
        tt, r_tt = f32t("rw_tt")
        keys, r_keys = f32t("rw_keys")
        bb, r_bb = f32t("rw_bb")
        BhT, r_BhT = f32t("rw_BhT")
        KhT, r_KhT = f32t("rw_KhT")
        At, r_At = b16t("rw_At")
        Rt, r_Rt = b16t("rw_Rt")
        Bm1, r_Bm1 = b16t("rw_Bm1")
        Bm2, r_Bm2 = b16t("rw_Bm2")
        Km1, r_Km1 = b16t("rw_Km1")
        Km2, r_Km2 = b16t("rw_Km2")
        for t_, r_ in ((Bm1, r_Bm1), (Bm2, r_Bm2), (Km1, r_Km1), (Km2, r_Km2)):
            P.op("pool", lambda e, t_=t_: e.memset(t_[:], 0.0), writes=[r_])
        bsm = P.sb("rw_bsm", [128, 8], F32)
        r_bsm = Res()
        pnd = P.sb("rw_pnd", [128, 2], F32)
        pnm = P.sb("rw_pnm", [128, 2], F32)
        Hbm = [b16t(f"rw_Hbm{i}", 128) for i in range(2)]
        r_pnd = Res()
        NCH = RB // 128
        SC1a = [f32t(f"rw_SC1a_{i}", 256) for i in range(NCH)]
        SC1b = [b16t(f"rw_SC1b_{i}", 256) for i in range(NCH)]
        SC2 = [b16t(f"rw_SC2_{i}", 512) for i in range(NCH)]
        SA = [f32t(f"rw_SA_{i}", 256) for i in range(NCH)]
        TT = [f32t(f"rw_TT_{i}", 256) for i in range(NCH)]
        XX = [[f32t(f"rw_XX_{i}_{j}", 512) for j in range(2)] for i in range(NCH)]
        Bhp = [b16t(f"rw_Bhp_{i}", 256) for i in range(NCH)]
        Khp = [b16t(f"rw_Khp_{i}", 256) for i in range(NCH)]
        RHp = [f32t(f"rw_RHp_{i}", 256) for i in range(2)]
        Up = [b16t(f"rw_Up_{i}", 256) for i in range(2)]
        for lst in (Bhp, Khp, RHp, Up):
            for t_, r_ in lst:
                P.op("pool", lambda e, t_=t_: e.memset(t_[:], 0.0), writes=[r_])
        Hf = P.sb("rw_Hf", [128, 128], F32)
        Hbf = P.sb("rw_Hbf", [128, 128], BF16)
        r_Hf = Res()
        r_Hbf = Res()
        yc, r_yc = s0, r_s0
        sq, r_sq = e0, r_e0
        rsd, r_rsd = e1, r_e1
        aa0, r_aa0 = e2, r_e2
        aa1, r_aa1 = e3, r_e3

        def padcopy(dst_t, dst_off, pstride, src_ap, r_dst, r_src):
            out_ap = bass.AP(dst_t, dst_off, [[pstride, 128], [192, 2], [1, 64]])
            P.op("dve", lambda e: e.tensor_copy(out=out_ap, in_=src_ap.rearrange("p (h j) -> p h j", h=2)), reads=[r_src], writes=[r_dst])

        kkc = lambda j: pcol(l, "kk", j)
        for hp in range(4):
            P.dma("sp", rT[:], PR_d[hp * 128:(hp + 1) * 128, :], reads=[rPR[hp]], writes=[r_rkv])
            P.dma("sp", kT[:], PR_d[512 + hp * 128:512 + (hp + 1) * 128, :], reads=[rPR[4 + hp]], writes=[r_rkv])
            P.dma("sp", vT[:], PR_d[1024 + hp * 128:1024 + (hp + 1) * 128, :], reads=[rPR[8 + hp]], writes=[r_rkv])
            ka = pcol(l, "ka", hp)
            omka = dpar[:, l * 32 + 14 + hp:l * 32 + 15 + hp]
            for bs in range(0, NT, RB):
                n = RB
                P.op("dve", lambda e: e.tensor_scalar(out=tt[:], in0=kT[:, bs:bs + n], scalar1=kkc(hp), scalar2=None, op0=ALU.mult),
                     reads=[r_rkv, r_par], writes=[r_tt])
                P.op("act", lambda e: e.activation(out=sq[:], in_=tt[:], func=AF.Square), reads=[r_tt], writes=[r_sq])
                bk, rb = nbank()
                P.op("pe", lambda e: e.matmul(bk[:, 0:n], onesbd_f[:], sq[:], start=True, stop=True), reads=[r_sq, r_const], writes=[rb])
                P.op("act", lambda e: e.activation(out=rsd[:], in_=bk[:, 0:n], func=AF.Sqrt, scale=1.0, bias=cst[:, 4:5]),
                     reads=[rb, r_cst], writes=[r_rsd])
                P.op("dve", lambda e: e.reciprocal(out=rsd[:], in_=rsd[:]), reads=[r_rsd], writes=[r_rsd])
                P.op("dve", lambda e: e.tensor_tensor(out=kkT[:, bs:bs + n], in0=tt[:], in1=rsd[:], op=ALU.mult),
                     reads=[r_tt, r_rsd], writes=[r_kk])
            for d in range(2):
                P.op("pool", lambda e: e.memset(Hf[:], 0.0), writes=[r_Hf])
                P.op("pool", lambda e: e.memset(Hbf[:], 0.0), writes=[r_Hbf])
                lat = list(range(0, LAT, RB))
                order = [LAT] + (lat if d == 0 else lat[::-1])
                mo = d * 1280
                w0c = pcol(l, f"w0_{d}", hp)
                a0c = pcol(l, f"a0_{d}", hp)
                seqi = 0
                for bs in order:
                    n = RB
                    bk, rb = nbank()
                    P.op("pe", lambda e: e.matmul(bk[:, 0:n], w2b[d][:, hp * 128:(hp + 1) * 128], twT[:, bs:bs + n], start=True, stop=True),
                         reads=[r_w, r_lora], writes=[rb])
                    P.op("act", lambda e: e.activation(out=sg[:], in_=bk[:, 0:n], func=AF.Sigmoid, scale=1.0, bias=w0c),
                         reads=[rb, r_par], writes=[r_sg])
                    bk, rb = nbank()
                    P.op("pe", lambda e: e.matmul(bk[:, 0:n], a2b[d][:, hp * 128:(hp + 1) * 128], adT[:, bs:bs + n], start=True, stop=True),
                         reads=[r_w, r_lora], writes=[rb])
                    P.op("act", lambda e: e.activation(out=a_[:], in_=bk[:, 0:n], func=AF.Sigmoid, scale=1.0, bias=a0c),
                         reads=[rb, r_par], writes=[r_a])
                    P.op("dve", lambda e: e.tensor_tensor_scan(out=cs[:], data0=rmask[:, 0:n], data1=sg[:], initial=0.0, op0=ALU.mult, op1=ALU.add),
                         reads=[r_w, r_sg], writes=[r_cs_])
                    if d == 0:
                        sS, r_sS = cs, r_cs_
                        P.op("dve", lambda e: e.tensor_tensor(out=s0[:], in0=cs[:], in1=sg[:], op=ALU.subtract), reads=[r_cs_, r_sg], writes=[r_s0])
                    else:
                        for ci in range(NCH):
                            tot = cs[:, ci * 128 + 127:ci * 128 + 128]
                            P.op("dve", lambda e: e.tensor_scalar(out=s0[:, ci * 128:(ci + 1) * 128], in0=cs[:, ci * 128:(ci + 1) * 128],
                                                                 scalar1=-1.0, scalar2=tot, op0=ALU.mult, op1=ALU.add),
                                 reads=[r_cs_], writes=[r_s0])
                        P.op("dve", lambda e: e.tensor_tensor(out=s1[:], in0=s0[:], in1=sg[:], op=ALU.add), reads=[r_s0, r_sg], writes=[r_s1])
                        sS, r_sS = s1, r_s1
                    for ci in range(NCH):
                        tot = cs[:, ci * 128 + 127:ci * 128 + 128]
                        for q_, fac in enumerate((LW / 2, -LW / 2, LW)):
                            P.op("dve", lambda e: e.tensor_scalar(out=bsm[:, ci * 4 + q_:ci * 4 + q_ + 1], in0=tot, scalar1=fac, scalar2=None, op0=ALU.mult),
                                 reads=[r_cs_], writes=[r_bsm])
                        P.op("act", lambda e: e.activation(out=pnd[:, ci:ci + 1], in_=tot, func=AF.Exp, scale=LW), reads=[r_cs_], writes=[r_pnd])
                        P.op("act", lambda e: e.activation(out=pnm[:, ci:ci + 1], in_=tot, func=AF.Exp, scale=LW / 2), reads=[r_cs_], writes=[r_pnd])
                        cl = slice(ci * 128, (ci + 1) * 128)
                        mpos, mneg, cb_ = (bsm[:, ci * 4 + q_:ci * 4 + q_ + 1] for q_ in range(3))
                        P.op("act", lambda e: e.activation(out=e1[:, cl], in_=sS[:, cl], func=AF.Exp, scale=LW, bias=mneg), reads=[r_sS, r_bsm], writes=[r_e1])
                        P.op("act", lambda e: e.activation(out=e0[:, cl], in_=s0[:, cl], func=AF.Exp, scale=LW, bias=mneg), reads=[r_s0, r_bsm], writes=[r_e0])
                        P.op("act", lambda e: e.activation(out=e2[:, cl], in_=sS[:, cl], func=AF.Exp, scale=-LW, bias=mpos), reads=[r_sS, r_bsm], writes=[r_e2])
                        P.op("act", lambda e: e.activation(out=e3[:, cl], in_=sS[:, cl], func=AF.Exp, scale=-LW, bias=cb_), reads=[r_sS, r_bsm], writes=[r_e3])
                    P.op("dve", lambda e: e.tensor_scalar(out=tt[:], in0=a_[:], scalar1=ka, scalar2=omka, op0=ALU.mult, op1=ALU.add),
                         reads=[r_a, r_par, r_dpar], writes=[r_tt])
                    P.op("pool", lambda e: e.tensor_tensor(out=keys[:], in0=tt[:], in1=kT[:, bs:bs + n], op=ALU.mult), reads=[r_tt, r_rkv], writes=[r_keys])
                    P.op("pool", lambda e: e.tensor_tensor(out=bb[:], in0=kkT[:, bs:bs + n], in1=a_[:], op=ALU.mult), reads=[r_kk, r_a], writes=[r_bb])
                    P.op("dve", lambda e: e.scalar_tensor_tensor(out=At[:], in0=kkT[:, bs:bs + n], scalar=-1.0, in1=e0[:], op0=ALU.mult, op1=ALU.mult),
                         reads=[r_kk, r_e0], writes=[r_At])
                    P.op("pool", lambda e: e.tensor_tensor(out=Rt[:], in0=rT[:, bs:bs + n], in1=e1[:], op=ALU.mult), reads=[r_rkv, r_e1], writes=[r_Rt])
                    P.op("dve", lambda e: e.tensor_tensor(out=Bm1[0:64, :], in0=bb[0:64, :], in1=e2[0:64, :], op=ALU.mult), reads=[r_bb, r_e2], writes=[r_Bm1])
                    P.op("dve", lambda e: e.tensor_tensor(out=Bm2[64:128, :], in0=bb[64:128, :], in1=e2[64:128, :], op=ALU.mult), reads=[r_bb, r_e2], writes=[r_Bm2])
                    P.op("pool", lambda e: e.tensor_tensor(out=Km1[0:64, :], in0=keys[0:64, :], in1=e2[0:64, :], op=ALU.mult), reads=[r_keys, r_e2], writes=[r_Km1])
                    P.op("pool", lambda e: e.tensor_tensor(out=Km2[64:128, :], in0=keys[64:128, :], in1=e2[64:128, :], op=ALU.mult), reads=[r_keys, r_e2], writes=[r_Km2])
                    P.op("dve", lambda e: e.tensor_tensor(out=BhT[:], in0=bb[:], in1=e3[:], op=ALU.mult), reads=[r_bb, r_e3], writes=[r_BhT])
                    P.op("pool", lambda e: e.tensor_tensor(out=KhT[:], in0=keys[:], in1=e3[:], op=ALU.mult), reads=[r_keys, r_e3], writes=[r_KhT])
                    for ci in range(NCH):
                        cl = slice(ci * 128, (ci + 1) * 128)
                        kc = (bs + ci * 128) // 128
                        bk, rb = nbank()
                        P.op("pe", lambda e: e.transpose(bk[:, 0:128], BhT[:, cl], ident_f[:]), reads=[r_BhT, r_const], writes=[rb])
                        P.op("pe", lambda e: e.transpose(bk[:, 128:256], KhT[:, cl], ident_f[:]), reads=[r_KhT, r_const], writes=[rb])
                        if d == 0:
                            P.op("pe", lambda e: e.transpose(bk[:, 256:384], vT[:, bs + ci * 128:bs + (ci + 1) * 128], ident_f[:]),
                                 reads=[r_rkv, r_const], writes=[rb])
                            padcopy(Vp, kc * 256, 18 * 256, bk[:, 256:384], r_Vp, rb)
                        padcopy(Bhp[ci][0], 0, 256, bk[:, 0:128], Bhp[ci][1], rb)
                        padcopy(Khp[ci][0], 0, 256, bk[:, 128:256], Khp[ci][1], rb)
                        b1, rb1 = nbank()
                        b2, rb2 = nbank()
                        b3, rb3 = nbank()
                        for q_, (lt_, rl_) in enumerate(((Bm1, r_Bm1), (Bm2, r_Bm2), (Km1, r_Km1), (Km2, r_Km2))):
                            P.op("pe", lambda e: e.matmul(b1[:, q_ * 128:(q_ + 1) * 128], lt_[:, cl], At[:, cl], start=True, stop=True),
                                 reads=[rl_, r_At], writes=[rb1])
                            P.op("pe", lambda e: e.matmul(b2[:, q_ * 128:(q_ + 1) * 128], lt_[:, cl], Rt[:, cl], start=True, stop=True),
                                 reads=[rl_, r_Rt], writes=[rb2])
                        P.op("pe", lambda e: e.matmul(b3[:, 0:128], At[:, cl], Bm1[:, cl], start=True, stop=True), reads=[r_At, r_Bm1], writes=[rb3])
                        P.op("pe", lambda e: e.matmul(b3[:, 128:256], At[:, cl], Bm2[:, cl], start=True, stop=True), reads=[r_At, r_Bm2], writes=[rb3])
                        P.op("dve", lambda e: e.tensor_tensor(out=SC1a[ci][0][:], in0=b1[:, 0:256], in1=mk[:, mo:mo + 256], op=ALU.mult),
                             reads=[rb1, r_w], writes=[SC1a[ci][1]])
                        P.op("dve", lambda e: e.tensor_tensor(out=SC1b[ci][0][:], in0=b1[:, 256:512], in1=mk[:, mo + 256:mo + 512], op=ALU.mult),
                             reads=[rb1, r_w], writes=[SC1b[ci][1]])
                        P.op("dve", lambda e: e.tensor_tensor(out=SC2[ci][0][:], in0=b2[:], in1=mk[:, mo + 512:mo + 1024], op=ALU.mult),
                             reads=[rb2, r_w], writes=[SC2[ci][1]])
                        P.op("dve", lambda e: e.tensor_tensor(out=SA[ci][0][:], in0=b3[:, 0:256], in1=mk[:, mo + 1024:mo + 1280], op=ALU.mult),
                             reads=[rb3, r_w], writes=[SA[ci][1]])
                        P.op("pool", lambda e: e.tensor_tensor(out=TT[ci][0][:], in0=SC1a[ci][0][:], in1=ident2_f[:], op=ALU.add),
                             reads=[SC1a[ci][1], r_const], writes=[TT[ci][1]])
                    for j in range(1, 7):
                        for ci in range(NCH):
                            if j == 1:
                                Xs, rXs, XTs, rXTs, xo, xto = SA[ci][0], SA[ci][1], SC1a[ci][0], SC1a[ci][1], 0, 0
                            else:
                                Xs, rXs = XX[ci][j % 2]
                                XTs, rXTs, xo, xto = Xs, rXs, 0, 256
                            Xn, rXn = XX[ci][(j + 1) % 2]
                            bn, rbn = nbank()
                            for h in range(2):
                                hs = slice(h * 128, (h + 1) * 128)
                                Xh = Xs[:, xo + h * 128:xo + (h + 1) * 128]
                                XTh = XTs[:, xto + h * 128:xto + (h + 1) * 128]
                                P.op("pe", lambda e: e.matmul(bn[:, hs], XTh, Xh, start=True, stop=True), reads=[rXs, rXTs], writes=[rbn])
                                if j < 6:
                                    P.op("pe", lambda e: e.matmul(bn[:, 256 + h * 128:256 + (h + 1) * 128], Xh, XTh, start=True, stop=True),
                                         reads=[rXs, rXTs], writes=[rbn])
                            w_ = 512 if j < 6 else 256
                            P.op("act", lambda e: e.copy(out=Xn[:, 0:w_], in_=bn[:, 0:w_]), reads=[rbn], writes=[rXn])
                            bt, rbt = nbank()
                            for h in range(2):
                                hs = slice(h * 128, (h + 1) * 128)
                                P.op("pe", lambda e: e.matmul(bt[:, hs], Xn[:, hs], TT[ci][0][:, hs], start=True, stop=True),
                                     reads=[rXn, TT[ci][1]], writes=[rbt])
                            P.op("dve", lambda e: e.tensor_tensor(out=TT[ci][0][:], in0=bt[:, 0:256], in1=TT[ci][0][:], op=ALU.add),
                                 reads=[rbt, TT[ci][1]], writes=[TT[ci][1]])
                    cis = list(range(NCH)) if d == 0 else list(range(NCH))[::-1]
                    for ci in cis:
                        cl = slice(ci * 128, (ci + 1) * 128)
                        c0 = bs + ci * 128
                        kc = c0 // 128
                        RH, rRH = RHp[seqi % 2]
                        U_, rU = Up[seqi % 2]
                        seqi += 1
                        Hm, rHm = Hbm[seqi % 2]
                        P.op("dve", lambda e: e.tensor_scalar(out=Hm[:], in0=Hf[:], scalar1=pnm[:, ci:ci + 1], scalar2=None, op0=ALU.mult),
                             reads=[r_Hf, r_pnd], writes=[rHm])
                        q1, rq1 = nbank()
                        P.op("pe", lambda e: e.matmul(q1[:, 0:128], At[:, cl], Hm[:], start=True, stop=False), reads=[r_At, rHm], writes=[rq1])
                        P.op("pe", lambda e: e.matmul(q1[:, 0:128], SC1b[ci][0][:, 0:128], Vp[:, kc, 0:128], start=False, stop=False),
                             reads=[SC1b[ci][1], r_Vp], writes=[rq1])
                        P.op("pe", lambda e: e.matmul(q1[:, 0:128], SC1b[ci][0][:, 128:256], Vp[:, kc, 128:256], start=False, stop=True),
                             reads=[SC1b[ci][1], r_Vp], writes=[rq1])
                        padcopy(RH, 0, 256, q1[:, 0:128], rRH, rq1)
                        q2, rq2 = nbank()
                        P.op("pe", lambda e: e.matmul(q2[:, 0:128], TT[ci][0][:, 0:128], RH[:, 0:128], start=True, stop=False),
                             reads=[TT[ci][1], rRH], writes=[rq2])
                        P.op("pe", lambda e: e.matmul(q2[:, 0:128], TT[ci][0][:, 128:256], RH[:, 128:256], start=False, stop=True),
                             reads=[TT[ci][1], rRH], writes=[rq2])
                        padcopy(U_, 0, 256, q2[:, 0:128], rU, rq2)
                        q4, rq4 = nbank()
                        P.op("pe", lambda e: e.matmul(q4[:, 0:128], Hm[:], Rt[:, cl], start=True, stop=False), reads=[rHm, r_Rt], writes=[rq4])
                        P.op("pe", lambda e: e.matmul(q4[:, 0:128], U_[:, 0:128], SC2[ci][0][:, 0:128], start=False, stop=False),
                             reads=[rU, SC2[ci][1]], writes=[rq4])
                        P.op("pe", lambda e: e.matmul(q4[:, 0:128], U_[:, 128:256], SC2[ci][0][:, 128:256], start=False, stop=False),
                             reads=[rU, SC2[ci][1]], writes=[rq4])
                        P.op("pe", lambda e: e.matmul(q4[:, 0:128], Vp[:, kc, 0:128], SC2[ci][0][:, 256:384], start=False, stop=False),
                             reads=[r_Vp, SC2[ci][1]], writes=[rq4])
                        P.op("pe", lambda e: e.matmul(q4[:, 0:128], Vp[:, kc, 128:256], SC2[ci][0][:, 384:512], start=False, stop=True),
                             reads=[r_Vp, SC2[ci][1]], writes=[rq4])
                        if d == 0:
                            P.op("act", lambda e: e.copy(out=Yacc[:, c0:c0 + 128], in_=q4[:, 0:128]), reads=[rq4], writes=[r_Y])
                        else:
                            P.op("dve", lambda e: e.tensor_tensor(out=Yacc[:, c0:c0 + 128], in0=q4[:, 0:128], in1=Yacc[:, c0:c0 + 128], op=ALU.add),
                                 reads=[rq4, r_Y], writes=[r_Y])
                        q3, rq3 = nbank()
                        P.op("pe", lambda e: e.matmul(q3[:, 0:128], Bhp[ci][0][:, 0:128], U_[:, 0:128], start=True, stop=False),
                             reads=[Bhp[ci][1], rU], writes=[rq3])
                        P.op("pe", lambda e: e.matmul(q3[:, 0:128], Bhp[ci][0][:, 128:256], U_[:, 128:256], start=False, stop=False),
                             reads=[Bhp[ci][1], rU], writes=[rq3])
                        P.op("pe", lambda e: e.matmul(q3[:, 0:128], Khp[ci][0][:, 0:128], Vp[:, kc, 0:128], start=False, stop=False),
                             reads=[Khp[ci][1], r_Vp], writes=[rq3])
                        P.op("pe", lambda e: e.matmul(q3[:, 0:128], Khp[ci][0][:, 128:256], Vp[:, kc, 128:256], start=False, stop=True),
                             reads=[Khp[ci][1], r_Vp], writes=[rq3])
                        P.op("dve", lambda e: e.scalar_tensor_tensor(out=Hf[:], in0=Hf[:], scalar=pnd[:, ci:ci + 1], in1=q3[:, 0:128],
                                                                    op0=ALU.mult, op1=ALU.add), reads=[r_Hf, r_pnd, rq3], writes=[r_Hf])
            if "rwkv" in dbg and l == 0:
                dy = dbg_out["dbg_ysum"] if "dbg_ysum" in dbg_out else dbg_tensor("dbg_ysum", [512, NT], F32)
                P.dma("sp", dy[hp * 128:(hp + 1) * 128, :], Yacc[:], reads=[r_Y])
            lw_, lb_, rk_ = pcol(l, "lnxw", hp), pcol(l, "lnxb", hp), pcol(l, "rk", hp)
            for bs in range(0, NT, RB):
                n = RB
                bk, rb = nbank()
                P.op("pe", lambda e: e.matmul(bk[:, 0:n], onesbd_f[:], Yacc[:, bs:bs + n], start=True, stop=True), reads=[r_Y, r_const], writes=[rb])
                P.op("dve", lambda e: e.scalar_tensor_tensor(out=yc[:], in0=bk[:, 0:n], scalar=-1.0 / 64, in1=Yacc[:, bs:bs + n],
                                                            op0=ALU.mult, op1=ALU.add), reads=[rb, r_Y], writes=[r_yc])
                P.op("act", lambda e: e.activation(out=sq[:], in_=yc[:], func=AF.Square), reads=[r_yc], writes=[r_sq])
                bk, rb = nbank()
                P.op("pe", lambda e: e.matmul(bk[:, 0:n], onesbd_f[:], sq[:], start=True, stop=True), reads=[r_sq, r_const], writes=[rb])
                P.op("act", lambda e: e.activation(out=rsd[:], in_=bk[:, 0:n], func=AF.Sqrt, scale=1.0 / 64, bias=cst[:, 1:2]),
                     reads=[rb, r_cst], writes=[r_rsd])
                P.op("dve", lambda e: e.reciprocal(out=rsd[:], in_=rsd[:]), reads=[r_rsd], writes=[r_rsd])
                P.op("dve", lambda e: e.tensor_tensor(out=yc[:], in0=yc[:], in1=rsd[:], op=ALU.mult), reads=[r_yc, r_rsd], writes=[r_yc])
                P.op("act", lambda e: e.activation(out=yc[:], in_=yc[:], func=AF.Identity, scale=lw_, bias=lb_), reads=[r_yc, r_par], writes=[r_yc])
                for d, (aa, r_aa) in enumerate(((aa0, r_aa0), (aa1, r_aa1))):
                    bk, rb = nbank()
                    P.op("pe", lambda e: e.matmul(bk[:, 0:n], a2b[d][:, hp * 128:(hp + 1) * 128], adT[:, bs:bs + n], start=True, stop=True),
                         reads=[r_w, r_lora], writes=[rb])
                    P.op("act", lambda e: e.activation(out=aa[:], in_=bk[:, 0:n], func=AF.Sigmoid, scale=1.0, bias=pcol(l, f"a0_{d}", hp)),
                         reads=[rb, r_par], writes=[r_aa])
                P.op("pool", lambda e: e.tensor_tensor(out=aa0[:], in0=aa0[:], in1=aa1[:], op=ALU.add), reads=[r_aa0, r_aa1], writes=[r_aa0])
                P.op("dve", lambda e: e.tensor_scalar(out=tt[:], in0=aa0[:], scalar1=0.5, scalar2=ka, op0=ALU.mult, op1=ALU.mult),
                     reads=[r_aa0, r_par], writes=[r_tt])
                P.op("dve", lambda e: e.tensor_scalar(out=tt[:], in0=tt[:], scalar1=omka, scalar2=None, op0=ALU.add), reads=[r_tt, r_dpar], writes=[r_tt])
                P.op("pool", lambda e: e.tensor_tensor(out=keys[:], in0=tt[:], in1=kT[:, bs:bs + n], op=ALU.mult), reads=[r_tt, r_rkv], writes=[r_keys])
                P.op("dve", lambda e: e.scalar_tensor_tensor(out=bb[:], in0=rT[:, bs:bs + n], scalar=rk_, in1=keys[:], op0=ALU.mult, op1=ALU.mult),
                     reads=[r_rkv, r_par, r_keys], writes=[r_bb])
                bk, rb = nbank()
                P.op("pe", lambda e: e.matmul(bk[:, 0:n], onesbd_f[:], bb[:], start=True, stop=True), reads=[r_bb, r_const], writes=[rb])
                P.op("dve", lambda e: e.tensor_tensor(out=sq[:], in0=bk[:, 0:n], in1=vT[:, bs:bs + n], op=ALU.mult), reads=[rb, r_rkv], writes=[r_sq])
                P.op("pool", lambda e: e.tensor_tensor(out=sq[:], in0=sq[:], in1=yc[:], op=ALU.add), reads=[r_sq, r_yc], writes=[r_sq])
                bk, rb = nbank()
                P.op("pe", lambda e: e.matmul(bk[:, 0:n], g2b[:, hp * 128:(hp + 1) * 128], gsT[:, bs:bs + n], start=True, stop=True),
                     reads=[r_w, r_lora], writes=[rb])
                yo_, r_yo_ = yob[(bs // RB) % 2]
                P.op("dve", lambda e: e.tensor_tensor(out=yo_[:], in0=bk[:, 0:n], in1=sq[:], op=ALU.mult), reads=[rb, r_sq], writes=[r_yo_])
                P.dma("sp", YR_d[hp * 128:(hp + 1) * 128, bs:bs + n], yo_[:], reads=[r_yo_], writes=[rYR[hp]])
        P.pop_scope()

    def load_x(jb):
        for c in range(8):
            P.dma("sp", xT[:, c, 0:LAT], xT_d[jb, c * 128:(c + 1) * 128, :], writes=[rx[0], rx[1], rx[2], rx[3]])
            P.dma("sp", xT[:, c, LAT:NT], cxT_d[jb, c * 128:(c + 1) * 128, :], writes=[rx[4]])

    def dump(name, src, shape, dtype, rs):
        d = dbg_tensor(name, shape, dtype)
        P.dma("sp", d, src, reads=rs)

    prologue()
    if "mod" in dbg:
        d = dbg_tensor("dbg_mod", [128, 2 * 48 * 5], F32)
        P.dma("sp", d, modT[:].rearrange("p a b c -> p (a b c)"), reads=[r_mod])
    for jb in range(nb):
        load_x(jb)
        for l in range(nlayers):
            last = l == nlayers - 1
            tbs = [0, 1, 2, 3, 4]
            phase_proj(l, jb, tbs)
            if stop_after == "proj":
                dump("dbg_PR", PR_d, [RC, NT], F32, rPR)
                dump("dbg_PA", PA_d, [768, NT], F32, rPA)
                dump("dbg_PG", PG_d, [2048, NT], BF16, rPG)
                break
            if "noattn" not in dbg:
                phase_attn(l, not last)
            if "norwkv" not in dbg:
                phase_rwkv(l)
            if stop_after == "mix" or ("mix" in dbg and l == 0 and jb == 0):
                dump(f"dbg_YA", YA_d, [512, NT], BF16, rYA)
                dump(f"dbg_YR", YR_d, [512, NT], BF16, rYR)
                if stop_after == "mix":
                    break
            mtbs = tbs if not last else [0, 1, 2, 3]
            phase_merge(l, jb, mtbs)
            if "x" in dbg and jb == 0:
                d_ = dbg_tensor(f"dbg_xmix{l}", [128, 8 * NT], F32)
                P.dma("sp", d_, xT[:].rearrange("p c t -> p (c t)"), reads=rx)
            phase_ffn(l, jb, mtbs, moe=(l % 2 == 1))
            if "x" in dbg and jb == 0:
                d_ = dbg_tensor(f"dbg_xffn{l}", [128, 8 * NT], F32)
                P.dma("sp", d_, xT[:].rearrange("p c t -> p (c t)"), reads=rx)
            if stop_after == f"l{l}":
                break
        else:
            phase_final(jb)
        if stop_after is not None:
            break
    P.barrier()
    ninstr = P.ninstr
    P.close()
    return nc, dbg_out, ninstr


def make_inmaps(inp, ncores=8, nb=4):
    consts = host_consts()
    params = host_params(inp)
    xT = np.ascontiguousarray(np.transpose(inp["x"], (0, 2, 1)))
    cxT = np.ascontiguousarray(np.transpose(inp["ctx"], (0, 2, 1)))
    maps = []
    for i in range(ncores):
        m = {}
        m["xT"] = xT[i * nb:(i + 1) * nb]
        m["cxT"] = cxT[i * nb:(i + 1) * nb]
        c5 = np.concatenate([inp["c"][i * nb:(i + 1) * nb], np.zeros((4 - nb, D), np.float32), inp["c_ctx"][None, :]], axis=0)
        m["cT"] = np.ascontiguousarray(c5.reshape(5, 8, 128).transpose(2, 1, 0))
        m["params"] = params
        m.update(consts)
        for k in ("ada_w", "w_in", "rwkv_w2", "rwkv_a2", "rwkv_g2", "w_pa", "w_pb", "w_o", "ffn_wg", "ffn_wu", "ffn_wd",
                  "router", "moe_wg", "moe_wu", "moe_wd"):
            m[k] = inp[k]
        maps.append(m)
    return maps


def kernel(**inputs):
    inp = {k: np.asarray(v) for k, v in inputs.items()}
    ncores, nb = 8, 4
    nc, _, _ = build(nb=nb, nlayers=2)
    maps = make_inmaps(inp, ncores=ncores, nb=nb)
    res = run_bass_kernel_spmd(nc, maps, core_ids=list(range(ncores)))
    outT = np.concatenate([np.asarray(r["outT"]) for r in res.results], axis=0)
    return np.ascontiguousarray(np.transpose(outT, (0, 2, 1))).astype(np.float32)
```

```python
import contextlib
import numpy as np
import concourse.bass as bass
import concourse.mybir as mybir
from concourse.bass_utils import run_bass_kernel_spmd

F32 = mybir.dt.float32
BF16 = mybir.dt.bfloat16
AF = mybir.ActivationFunctionType
ALU = mybir.AluOpType
AX = mybir.AxisListType

ENGS = ("pe", "act", "dve", "pool", "sp")
EPOCH = 30000
NDMASEM = 48

D = 1024
LAT = 2048
CTX = 256
NT = LAT + CTX
NIN = 4608
RC = 1792
FF = 3584
NE = 8
LW = -0.6065306597126334
TBS = [(0, 512), (512, 512), (1024, 512), (1536, 512), (2048, 256)]
NPL = 130


class Res:
    __slots__ = ("last_w", "readers")

    def __init__(self):
        self.last_w = None
        self.readers = []


class Prog:
    def __init__(self, nc):
        self.nc = nc
        self.es = contextlib.ExitStack()
        self.seq = {e: 0 for e in ENGS}
        self.sems = []
        self.engsem = {}
        self.known = {e: {} for e in ENGS}
        self.dmasem = []
        self.ndma = 0
        self.dma_last = {}
        self.ninstr = 0
        self.eobj = {"pe": nc.tensor, "act": nc.scalar, "dve": nc.vector, "pool": nc.gpsimd, "sp": nc.sync}
        self.scope = [self.es]

    def new_sem(self, name):
        h = self.es.enter_context(self.nc.semaphore(name))
        self.sems.append(h)
        return len(self.sems) - 1

    def push_scope(self):
        st = contextlib.ExitStack()
        self.scope.append(st)

    def pop_scope(self):
        self.barrier()
        self.scope.pop().close()

    def sb(self, name, shape, dt):
        self.ntile = getattr(self, "ntile", 0) + 1
        return self.scope[-1].enter_context(self.nc.sbuf_tensor(f"{name}_{self.ntile}", list(shape), dt))

    def ps(self, name, shape, dt):
        return self.es.enter_context(self.nc.psum_tensor(name, list(shape), dt))

    def _waits_for(self, eng, reads, writes):
        need = {}
        known = self.known[eng]

        def add(key):
            if key is None:
                return
            s, v, ke = key
            if ke == eng and eng in ("pe", "sp"):
                return
            if known.get(s, 0) >= v:
                return
            if need.get(s, 0) < v:
                need[s] = v

        for r in reads:
            add(r.last_w)
        for w in writes:
            add(w.last_w)
            for k in w.readers:
                add(k)
        for s, v in need.items():
            known[s] = v
        return list(need.items())

    def _emit(self, eng, waits, fn, s, inc):
        e = self.eobj[eng]
        for ws, wv in waits:
            e.wait_ge(self.sems[ws], wv)
        if fn is not None:
            fn(e).then_inc(self.sems[s], inc)

    def op(self, eng, fn, reads=(), writes=()):
        waits = self._waits_for(eng, reads, writes)
        self.seq[eng] += 1
        n = self.seq[eng]
        ep = (n - 1) // EPOCH
        if (eng, ep) not in self.engsem:
            self.engsem[(eng, ep)] = self.new_sem(f"s_{eng}_{ep}")
        s = self.engsem[(eng, ep)]
        v = (n - 1) % EPOCH + 1
        key = (s, v, eng)
        self._emit(eng, waits, fn, s, 1)
        for r in reads:
            r.readers.append(key)
        for w in writes:
            w.last_w = key
            w.readers = []
        self.ninstr += 1
        return key

    def dma(self, eng, out_ap, in_ap, reads=(), writes=()):
        if not self.dmasem:
            self.dmasem = [self.new_sem(f"s_dma_{i}") for i in range(NDMASEM)]
        j = self.ndma
        self.ndma += 1
        slot = j % NDMASEM
        s = self.dmasem[slot]
        v = 16 * (j // NDMASEM + 1)
        waits = self._waits_for(eng, reads, writes)
        prev = self.dma_last.get(slot)
        if prev is not None and self.known[eng].get(prev[0], 0) < prev[1]:
            d = dict(waits)
            d[prev[0]] = max(d.get(prev[0], 0), prev[1])
            self.known[eng][prev[0]] = prev[1]
            waits = list(d.items())
        key = (s, v, "dma")
        self.dma_last[slot] = key
        self._emit(eng, waits, lambda e: e.dma_start(out=out_ap, in_=in_ap), s, 16)
        for r in reads:
            r.readers.append(key)
        for w in writes:
            w.last_w = key
            w.readers = []
        self.ninstr += 1
        return key

    def barrier(self):
        keys = []
        for (eng, ep), s in self.engsem.items():
            if self.seq[eng] > 0 and ep == (self.seq[eng] - 1) // EPOCH:
                keys.append((s, (self.seq[eng] - 1) % EPOCH + 1))
        for slot, k in self.dma_last.items():
            keys.append((k[0], k[1]))
        for eng in ENGS:
            for s, v in keys:
                if self.known[eng].get(s, 0) < v:
                    self.known[eng][s] = v
                    self.eobj[eng].wait_ge(self.sems[s], v)

    def close(self):
        while len(self.scope) > 1:
            self.scope.pop().close()
        self.es.close()


def _colz(v):
    return np.ascontiguousarray(np.asarray(v, np.float32).reshape(-1, 128).T)


def host_consts():
    c = {}
    c["ident"] = np.eye(128, dtype=np.float32)
    bd = np.zeros((128, 128), np.float32)
    bd[:64, :64] = 1.0
    bd[64:, 64:] = 1.0
    c["onesbd"] = bd
    p = np.arange(128)[:, None]
    f = np.arange(128)[None, :]
    lt = (p < f).astype(np.float32)
    le = (p <= f).astype(np.float32)
    gt = (p > f).astype(np.float32)
    ge = (p >= f).astype(np.float32)
    c["mask"] = np.concatenate([
        np.tile(lt, (1, 4)), np.tile(le, (1, 4)), np.tile(gt, (1, 2)),
        np.tile(gt, (1, 4)), np.tile(ge, (1, 4)), np.tile(lt, (1, 2)),
    ], axis=1).astype(np.float32)
    R = np.zeros((128, 128), np.float32)
    for i in range(64):
        R[2 * i + 1, 2 * i] = -1.0
        R[2 * i, 2 * i + 1] = 1.0
    c["rot"] = R
    t = np.arange(LAT)
    row = (t // 64).astype(np.float32)
    col = (t % 64).astype(np.float32)
    inv = (10000.0 ** (-np.arange(0, 32, 2, dtype=np.float32) / 32.0)).astype(np.float32)
    ang = np.concatenate([row[:, None] * inv[None, :], col[:, None] * inv[None, :]], axis=1).astype(np.float32)
    cos = np.cos(ang).astype(np.float32)
    sin = np.sin(ang).astype(np.float32)
    cosf = np.repeat(cos, 2, axis=1).T
    sinf = np.repeat(sin, 2, axis=1).T
    c["cos"] = np.ascontiguousarray(np.concatenate([cosf, cosf], axis=0))
    c["sin"] = np.ascontiguousarray(np.concatenate([sinf, sinf], axis=0))
    rm = np.ones((128, 512), np.float32)
    rm[:, ::128] = 0.0
    c["rmask"] = rm
    return c


def host_params(inp):
    cols = []
    for l in range(2):
        cols += [_colz(inp["norm1"][l]), _colz(inp["norm2"][l]), _colz(inp["ada_b"][l]),
                 _colz(inp["shift_mu"][l, 0]), _colz(inp["shift_mu"][l, 1]),
                 _colz(inp["rwkv_w0"][l, 0]), _colz(inp["rwkv_w0"][l, 1]),
                 _colz(inp["rwkv_a0"][l, 0]), _colz(inp["rwkv_a0"][l, 1]),
                 _colz(inp["rwkv_kk"][l]), _colz(inp["rwkv_ka"][l]), _colz(inp["rwkv_rk"][l]),
                 _colz(inp["lnx_w"][l]), _colz(inp["lnx_b"][l]),
                 _colz(np.tile(inp["q_norm"][l], 2)), _colz(np.tile(inp["k_norm"][l], 2))]
    cols.append(_colz(inp["final_norm"]))
    return np.ascontiguousarray(np.concatenate(cols, axis=1))


PO = {}
_o = 0
for _n, _w in [("norm1", 8), ("norm2", 8), ("adab", 48), ("mu0", 14), ("mu1", 14), ("w0_0", 4), ("w0_1", 4),
               ("a0_0", 4), ("a0_1", 4), ("kk", 4), ("ka", 4), ("rk", 4), ("lnxw", 4), ("lnxb", 4), ("qn", 1), ("kn", 1)]:
    PO[_n] = _o
    _o += _w
assert _o == NPL


def build(nb=4, nlayers=2, dbg=None, stop_after=None):
    dbg = dbg or set()
    nc = bass.Bass("TRN2", target_bir_lowering=False)
    P = Prog(nc)
    dt = nc.dram_tensor

    def din(name, shape, dtype=F32):
        return dt(name, list(shape), dtype, kind="ExternalInput").ap()

    def dscr(name, shape, dtype):
        return dt(name, list(shape), dtype, kind="Internal").ap()

    xT_d = din("xT", [nb, D, LAT])
    cxT_d = din("cxT", [nb, D, CTX])
    cT_d = din("cT", [128, 8, 5])
    par_d = din("params", [128, 2 * NPL + 8])
    ident_d = din("ident", [128, 128])
    onesbd_d = din("onesbd", [128, 128])
    mask_d = din("mask", [128, 2560])
    rot_d = din("rot", [128, 128])
    cos_d = din("cos", [128, LAT])
    sin_d = din("sin", [128, LAT])
    rmask_d = din("rmask", [128, 512])
    ada_w_d = din("ada_w", [2, D, 6 * D])
    w_in_d = din("w_in", [2, D, NIN])
    w2_d = din("rwkv_w2", [2, 2, 64, 512])
    a2_d = din("rwkv_a2", [2, 2, 64, 512])
    g2_d = din("rwkv_g2", [2, 128, 512])
    w_pa_d = din("w_pa", [2, 512, D])
    w_pb_d = din("w_pb", [2, 512, D])
    w_o_d = din("w_o", [2, D, D])
    ffn_wg_d = din("ffn_wg", [1, D, FF])
    ffn_wu_d = din("ffn_wu", [1, D, FF])
    ffn_wd_d = din("ffn_wd", [1, FF, D])
    router_d = din("router", [1, D, NE])
    if nlayers > 1:
        moe_wg_d = din("moe_wg", [1, NE, D, FF])
        moe_wu_d = din("moe_wu", [1, NE, D, FF])
        moe_wd_d = din("moe_wd", [1, NE, FF, D])
    outT_d = dt("outT", [nb, D, LAT], F32, kind="ExternalOutput").ap()

    PR_d = dscr("PR", [RC, NT], F32)
    PA_d = dscr("PA", [768, NT], F32)
    PG_d = dscr("PG", [2048, NT], BF16)
    YR_d = dscr("YR", [512, NT], BF16)
    YA_d = dscr("YA", [512, NT], BF16)
    rPR = [Res() for _ in range(14)]
    rPA = [Res() for _ in range(6)]
    rPG = [Res() for _ in range(16)]
    rYR = [Res() for _ in range(4)]
    rYA = [Res() for _ in range(4)]
    dbg_out = {}

    def dbg_tensor(name, shape, dtype=F32):
        dbg_out[name] = dt(name, list(shape), dtype, kind="ExternalOutput").ap()
        return dbg_out[name]

    xT = P.sb("xT_sb", [128, 8, NT], F32)
    rx = [Res() for _ in TBS]
    par = P.sb("par", [128, 2 * NPL + 8], F32)
    r_par = Res()
    dpar = P.sb("dpar", [128, 64], F32)
    r_dpar = Res()
    cst = P.sb("cst", [128, 8], F32)
    r_cst = Res()
    modT = P.sb("modT", [128, 2, 48, 5], F32)
    r_mod = Res()
    mdv = P.sb("mdv", [128, 2, 6, 8, 5], F32)
    r_mdv = Res()
    ident_f = P.sb("ident_f", [128, 128], F32)
    ident_b = P.sb("ident_b", [128, 128], BF16)
    ident2_f = P.sb("ident2_f", [128, 256], F32)
    onesbd_f = P.sb("onesbd_f", [128, 128], F32)
    onesbd_b = P.sb("onesbd_b", [128, 128], BF16)
    ones_f = P.sb("ones_f", [128, 128], F32)
    rot_b = P.sb("rot_b", [128, 128], BF16)
    r_const = Res()

    banks = [P.ps(f"bank{i}", [128, 512], F32) for i in range(8)]
    rbank = [Res() for _ in range(8)]
    bank_ctr = [0]

    def nbank(lo=0, hi=8):
        i = lo + bank_ctr[0] % (hi - lo)
        bank_ctr[0] += 1
        return banks[i], rbank[i]

    def pcol(l, name, j=0):
        o = l * NPL + PO[name] + j
        return par[:, o:o + 1]

    def prologue():
        P.push_scope()
        stg = P.sb("pl_stg", [128, 128], F32)
        r_stg = Res()
        P.dma("sp", par[:], par_d, writes=[r_par])
        P.dma("sp", ident_f[:], ident_d, writes=[r_const])
        P.dma("sp", onesbd_f[:], onesbd_d, writes=[r_const])
        P.dma("sp", stg[:], rot_d, writes=[r_stg])
        P.op("dve", lambda e: e.tensor_copy(out=rot_b[:], in_=stg[:]), reads=[r_stg], writes=[r_const])
        P.op("dve", lambda e: e.tensor_copy(out=ident_b[:], in_=ident_f[:]), reads=[r_const], writes=[r_const])
        P.op("dve", lambda e: e.tensor_copy(out=ident2_f[:, 0:128], in_=ident_f[:]), reads=[r_const], writes=[r_const])
        P.op("dve", lambda e: e.tensor_copy(out=ident2_f[:, 128:256], in_=ident_f[:]), reads=[r_const], writes=[r_const])
        P.op("dve", lambda e: e.tensor_copy(out=onesbd_b[:], in_=onesbd_f[:]), reads=[r_const], writes=[r_const])
        P.op("dve", lambda e: e.memset(ones_f[:], 1.0), writes=[r_const])
        P.op("dve", lambda e: e.memset(cst[:, 0:1], 1e-6), writes=[r_cst])
        P.op("dve", lambda e: e.memset(cst[:, 1:2], 64e-5), writes=[r_cst])
        P.op("dve", lambda e: e.memset(cst[:, 2:3], 1.0), writes=[r_cst])
        P.op("dve", lambda e: e.memset(cst[:, 3:4], 0.0), writes=[r_cst])
        P.op("dve", lambda e: e.memset(cst[:, 4:5], 1e-24), writes=[r_cst])
        for l in range(2):
            b0 = l * 32
            m0 = par[:, l * NPL + PO["mu0"]: l * NPL + PO["mu0"] + 14]
            m1 = par[:, l * NPL + PO["mu1"]: l * NPL + PO["mu1"] + 14]
            P.op("dve", lambda e, m0=m0, m1=m1, b0=b0: e.tensor_tensor(out=dpar[:, b0:b0 + 14], in0=m0, in1=m1, op=ALU.add),
                 reads=[r_par], writes=[r_dpar])
            P.op("dve", lambda e, b0=b0: e.tensor_scalar(out=dpar[:, b0:b0 + 14], in0=dpar[:, b0:b0 + 14], scalar1=-1.0, scalar2=1.0,
                                                        op0=ALU.mult, op1=ALU.add), reads=[r_dpar], writes=[r_dpar])
            ka = par[:, l * NPL + PO["ka"]: l * NPL + PO["ka"] + 4]
            P.op("dve", lambda e, ka=ka, b0=b0: e.tensor_scalar(out=dpar[:, b0 + 14:b0 + 18], in0=ka, scalar1=-1.0, scalar2=1.0,
                                                               op0=ALU.mult, op1=ALU.add), reads=[r_par], writes=[r_dpar])
            qn = pcol(l, "qn")
            P.op("dve", lambda e, qn=qn, b0=b0: e.tensor_scalar(out=dpar[:, b0 + 18:b0 + 19], in0=qn, scalar1=0.125, scalar2=None,
                                                               op0=ALU.mult), reads=[r_par], writes=[r_dpar])
        cact = P.sb("pl_cact", [128, 8, 5], F32)
        r_cact = Res()
        P.dma("sp", cact[:], cT_d, writes=[r_cact])
        P.op("act", lambda e: e.activation(out=cact[:], in_=cact[:], func=AF.Silu), reads=[r_cact], writes=[r_cact])
        aw = [P.sb(f"pl_aw{i}", [128, 8, 512], F32) for i in range(2)]
        r_aw = [Res(), Res()]
        for l in range(nlayers):
            for cb in range(12):
                i = cb % 2
                src = ada_w_d[l].rearrange("(kc p) n -> p kc n", p=128)[:, :, cb * 512:(cb + 1) * 512]
                P.dma("sp", aw[i][:], src, writes=[r_aw[i]])
                for cc in range(4):
                    ch = cb * 4 + cc
                    bk, rb = nbank()
                    for kc in range(8):
                        P.op("pe", lambda e, bk=bk, i=i, kc=kc, cc=cc: e.matmul(
                            bk[:, 0:5], aw[i][:, kc, cc * 128:(cc + 1) * 128], cact[:, kc, :], start=(kc == 0), stop=(kc == 7)),
                            reads=[r_aw[i], r_cact], writes=[rb])
                    bcol = pcol(l, "adab", ch)
                    P.op("dve", lambda e, bk=bk, l=l, ch=ch, bcol=bcol: e.tensor_scalar(
                        out=modT[:, l, ch, :], in0=bk[:, 0:5], scalar1=bcol, scalar2=None, op0=ALU.add),
                        reads=[rb, r_par], writes=[r_mod])
            for half, nm in ((0, "norm1"), (1, "norm2")):
                base = half * 24
                for c in range(8):
                    nrm = pcol(l, nm, c)
                    P.op("dve", lambda e, l=l, half=half, c=c, base=base, nrm=nrm: e.tensor_scalar(
                        out=mdv[:, l, half * 3 + 0, c, :], in0=modT[:, l, base + 8 + c, :], scalar1=1.0, scalar2=nrm,
                        op0=ALU.add, op1=ALU.mult), reads=[r_mod, r_par], writes=[r_mdv])
                    P.op("dve", lambda e, l=l, half=half, c=c, base=base: e.tensor_copy(
                        out=mdv[:, l, half * 3 + 1, c, :], in_=modT[:, l, base + c, :]), reads=[r_mod], writes=[r_mdv])
                    P.op("dve", lambda e, l=l, half=half, c=c, base=base: e.tensor_copy(
                        out=mdv[:, l, half * 3 + 2, c, :], in_=modT[:, l, base + 16 + c, :]), reads=[r_mod], writes=[r_mdv])
        P.pop_scope()

    def norm_block(l, half, jb, tb, hT, rh, sqb, r_sqb, tmpb, r_tmpb, rstd, r_rstd, hT32=None, rh32=None):
        s, n = TBS[tb]
        j = 4 if tb == 4 else jb
        bk, rb = nbank()
        for c in range(8):
            i = c % 2
            P.op("act", lambda e, c=c, i=i: e.activation(out=sqb[i][:, 0:n], in_=xT[:, c, s:s + n], func=AF.Square),
                 reads=[rx[tb]], writes=[r_sqb[i]])
            P.op("pe", lambda e, c=c, i=i, bk=bk: e.matmul(bk[:, 0:n], ones_f[:], sqb[i][:, 0:n], start=(c == 0), stop=(c == 7)),
                 reads=[r_sqb[i], r_const], writes=[rb])
        P.op("act", lambda e, bk=bk: e.activation(out=rstd[:, 0:n], in_=bk[:, 0:n], func=AF.Sqrt, scale=1.0 / D, bias=cst[:, 0:1]),
             reads=[rb, r_cst], writes=[r_rstd])
        P.op("dve", lambda e: e.reciprocal(out=rstd[:, 0:n], in_=rstd[:, 0:n]), reads=[r_rstd], writes=[r_rstd])
        for c in range(8):
            i = c % 2
            g = mdv[:, l, half * 3 + 0, c, j:j + 1]
            sh = mdv[:, l, half * 3 + 1, c, j:j + 1]
            P.op("dve", lambda e, c=c, i=i, g=g: e.scalar_tensor_tensor(out=tmpb[i][:, 0:n], in0=xT[:, c, s:s + n], scalar=g,
                                                                      in1=rstd[:, 0:n], op0=ALU.mult, op1=ALU.mult),
                 reads=[rx[tb], r_mdv, r_rstd], writes=[r_tmpb[i]])
            P.op("act", lambda e, c=c, i=i, sh=sh: e.activation(out=hT[:, c, s:s + n], in_=tmpb[i][:, 0:n], func=AF.Identity,
                                                                scale=1.0, bias=sh),
                 reads=[r_tmpb[i], r_mdv], writes=[rh[tb]])
            if hT32 is not None:
                P.op("pool", lambda e, c=c, i=i, sh=sh: e.tensor_scalar(out=hT32[:, c, 0:n], in0=tmpb[i][:, 0:n], scalar1=sh, scalar2=None,
                                                                       op0=ALU.add),
                     reads=[r_tmpb[i], r_mdv], writes=[rh32])

    def phase_proj(l, jb, tbs):
        P.push_scope()
        hT = P.sb("hT", [128, 8, NT], BF16)
        rh = [Res() for _ in TBS]
        sqb = [P.sb(f"p1_sq{i}", [128, 512], F32) for i in range(2)]
        r_sqb = [Res(), Res()]
        tmpb = [P.sb(f"p1_tmp{i}", [128, 512], F32) for i in range(2)]
        r_tmpb = [Res(), Res()]
        rstd = P.sb("p1_rstd", [128, 512], F32)
        r_rstd = Res()
        for tb in tbs:
            norm_block(l, 0, jb, tb, hT, rh, sqb, r_sqb, tmpb, r_tmpb, rstd, r_rstd)
        if "h" in dbg and l == 0:
            hd = dbg_tensor("dbg_h", [D, NT], BF16)
            for c in range(8):
                P.dma("sp", hd[c * 128:(c + 1) * 128, :], hT[:, c, :], reads=rh)
        wst = [P.sb(f"p2_wst{i}", [128, 8, 256], F32) for i in range(2)]
        r_wst = [Res(), Res()]
        wbf = [P.sb(f"p2_wbf{i}", [128, 8, 256], BF16) for i in range(2)]
        r_wbf = [Res(), Res()]
        pf = P.sb("p2_pf", [128, NT + 3], F32)
        r_pf = Res()
        po = [P.sb(f"p2_po{i}", [128, NT], F32) for i in range(2)]
        r_po = [Res(), Res()]
        pg = [P.sb(f"p2_pg{i}", [128, NT], BF16) for i in range(2)]
        r_pg = [Res(), Res()]
        P.op("pool", lambda e: e.memset(pf[:], 0.0), writes=[r_pf])
        wsrc = w_in_d[l].rearrange("(kc p) n -> p kc n", p=128)
        nctx = 4 in tbs
        for cg in range(18):
            i = cg % 2
            P.dma("sp", wst[i][:], wsrc[:, :, cg * 256:(cg + 1) * 256], writes=[r_wst[i]])
            P.op("pool", lambda e, i=i: e.tensor_copy(out=wbf[i][:], in_=wst[i][:]), reads=[r_wst[i]], writes=[r_wbf[i]])
            for cc in range(2):
                gc = cg * 2 + cc
                o = gc % 2
                for tb in tbs:
                    s, n = TBS[tb]
                    bk, rb = nbank()
                    for kc in range(8):
                        P.op("pe", lambda e, bk=bk, i=i, kc=kc, cc=cc, s=s, n=n: e.matmul(
                            bk[:, 0:n], wbf[i][:, kc, cc * 128:(cc + 1) * 128], hT[:, kc, s:s + n], start=(kc == 0), stop=(kc == 7)),
                            reads=[r_wbf[i], rh[tb]], writes=[rb])
                    if gc < 14:
                        off = 1 + s if tb < 4 else 2 + s
                        P.op("act", lambda e, bk=bk, off=off, n=n: e.copy(out=pf[:, off:off + n], in_=bk[:, 0:n]),
                             reads=[rb], writes=[r_pf])
                    elif gc < 20:
                        P.op("act", lambda e, bk=bk, o=o, s=s, n=n: e.copy(out=po[o][:, s:s + n], in_=bk[:, 0:n]),
                             reads=[rb], writes=[r_po[o]])
                    else:
                        P.op("act", lambda e, bk=bk, o=o, s=s, n=n: e.activation(out=pg[o][:, s:s + n], in_=bk[:, 0:n], func=AF.Sigmoid),
                             reads=[rb], writes=[r_pg[o]])
                if gc < 14:
                    muc = dpar[:, l * 32 + gc: l * 32 + gc + 1]
                    mu0 = pcol(l, "mu0", gc)
                    mu1 = pcol(l, "mu1", gc)
                    segs = [(0, LAT, 1)] + ([(LAT, CTX, 2 + LAT)] if nctx else [])
                    for (os_, n, ps_) in segs:
                        P.op("act", lambda e, o=o, os_=os_, n=n, ps_=ps_, muc=muc: e.activation(
                            out=po[o][:, os_:os_ + n], in_=pf[:, ps_:ps_ + n], func=AF.Identity, scale=muc, bias=cst[:, 3:4]),
                            reads=[r_pf, r_dpar, r_cst], writes=[r_po[o]])
                        P.op("dve", lambda e, o=o, os_=os_, n=n, ps_=ps_, mu0=mu0: e.scalar_tensor_tensor(
                            out=po[o][:, os_:os_ + n], in0=pf[:, ps_ - 1:ps_ - 1 + n], scalar=mu0, in1=po[o][:, os_:os_ + n],
                            op0=ALU.mult, op1=ALU.add), reads=[r_pf, r_par, r_po[o]], writes=[r_po[o]])
                        P.op("dve", lambda e, o=o, os_=os_, n=n, ps_=ps_, mu1=mu1: e.scalar_tensor_tensor(
                            out=po[o][:, os_:os_ + n], in0=pf[:, ps_ + 1:ps_ + 1 + n], scalar=mu1, in1=po[o][:, os_:os_ + n],
                            op0=ALU.mult, op1=ALU.add), reads=[r_pf, r_par, r_po[o]], writes=[r_po[o]])
                    P.dma("sp", PR_d[gc * 128:(gc + 1) * 128, :], po[o][:], reads=[r_po[o]], writes=[rPR[gc]])
                elif gc < 20:
                    P.dma("sp", PA_d[(gc - 14) * 128:(gc - 13) * 128, :], po[o][:], reads=[r_po[o]], writes=[rPA[gc - 14]])
                else:
                    P.dma("sp", PG_d[(gc - 20) * 128:(gc - 19) * 128, :], pg[o][:], reads=[r_pg[o]], writes=[rPG[gc - 20]])
        P.pop_scope()

    KR_d = dscr("KR", [128, NT], BF16)
    rKR = Res()

    def phase_attn(l, do_ctx):
        P.push_scope()
        cos_t = P.sb("at_cos", [128, LAT], F32)
        sin_t = P.sb("at_sin", [128, LAT], F32)
        r_cs = Res()
        P.dma("sp", cos_t[:], cos_d, writes=[r_cs])
        P.dma("sp", sin_t[:], sin_d, writes=[r_cs])
        ones_b = P.sb("at_ones", [128, 64], BF16)
        P.op("pool", lambda e: e.memset(ones_b[:], 1.0), writes=[r_cs])
        raw = P.sb("at_raw", [128, NT], F32)
        r_raw = Res()
        QR = [P.sb(f"at_qr{c}", [128, NT], BF16) for c in range(4)]
        r_QR = [Res() for _ in range(4)]
        krp = P.sb("at_krp", [128, NT], BF16)
        r_krp = Res()
        KX = [[P.sb(f"at_kx{g}{hf}", [128, NT], BF16) for hf in range(2)] for g in range(2)]
        r_KX = [[Res(), Res()], [Res(), Res()]]
        Vt = P.sb("at_vt", [128, 18, 128], BF16)
        r_Vt = Res()
        sq = P.sb("at_sq", [128, 512], BF16)
        r_sq = Res()
        rs = P.sb("at_rs", [128, 512], F32)
        r_rs = Res()
        qn = P.sb("at_qn", [128, 512], BF16)
        r_qn = Res()
        t1 = P.sb("at_t1", [128, 512], F32)
        r_t1 = Res()
        t2 = P.sb("at_t2", [128, 512], F32)
        r_t2 = Res()

        def proc_chunk(src_rows, gain, dst, r_dst):
            P.dma("sp", raw[:], PA_d[src_rows * 128:(src_rows + 1) * 128, :], reads=[rPA[src_rows]], writes=[r_raw])
            for tb in range(5):
                s, n = TBS[tb]
                P.op("act", lambda e: e.activation(out=sq[:, 0:n], in_=raw[:, s:s + n], func=AF.Square), reads=[r_raw], writes=[r_sq])
                bk, rb = nbank()
                P.op("pe", lambda e: e.matmul(bk[:, 0:n], onesbd_b[:], sq[:, 0:n], start=True, stop=True), reads=[r_sq, r_const], writes=[rb])
                P.op("act", lambda e: e.activation(out=rs[:, 0:n], in_=bk[:, 0:n], func=AF.Sqrt, scale=1.0 / 64, bias=cst[:, 0:1]),
                     reads=[rb, r_cst], writes=[r_rs])
                P.op("dve", lambda e: e.reciprocal(out=rs[:, 0:n], in_=rs[:, 0:n]), reads=[r_rs], writes=[r_rs])
                if tb < 4:
                    P.op("dve", lambda e: e.scalar_tensor_tensor(out=qn[:, 0:n], in0=raw[:, s:s + n], scalar=gain, in1=rs[:, 0:n],
                                                                op0=ALU.mult, op1=ALU.mult), reads=[r_raw, r_rs, r_par, r_dpar], writes=[r_qn])
                    bk2, rb2 = nbank()
                    P.op("pe", lambda e: e.matmul(bk2[:, 0:n], rot_b[:], qn[:, 0:n], start=True, stop=True), reads=[r_qn, r_const], writes=[rb2])
                    P.op("pool", lambda e: e.tensor_tensor(out=t1[:, 0:n], in0=qn[:, 0:n], in1=cos_t[:, s:s + n], op=ALU.mult),
                         reads=[r_qn, r_cs], writes=[r_t1])
                    P.op("dve", lambda e: e.tensor_tensor(out=t2[:, 0:n], in0=bk2[:, 0:n], in1=sin_t[:, s:s + n], op=ALU.mult),
                         reads=[rb2, r_cs], writes=[r_t2])
                    P.op("pool", lambda e: e.tensor_tensor(out=dst[:, s:s + n], in0=t1[:, 0:n], in1=t2[:, 0:n], op=ALU.add),
                         reads=[r_t1, r_t2], writes=[r_dst])
                else:
                    P.op("dve", lambda e: e.scalar_tensor_tensor(out=dst[:, s:s + n], in0=raw[:, s:s + n], scalar=gain, in1=rs[:, 0:n],
                                                                op0=ALU.mult, op1=ALU.mult), reads=[r_raw, r_rs, r_par, r_dpar], writes=[r_dst])

        qg = dpar[:, l * 32 + 18:l * 32 + 19]
        for c in range(4):
            proc_chunk(c, qg, QR[c], r_QR[c])
        proc_chunk(4, pcol(l, "kn"), krp, r_krp)
        P.dma("sp", KR_d, krp[:], reads=[r_krp], writes=[rKR])
        for g in range(2):
            for hf in range(2):
                P.op("pool", lambda e, g=g, hf=hf: e.memset(KX[g][hf][:], 0.0), writes=[r_KX[g][hf]])
                P.dma("sp", KX[g][hf][hf * 64:(hf + 1) * 64, :], KR_d[g * 64:(g + 1) * 64, :], reads=[rKR], writes=[r_KX[g][hf]])
        P.dma("sp", raw[:], PA_d[5 * 128:6 * 128, :], reads=[rPA[5]], writes=[r_raw])
        for k4 in range(0, 18, 4):
            nk = min(4, 18 - k4)
            bk, rb = nbank()
            for i in range(nk):
                kc = k4 + i
                P.op("pe", lambda e, i=i, kc=kc: e.transpose(bk[:, i * 128:(i + 1) * 128], raw[:, kc * 128:(kc + 1) * 128], ident_f[:]),
                     reads=[r_raw, r_const], writes=[rb])
            P.op("act", lambda e, k4=k4, nk=nk: e.copy(out=Vt[:, k4:k4 + nk, :].rearrange("p a b -> p (a b)"), in_=bk[:, 0:nk * 128]),
                 reads=[rb], writes=[r_Vt])
        if "attn" in dbg and l == 0:
            dq = dbg_tensor("dbg_qr", [512, NT], BF16)
            for c in range(4):
                P.dma("sp", dq[c * 128:(c + 1) * 128, :], QR[c][:], reads=[r_QR[c]])
            dk = dbg_tensor("dbg_kr", [128, NT], BF16)
            P.dma("sp", dk, krp[:], reads=[r_krp])
        pT = [P.sb(f"at_pT{i}", [128, 512], BF16) for i in range(3)]
        r_pT = [Res() for _ in range(3)]
        rec = P.sb("at_rec", [64, 512], F32)
        r_rec = Res()
        yo = [P.sb(f"at_yo{i}", [64, 512], BF16) for i in range(2)]
        r_yo = [Res(), Res()]
        cnt = 0
        for h in range(8):
            c, hf, g = h // 2, h % 2, h // 4
            for tb in range(5 if do_ctx else 4):
                s, n = TBS[tb]
                kcs = list(range(18)) if tb < 4 else [16, 17]
                io = cnt % 2
                bkO, rbO = banks[io * 2], rbank[io * 2]
                bkD, rbD = banks[io * 2 + 1], rbank[io * 2 + 1]
                for ki, kc in enumerate(kcs):
                    bkS, rbS = nbank(4, 8)
                    ip = (cnt * 18 + ki) % 3
                    P.op("pe", lambda e, bkS=bkS, kc=kc: e.matmul(bkS[:, 0:n], KX[g][hf][:, kc * 128:(kc + 1) * 128], QR[c][:, s:s + n],
                                                               start=True, stop=True), reads=[r_KX[g][hf], r_QR[c]], writes=[rbS])
                    P.op("act", lambda e, bkS=bkS, ip=ip: e.activation(out=pT[ip][:, 0:n], in_=bkS[:, 0:n], func=AF.Exp),
                         reads=[rbS], writes=[r_pT[ip]])
                    P.op("pe", lambda e, kc=kc, ip=ip, ki=ki: e.matmul(bkO[0:64, 0:n], Vt[:, kc, g * 64:(g + 1) * 64], pT[ip][:, 0:n],
                                                                      start=(ki == 0), stop=(ki == len(kcs) - 1)),
                         reads=[r_Vt, r_pT[ip]], writes=[rbO])
                    P.op("pe", lambda e, ip=ip, ki=ki: e.matmul(bkD[0:64, 0:n], ones_b[:], pT[ip][:, 0:n],
                                                               start=(ki == 0), stop=(ki == len(kcs) - 1)),
                         reads=[r_cs, r_pT[ip]], writes=[rbD])
                P.op("dve", lambda e: e.reciprocal(out=rec[:, 0:n], in_=bkD[0:64, 0:n]), reads=[rbD], writes=[r_rec])
                P.op("dve", lambda e, io=io: e.tensor_tensor(out=yo[io][:, 0:n], in0=bkO[0:64, 0:n], in1=rec[:, 0:n], op=ALU.mult),
                     reads=[rbO, r_rec], writes=[r_yo[io]])
                P.dma("sp", YA_d[h * 64:(h + 1) * 64, s:s + n], yo[io][:, 0:n], reads=[r_yo[io]], writes=[rYA[h // 2]])
                cnt += 1
        P.pop_scope()

    def load_cast(dst_ap, src_ap, stg_list, r_stg_list, ctr, r_dst, shape_slice):
        i = ctr[0] % len(stg_list)
        ctr[0] += 1
        st = shape_slice(stg_list[i])
        P.dma("sp", st, src_ap, writes=[r_stg_list[i]])
        P.op("pool", lambda e: e.tensor_copy(out=dst_ap, in_=st), reads=[r_stg_list[i]], writes=[r_dst])

    def phase_merge(l, jb, tbs):
        P.push_scope()
        stg = [P.sb(f"mg_stg{i}", [128, 1024], F32) for i in range(2)]
        r_stg = [Res(), Res()]
        ctr = [0]
        wpa = P.sb("mg_wpa", [128, 4, 1024], BF16)
        wpb = P.sb("mg_wpb", [128, 4, 1024], BF16)
        wo = P.sb("mg_wo", [128, 8, 1024], BF16)
        r_w = Res()
        for kc in range(4):
            load_cast(wpa[:, kc, :], w_pa_d[l, kc * 128:(kc + 1) * 128, :], stg, r_stg, ctr, r_w, lambda t: t[:])
            load_cast(wpb[:, kc, :], w_pb_d[l, kc * 128:(kc + 1) * 128, :], stg, r_stg, ctr, r_w, lambda t: t[:])
        for kc in range(8):
            load_cast(wo[:, kc, :], w_o_d[l, kc * 128:(kc + 1) * 128, :], stg, r_stg, ctr, r_w, lambda t: t[:])
        yr = P.sb("mg_yr", [128, 4, 512], BF16)
        ya = P.sb("mg_ya", [128, 4, 512], BF16)
        ga = P.sb("mg_ga", [128, 8, 512], BF16)
        gb = P.sb("mg_gb", [128, 8, 512], BF16)
        r_in = Res()
        z = P.sb("mg_z", [128, 8, 512], BF16)
        r_z = Res()
        ta = [P.sb(f"mg_ta{i}", [128, 512], F32) for i in range(2)]
        r_ta = [Res(), Res()]
        tb_ = [P.sb(f"mg_tb{i}", [128, 512], F32) for i in range(2)]
        r_tb = [Res(), Res()]
        for tb in tbs:
            s, n = TBS[tb]
            j = 4 if tb == 4 else jb
            P.dma("sp", yr[:, :, 0:n], YR_d.rearrange("(c p) t -> p c t", p=128)[:, :, s:s + n], reads=rYR, writes=[r_in])
            P.dma("sp", ya[:, :, 0:n], YA_d.rearrange("(c p) t -> p c t", p=128)[:, :, s:s + n], reads=rYA, writes=[r_in])
            P.dma("sp", ga[:, :, 0:n], PG_d[0:1024, :].rearrange("(c p) t -> p c t", p=128)[:, :, s:s + n], reads=rPG, writes=[r_in])
            P.dma("sp", gb[:, :, 0:n], PG_d[1024:2048, :].rearrange("(c p) t -> p c t", p=128)[:, :, s:s + n], reads=rPG, writes=[r_in])
            for dc in range(8):
                i = dc % 2
                bkA, rbA = nbank()
                for kc in range(4):
                    P.op("pe", lambda e, kc=kc: e.matmul(bkA[:, 0:n], wpa[:, kc, dc * 128:(dc + 1) * 128], yr[:, kc, 0:n],
                                                        start=(kc == 0), stop=(kc == 3)), reads=[r_w, r_in], writes=[rbA])
                bkB, rbB = nbank()
                for kc in range(4):
                    P.op("pe", lambda e, kc=kc: e.matmul(bkB[:, 0:n], wpb[:, kc, dc * 128:(dc + 1) * 128], ya[:, kc, 0:n],
                                                        start=(kc == 0), stop=(kc == 3)), reads=[r_w, r_in], writes=[rbB])
                P.op("dve", lambda e: e.tensor_tensor(out=ta[i][:, 0:n], in0=bkA[:, 0:n], in1=ga[:, dc, 0:n], op=ALU.mult),
                     reads=[rbA, r_in], writes=[r_ta[i]])
                P.op("dve", lambda e: e.tensor_tensor(out=tb_[i][:, 0:n], in0=bkB[:, 0:n], in1=gb[:, dc, 0:n], op=ALU.mult),
                     reads=[rbB, r_in], writes=[r_tb[i]])
                P.op("pool", lambda e: e.tensor_tensor(out=z[:, dc, 0:n], in0=ta[i][:, 0:n], in1=tb_[i][:, 0:n], op=ALU.add),
                     reads=[r_ta[i], r_tb[i]], writes=[r_z])
            for dc in range(8):
                bkO, rbO = nbank()
                for kc in range(8):
                    P.op("pe", lambda e, kc=kc: e.matmul(bkO[:, 0:n], wo[:, kc, dc * 128:(dc + 1) * 128], z[:, kc, 0:n],
                                                        start=(kc == 0), stop=(kc == 7)), reads=[r_w, r_z], writes=[rbO])
                gt = mdv[:, l, 2, dc, j:j + 1]
                P.op("dve", lambda e, gt=gt: e.scalar_tensor_tensor(out=xT[:, dc, s:s + n], in0=bkO[:, 0:n], scalar=gt, in1=xT[:, dc, s:s + n],
                                                                   op0=ALU.mult, op1=ALU.add), reads=[rbO, r_mdv, rx[tb]], writes=[rx[tb]])
        P.pop_scope()

    def phase_ffn(l, jb, tbs, moe):
        P.push_scope()
        hT = P.sb("hT2", [128, 8, NT], BF16)
        rh = [Res() for _ in TBS]
        if moe:
            wgT = P.sb("f1_wgT", [8, LAT], F32)
            r_wgT = Res()
            sel_all = P.sb("f1_sel", [8, NE, 128], F32)
            r_sel = Res()
        P.push_scope()
        sqb = [P.sb(f"f1_sq{i}", [128, 512], F32) for i in range(2)]
        r_sqb = [Res(), Res()]
        tmpb = [P.sb(f"f1_tmp{i}", [128, 512], F32) for i in range(2)]
        r_tmpb = [Res(), Res()]
        rstd = P.sb("f1_rstd", [128, 512], F32)
        r_rstd = Res()
        if moe:
            h32 = P.sb("f1_h32", [128, 8, 512], F32)
            r_h32 = Res()
            rt_f = P.sb("f1_rt", [128, 8, NE], F32)
            r_rt = Res()
            P.dma("sp", rt_f[:], router_d[0].rearrange("(kc p) e -> p kc e", p=128), writes=[r_rt])
            lgT = P.sb("f1_lgT", [8, 512], F32)
            r_lgT = Res()
            sm = P.sb("f1_sm", [128, 64], F32)
            r_sm = Res()
            for e_ in range(NE):
                P.op("dve", lambda e, e_=e_: e.tensor_copy(out=sel_all[:, e_, :], in_=bass.AP(ident_f, e_, [[128, 8], [0, 128]])),
                     reads=[r_const], writes=[r_sel])
        for tb in tbs:
            s, n = TBS[tb]
            if moe:
                norm_block(l, 1, jb, tb, hT, rh, sqb, r_sqb, tmpb, r_tmpb, rstd, r_rstd, hT32=h32, rh32=r_h32)
                bk, rb = nbank()
                for kc in range(8):
                    P.op("pe", lambda e, kc=kc: e.matmul(bk[0:8, 0:n], rt_f[:, kc, :], h32[:, kc, 0:n], start=(kc == 0), stop=(kc == 7)),
                         reads=[r_rt, r_h32], writes=[rb])
                P.op("act", lambda e: e.copy(out=lgT[:, 0:n], in_=bk[0:8, 0:n]), reads=[rb], writes=[r_lgT])
                bk2, rb2 = nbank()
                for sb_ in range(n // 128):
                    bk1, rb1 = nbank()
                    P.op("pe", lambda e: e.transpose(bk1[:, 0:8], lgT[:, sb_ * 128:(sb_ + 1) * 128], ident_f[0:8, 0:8]),
                         reads=[r_lgT, r_const], writes=[rb1])
                    lg, m1, eq, lg2, m2, sel, ex, den = (sm[:, 0:8], sm[:, 8:9], sm[:, 16:24], sm[:, 24:32], sm[:, 9:10], sm[:, 32:40],
                                                         sm[:, 40:48], sm[:, 10:11])
                    nm1 = sm[:, 11:12]
                    wg_ = sm[:, 48:56]
                    P.op("dve", lambda e: e.tensor_copy(out=lg, in_=bk1[:, 0:8]), reads=[rb1], writes=[r_sm])
                    P.op("dve", lambda e: e.tensor_reduce(out=m1, in_=lg, axis=AX.X, op=ALU.max), reads=[r_sm], writes=[r_sm])
                    P.op("dve", lambda e: e.tensor_scalar(out=eq, in0=lg, scalar1=m1, scalar2=-1e30, op0=ALU.is_equal, op1=ALU.mult),
                         reads=[r_sm], writes=[r_sm])
                    P.op("dve", lambda e: e.tensor_tensor(out=lg2, in0=lg, in1=eq, op=ALU.add), reads=[r_sm], writes=[r_sm])
                    P.op("dve", lambda e: e.tensor_reduce(out=m2, in_=lg2, axis=AX.X, op=ALU.max), reads=[r_sm], writes=[r_sm])
                    P.op("dve", lambda e: e.tensor_scalar(out=sel, in0=lg, scalar1=m2, scalar2=None, op0=ALU.is_ge), reads=[r_sm], writes=[r_sm])
                    P.op("dve", lambda e: e.tensor_scalar(out=nm1, in0=m1, scalar1=-1.0, scalar2=None, op0=ALU.mult), reads=[r_sm], writes=[r_sm])
                    P.op("act", lambda e: e.activation(out=ex, in_=lg, func=AF.Exp, scale=1.0, bias=nm1), reads=[r_sm], writes=[r_sm])
                    P.op("dve", lambda e: e.tensor_tensor(out=ex, in0=ex, in1=sel, op=ALU.mult), reads=[r_sm], writes=[r_sm])
                    P.op("dve", lambda e: e.tensor_reduce(out=den, in_=ex, axis=AX.X, op=ALU.add), reads=[r_sm], writes=[r_sm])
                    P.op("dve", lambda e: e.reciprocal(out=den, in_=den), reads=[r_sm], writes=[r_sm])
                    P.op("dve", lambda e: e.tensor_scalar(out=wg_, in0=ex, scalar1=den, scalar2=None, op0=ALU.mult), reads=[r_sm], writes=[r_sm])
                    P.op("pe", lambda e: e.transpose(bk2[0:8, sb_ * 128:(sb_ + 1) * 128], wg_, ident_f[:]), reads=[r_sm, r_const], writes=[rb2])
                P.op("act", lambda e: e.copy(out=wgT[:, s:s + n], in_=bk2[0:8, 0:n]), reads=[rb2], writes=[r_wgT])
            else:
                norm_block(l, 1, jb, tb, hT, rh, sqb, r_sqb, tmpb, r_tmpb, rstd, r_rstd)
        if "moe" in dbg and moe:
            dw = dbg_tensor("dbg_wgT", [8, LAT], F32)
            P.dma("sp", dw, wgT[:], reads=[r_wgT])
        P.pop_scope()
        NFI = 2
        stg = [P.sb(f"f2_stg{i}", [128, 1024], F32) for i in range(4)]
        r_stg = [Res() for _ in range(4)]
        ctr = [0]
        wgb = [P.sb(f"f2_wgb{i}", [128, 8, 128], BF16) for i in range(3)]
        wub = [P.sb(f"f2_wub{i}", [128, 8, 128], BF16) for i in range(3)]
        r_wgu = [Res(), Res(), Res()]
        wdb = [P.sb(f"f2_wdb{i}", [128, NFI, 1024], BF16) for i in range(2)]
        r_wdb = [Res(), Res()]
        actT = P.sb("f2_act", [128, NFI, NT], BF16)
        r_act = [Res() for _ in TBS]
        sl = [P.sb(f"f2_sl{i}", [128, 512], F32) for i in range(2)]
        r_sl = [Res(), Res()]
        sl2 = [P.sb(f"f2_sl2{i}", [128, 512], F32) for i in range(2)]
        r_sl2 = [Res(), Res()]
        if moe:
            wb = P.sb("f2_wb", [128, LAT], BF16)
            r_wb = Res()
        gi = 0
        ii = 0
        for ex_ in range(NE if moe else 1):
            if moe:
                wgs, wus, wds = moe_wg_d[0, ex_], moe_wu_d[0, ex_], moe_wd_d[0, ex_]
                for tb in tbs:
                    s, n = TBS[tb]
                    bk, rb = nbank()
                    P.op("pe", lambda e: e.matmul(bk[:, 0:n], sel_all[:, ex_, :], wgT[:, s:s + n], start=True, stop=True),
                         reads=[r_sel, r_wgT], writes=[rb])
                    P.op("act", lambda e: e.copy(out=wb[:, s:s + n], in_=bk[:, 0:n]), reads=[rb], writes=[r_wb])
            else:
                wgs, wus, wds = ffn_wg_d[0], ffn_wu_d[0], ffn_wd_d[0]
            wgs_r = wgs.rearrange("(kc p) f -> p kc f", p=128)
            wus_r = wus.rearrange("(kc p) f -> p kc f", p=128)
            for fg in range(FF // 128 // NFI):
                g2 = gi % 2
                gi += 1
                for fi in range(NFI):
                    fc = fg * NFI + fi
                    w2i = ii % 3
                    ii += 1
                    load_cast(wgb[w2i][:], wgs_r[:, :, fc * 128:(fc + 1) * 128], stg, r_stg, ctr, r_wgu[w2i],
                              lambda t: t[:].rearrange("p (a b) -> p a b", a=8))
                    load_cast(wub[w2i][:], wus_r[:, :, fc * 128:(fc + 1) * 128], stg, r_stg, ctr, r_wgu[w2i],
                              lambda t: t[:].rearrange("p (a b) -> p a b", a=8))
                    load_cast(wdb[g2][:, fi, :], wds[fc * 128:(fc + 1) * 128, :], stg, r_stg, ctr, r_wdb[g2], lambda t: t[:])
                    for tb in tbs:
                        s, n = TBS[tb]
                        si = (fi + tb) % 2
                        bkG, rbG = nbank()
                        for kc in range(8):
                            P.op("pe", lambda e, kc=kc: e.matmul(bkG[:, 0:n], wgb[w2i][:, kc, :], hT[:, kc, s:s + n], start=(kc == 0), stop=(kc == 7)),
                                 reads=[r_wgu[w2i], rh[tb]], writes=[rbG])
                        bkU, rbU = nbank()
                        for kc in range(8):
                            P.op("pe", lambda e, kc=kc: e.matmul(bkU[:, 0:n], wub[w2i][:, kc, :], hT[:, kc, s:s + n], start=(kc == 0), stop=(kc == 7)),
                                 reads=[r_wgu[w2i], rh[tb]], writes=[rbU])
                        P.op("act", lambda e: e.activation(out=sl[si][:, 0:n], in_=bkG[:, 0:n], func=AF.Silu), reads=[rbG], writes=[r_sl[si]])
                        if moe:
                            P.op("dve", lambda e: e.tensor_tensor(out=sl2[si][:, 0:n], in0=bkU[:, 0:n], in1=sl[si][:, 0:n], op=ALU.mult),
                                 reads=[rbU, r_sl[si]], writes=[r_sl2[si]])
                            P.op("pool", lambda e: e.tensor_tensor(out=actT[:, fi, s:s + n], in0=sl2[si][:, 0:n], in1=wb[:, s:s + n], op=ALU.mult),
                                 reads=[r_sl2[si], r_wb], writes=[r_act[tb]])
                        else:
                            P.op("dve", lambda e: e.tensor_tensor(out=actT[:, fi, s:s + n], in0=bkU[:, 0:n], in1=sl[si][:, 0:n], op=ALU.mult),
                                 reads=[rbU, r_sl[si]], writes=[r_act[tb]])
                for tb in tbs:
                    s, n = TBS[tb]
                    j = 4 if tb == 4 else jb
                    for dc in range(8):
                        bkO, rbO = nbank()
                        for fi in range(NFI):
                            P.op("pe", lambda e, fi=fi: e.matmul(bkO[:, 0:n], wdb[g2][:, fi, dc * 128:(dc + 1) * 128], actT[:, fi, s:s + n],
                                                                start=(fi == 0), stop=(fi == NFI - 1)), reads=[r_wdb[g2], r_act[tb]], writes=[rbO])
                        gt = mdv[:, l, 5, dc, j:j + 1]
                        P.op("dve", lambda e, gt=gt: e.scalar_tensor_tensor(out=xT[:, dc, s:s + n], in0=bkO[:, 0:n], scalar=gt, in1=xT[:, dc, s:s + n],
                                                                           op0=ALU.mult, op1=ALU.add), reads=[rbO, r_mdv, rx[tb]], writes=[rx[tb]])
        P.pop_scope()

    def phase_final(jb):
        P.push_scope()
        sqb = [P.sb(f"fn_sq{i}", [128, 512], F32) for i in range(2)]
        r_sqb = [Res(), Res()]
        ob = [P.sb(f"fn_o{i}", [128, 512], F32) for i in range(2)]
        r_ob = [Res(), Res()]
        rstd = P.sb("fn_rstd", [128, 512], F32)
        r_rstd = Res()
        for tb in range(4):
            s, n = TBS[tb]
            bk, rb = nbank()
            for c in range(8):
                i = c % 2
                P.op("act", lambda e: e.activation(out=sqb[i][:, 0:n], in_=xT[:, c, s:s + n], func=AF.Square), reads=[rx[tb]], writes=[r_sqb[i]])
                P.op("pe", lambda e: e.matmul(bk[:, 0:n], ones_f[:], sqb[i][:, 0:n], start=(c == 0), stop=(c == 7)),
                     reads=[r_sqb[i], r_const], writes=[rb])
            P.op("act", lambda e: e.activation(out=rstd[:, 0:n], in_=bk[:, 0:n], func=AF.Sqrt, scale=1.0 / D, bias=cst[:, 0:1]),
                 reads=[rb, r_cst], writes=[r_rstd])
            P.op("dve", lambda e: e.reciprocal(out=rstd[:, 0:n], in_=rstd[:, 0:n]), reads=[r_rstd], writes=[r_rstd])
            for c in range(8):
                i = c % 2
                fnc = par[:, 2 * NPL + c:2 * NPL + c + 1]
                P.op("dve", lambda e: e.scalar_tensor_tensor(out=ob[i][:, 0:n], in0=xT[:, c, s:s + n], scalar=fnc, in1=rstd[:, 0:n],
                                                            op0=ALU.mult, op1=ALU.mult), reads=[rx[tb], r_par, r_rstd], writes=[r_ob[i]])
                P.dma("sp", outT_d[jb, c * 128:(c + 1) * 128, s:s + n], ob[i][:, 0:n], reads=[r_ob[i]])
        P.pop_scope()

    RB = 256

    def phase_rwkv(l):
        P.push_scope()
        twT = P.sb("rw_tw", [64, NT], BF16)
        adT = P.sb("rw_ad", [64, NT], BF16)
        gsT = P.sb("rw_gs", [128, NT], BF16)
        r_lora = Res()
        w2b = [P.sb(f"rw_w2b{d}", [64, 512], BF16) for d in range(2)]
        a2b = [P.sb(f"rw_a2b{d}", [64, 512], BF16) for d in range(2)]
        g2b = P.sb("rw_g2b", [128, 512], BF16)
        mk = P.sb("rw_mk", [128, 2560], BF16)
        rmask = P.sb("rw_rmask", [128, 512], F32)
        r_w = Res()
        P.push_scope()
        stg = P.sb("rw_stg", [128, NT], F32)
        r_stg = Res()
        P.dma("sp", stg[0:64, :], PR_d[1536:1600, :], reads=[rPR[12]], writes=[r_stg])
        P.op("act", lambda e: e.activation(out=twT[:], in_=stg[0:64, :], func=AF.Tanh), reads=[r_stg], writes=[r_lora])
        P.dma("sp", stg[0:64, :], PR_d[1600:1664, :], reads=[rPR[12]], writes=[r_stg])
        P.op("act", lambda e: e.copy(out=adT[:], in_=stg[0:64, :]), reads=[r_stg], writes=[r_lora])
        P.dma("sp", stg[:], PR_d[1664:1792, :], reads=[rPR[13]], writes=[r_stg])
        P.op("act", lambda e: e.activation(out=gsT[:], in_=stg[:], func=AF.Sigmoid), reads=[r_stg], writes=[r_lora])
        for d in range(2):
            P.dma("sp", stg[0:64, 0:512], w2_d[l, d], writes=[r_stg])
            P.op("dve", lambda e: e.tensor_copy(out=w2b[d][:], in_=stg[0:64, 0:512]), reads=[r_stg], writes=[r_w])
            P.dma("sp", stg[0:64, 0:512], a2_d[l, d], writes=[r_stg])
            P.op("dve", lambda e: e.tensor_copy(out=a2b[d][:], in_=stg[0:64, 0:512]), reads=[r_stg], writes=[r_w])
        P.dma("sp", stg[:, 0:512], g2_d[l], writes=[r_stg])
        P.op("dve", lambda e: e.tensor_copy(out=g2b[:], in_=stg[:, 0:512]), reads=[r_stg], writes=[r_w])
        for i in range(5):
            P.dma("sp", stg[:, 0:512], mask_d[:, i * 512:(i + 1) * 512], writes=[r_stg])
            P.op("dve", lambda e: e.tensor_copy(out=mk[:, i * 512:(i + 1) * 512], in_=stg[:, 0:512]), reads=[r_stg], writes=[r_w])
        P.dma("sp", rmask[:], rmask_d, writes=[r_w])
        P.pop_scope()

        rT = P.sb("rw_r", [128, NT], F32)
        kT = P.sb("rw_k", [128, NT], F32)
        vT = P.sb("rw_v", [128, NT], F32)
        kkT = P.sb("rw_kk", [128, NT], F32)
        r_rkv = Res()
        r_kk = Res()
        Yacc = P.sb("rw_yacc", [128, NT], F32)
        r_Y = Res()
        yob = [(P.sb(f"rw_yob{i}", [128, RB], BF16), Res()) for i in range(2)]
        Vp = P.sb("rw_vp", [128, 18, 256], BF16)
        r_Vp = Res()
        P.op("pool", lambda e: e.memset(Vp[:], 0.0), writes=[r_Vp])

        def f32t(nm, w=RB):
            return P.sb(nm, [128, w], F32), Res()

        def b16t(nm, w=RB):
            return P.sb(nm, [128, w], BF16), Res()
        sg, r_sg = f32t("rw_sg")
        a_, r_a = f32t("rw_a")
        cs, r_cs_ = f32t("rw_cs")
        s1, r_s1 = f32t("rw_s1")
        s0, r_s0 = f32t("rw_s0")
        e0, r_e0 = f32t("rw_e0")
        e1, r_e1 = f32t("rw_e1")
        e2, r_e2 = f32t("rw_e2")
        e3, r_e3 = f32t("rw_e3")
        tt, r_tt = f32t("rw_tt")
        keys, r_keys = f32t("rw_keys")
        bb, r_bb = f32t("rw_bb")
        BhT, r_BhT = f32t("rw_BhT")
        KhT, r_KhT = f32t("rw_KhT")
        At, r_At = b16t("rw_At")
        Rt, r_Rt = b16t("rw_Rt")
        Bm1, r_Bm1 = b16t("rw_Bm1")
        Bm2, r_Bm2 = b16t("rw_Bm2")
        Km1, r_Km1 = b16t("rw_Km1")
        Km2, r_Km2 = b16t("rw_Km2")
        for t_, r_ in ((Bm1, r_Bm1), (Bm2, r_Bm2), (Km1, r_Km1), (Km2, r_Km2)):
            P.op("pool", lambda e, t_=t_: e.memset(t_[:], 0.0), writes=[r_])
        bsm = P.sb("rw_bsm", [128, 8], F32)
        r_bsm = Res()
        pnd = P.sb("rw_pnd", [128, 2], F32)
        pnm = P.sb("rw_pnm", [128, 2], F32)
        Hbm = [b16t(f"rw_Hbm{i}", 128) for i in range(2)]
        r_pnd = Res()
        NCH = RB // 128
        SC1a = [f32t(f"rw_SC1a_{i}", 256) for i in range(NCH)]
        SC1b = [b16t(f"rw_SC1b_{i}", 256) for i in range(NCH)]
        SC2 = [b16t(f"rw_SC2_{i}", 512) for i in range(NCH)]
        SA = [f32t(f"rw_SA_{i}", 256) for i in range(NCH)]
        TT = [f32t(f"rw_TT_{i}", 256) for i in range(NCH)]
        XX = [[f32t(f"rw_XX_{i}_{j}", 512) for j in range(2)] for i in range(NCH)]
        Bhp = [b16t(f"rw_Bhp_{i}", 256) for i in range(NCH)]
        Khp = [b16t(f"rw_Khp_{i}", 256) for i in range(NCH)]
        RHp = [f32t(f"rw_RHp_{i}", 256) for i in range(2)]
        Up = [b16t(f"rw_Up_{i}", 256) for i in range(2)]
        for lst in (Bhp, Khp, RHp, Up):
            for t_, r_ in lst:
                P.op("pool", lambda e, t_=t_: e.memset(t_[:], 0.0), writes=[r_])
        Hf = P.sb("rw_Hf", [128, 128], F32)
        Hbf = P.sb("rw_Hbf", [128, 128], BF16)
        r_Hf = Res()
        r_Hbf = Res()
        yc, r_yc = s0, r_s0
        sq, r_sq = e0, r_e0
        rsd, r_rsd = e1, r_e1
        aa0, r_aa0 = e2, r_e2
        aa1, r_aa1 = e3, r_e3

        def padcopy(dst_t, dst_off, pstride, src_ap, r_dst, r_src):
            out_ap = bass.AP(dst_t, dst_off, [[pstride, 128], [192, 2], [1, 64]])
            P.op("dve", lambda e: e.tensor_copy(out=out_ap, in_=src_ap.rearrange("p (h j) -> p h j", h=2)), reads=[r_src], writes=[r_dst])

        kkc = lambda j: pcol(l, "kk", j)
        for hp in range(4):
            P.dma("sp", rT[:], PR_d[hp * 128:(hp + 1) * 128, :], reads=[rPR[hp]], writes=[r_rkv])
            P.dma("sp", kT[:], PR_d[512 + hp * 128:512 + (hp + 1) * 128, :], reads=[rPR[4 + hp]], writes=[r_rkv])
            P.dma("sp", vT[:], PR_d[1024 + hp * 128:1024 + (hp + 1) * 128, :], reads=[rPR[8 + hp]], writes=[r_rkv])
            ka = pcol(l, "ka", hp)
            omka = dpar[:, l * 32 + 14 + hp:l * 32 + 15 + hp]
            for bs in range(0, NT, RB):
                n = RB
                P.op("dve", lambda e: e.tensor_scalar(out=tt[:], in0=kT[:, bs:bs + n], scalar1=kkc(hp), scalar2=None, op0=ALU.mult),
                     reads=[r_rkv, r_par], writes=[r_tt])
                P.op("act", lambda e: e.activation(out=sq[:], in_=tt[:], func=AF.Square), reads=[r_tt], writes=[r_sq])
                bk, rb = nbank()
                P.op("pe", lambda e: e.matmul(bk[:, 0:n], onesbd_f[:], sq[:], start=True, stop=True), reads=[r_sq, r_const], writes=[rb])
                P.op("act", lambda e: e.activation(out=rsd[:], in_=bk[:, 0:n], func=AF.Sqrt, scale=1.0, bias=cst[:, 4:5]),
                     reads=[rb, r_cst], writes=[r_rsd])
                P.op("dve", lambda e: e.reciprocal(out=rsd[:], in_=rsd[:]), reads=[r_rsd], writes=[r_rsd])
                P.op("dve", lambda e: e.tensor_tensor(out=kkT[:, bs:bs + n], in0=tt[:], in1=rsd[:], op=ALU.mult),
                     reads=[r_tt, r_rsd], writes=[r_kk])
            for d in range(2):
                P.op("pool", lambda e: e.memset(Hf[:], 0.0), writes=[r_Hf])
                P.op("pool", lambda e: e.memset(Hbf[:], 0.0), writes=[r_Hbf])
                lat = list(range(0, LAT, RB))
                order = [LAT] + (lat if d == 0 else lat[::-1])
                mo = d * 1280
                w0c = pcol(l, f"w0_{d}", hp)
                a0c = pcol(l, f"a0_{d}", hp)
                seqi = 0
                for bs in order:
                    n = RB
                    bk, rb = nbank()
                    P.op("pe", lambda e: e.matmul(bk[:, 0:n], w2b[d][:, hp * 128:(hp + 1) * 128], twT[:, bs:bs + n], start=True, stop=True),
                         reads=[r_w, r_lora], writes=[rb])
                    P.op("act", lambda e: e.activation(out=sg[:], in_=bk[:, 0:n], func=AF.Sigmoid, scale=1.0, bias=w0c),
                         reads=[rb, r_par], writes=[r_sg])
                    bk, rb = nbank()
                    P.op("pe", lambda e: e.matmul(bk[:, 0:n], a2b[d][:, hp * 128:(hp + 1) * 128], adT[:, bs:bs + n], start=True, stop=True),
                         reads=[r_w, r_lora], writes=[rb])
                    P.op("act", lambda e: e.activation(out=a_[:], in_=bk[:, 0:n], func=AF.Sigmoid, scale=1.0, bias=a0c),
                         reads=[rb, r_par], writes=[r_a])
                    P.op("dve", lambda e: e.tensor_tensor_scan(out=cs[:], data0=rmask[:, 0:n], data1=sg[:], initial=0.0, op0=ALU.mult, op1=ALU.add),
                         reads=[r_w, r_sg], writes=[r_cs_])
                    if d == 0:
                        sS, r_sS = cs, r_cs_
                        P.op("dve", lambda e: e.tensor_tensor(out=s0[:], in0=cs[:], in1=sg[:], op=ALU.subtract), reads=[r_cs_, r_sg], writes=[r_s0])
                    else:
                        for ci in range(NCH):
                            tot = cs[:, ci * 128 + 127:ci * 128 + 128]
                            P.op("dve", lambda e: e.tensor_scalar(out=s0[:, ci * 128:(ci + 1) * 128], in0=cs[:, ci * 128:(ci + 1) * 128],
                                                                 scalar1=-1.0, scalar2=tot, op0=ALU.mult, op1=ALU.add),
                                 reads=[r_cs_], writes=[r_s0])
                        P.op("dve", lambda e: e.tensor_tensor(out=s1[:], in0=s0[:], in1=sg[:], op=ALU.add), reads=[r_s0, r_sg], writes=[r_s1])
                        sS, r_sS = s1, r_s1
                    for ci in range(NCH):
                        tot = cs[:, ci * 128 + 127:ci * 128 + 128]
                        for q_, fac in enumerate((LW / 2, -LW / 2, LW)):
                            P.op("dve", lambda e: e.tensor_scalar(out=bsm[:, ci * 4 + q_:ci * 4 + q_ + 1], in0=tot, scalar1=fac, scalar2=None, op0=ALU.mult),
                                 reads=[r_cs_], writes=[r_bsm])
                        P.op("act", lambda e: e.activation(out=pnd[:, ci:ci + 1], in_=tot, func=AF.Exp, scale=LW), reads=[r_cs_], writes=[r_pnd])
                        P.op("act", lambda e: e.activation(out=pnm[:, ci:ci + 1], in_=tot, func=AF.Exp, scale=LW / 2), reads=[r_cs_], writes=[r_pnd])
                        cl = slice(ci * 128, (ci + 1) * 128)
                        mpos, mneg, cb_ = (bsm[:, ci * 4 + q_:ci * 4 + q_ + 1] for q_ in range(3))
                        P.op("act", lambda e: e.activation(out=e1[:, cl], in_=sS[:, cl], func=AF.Exp, scale=LW, bias=mneg), reads=[r_sS, r_bsm], writes=[r_e1])
                        P.op("act", lambda e: e.activation(out=e0[:, cl], in_=s0[:, cl], func=AF.Exp, scale=LW, bias=mneg), reads=[r_s0, r_bsm], writes=[r_e0])
                        P.op("act", lambda e: e.activation(out=e2[:, cl], in_=sS[:, cl], func=AF.Exp, scale=-LW, bias=mpos), reads=[r_sS, r_bsm], writes=[r_e2])
                        P.op("act", lambda e: e.activation(out=e3[:, cl], in_=sS[:, cl], func=AF.Exp, scale=-LW, bias=cb_), reads=[r_sS, r_bsm], writes=[r_e3])
                    P.op("dve", lambda e: e.tensor_scalar(out=tt[:], in0=a_[:], scalar1=ka, scalar2=omka, op0=ALU.mult, op1=ALU.add),
                         reads=[r_a, r_par, r_dpar], writes=[r_tt])
                    P.op("pool", lambda e: e.tensor_tensor(out=keys[:], in0=tt[:], in1=kT[:, bs:bs + n], op=ALU.mult), reads=[r_tt, r_rkv], writes=[r_keys])
                    P.op("pool", lambda e: e.tensor_tensor(out=bb[:], in0=kkT[:, bs:bs + n], in1=a_[:], op=ALU.mult), reads=[r_kk, r_a], writes=[r_bb])
                    P.op("dve", lambda e: e.scalar_tensor_tensor(out=At[:], in0=kkT[:, bs:bs + n], scalar=-1.0, in1=e0[:], op0=ALU.mult, op1=ALU.mult),
                         reads=[r_kk, r_e0], writes=[r_At])
                    P.op("pool", lambda e: e.tensor_tensor(out=Rt[:], in0=rT[:, bs:bs + n], in1=e1[:], op=ALU.mult), reads=[r_rkv, r_e1], writes=[r_Rt])
                    P.op("dve", lambda e: e.tensor_tensor(out=Bm1[0:64, :], in0=bb[0:64, :], in1=e2[0:64, :], op=ALU.mult), reads=[r_bb, r_e2], writes=[r_Bm1])
                    P.op("dve", lambda e: e.tensor_tensor(out=Bm2[64:128, :], in0=bb[64:128, :], in1=e2[64:128, :], op=ALU.mult), reads=[r_bb, r_e2], writes=[r_Bm2])
                    P.op("pool", lambda e: e.tensor_tensor(out=Km1[0:64, :], in0=keys[0:64, :], in1=e2[0:64, :], op=ALU.mult), reads=[r_keys, r_e2], writes=[r_Km1])
                    P.op("pool", lambda e: e.tensor_tensor(out=Km2[64:128, :], in0=keys[64:128, :], in1=e2[64:128, :], op=ALU.mult), reads=[r_keys, r_e2], writes=[r_Km2])
                    P.op("dve", lambda e: e.tensor_tensor(out=BhT[:], in0=bb[:], in1=e3[:], op=ALU.mult), reads=[r_bb, r_e3], writes=[r_BhT])
                    P.op("pool", lambda e: e.tensor_tensor(out=KhT[:], in0=keys[:], in1=e3[:], op=ALU.mult), reads=[r_keys, r_e3], writes=[r_KhT])
                    for ci in range(NCH):
                        cl = slice(ci * 128, (ci + 1) * 128)
                        kc = (bs + ci * 128) // 128
                        bk, rb = nbank()
                        P.op("pe", lambda e: e.transpose(bk[:, 0:128], BhT[:, cl], ident_f[:]), reads=[r_BhT, r_const], writes=[rb])
                        P.op("pe", lambda e: e.transpose(bk[:, 128:256], KhT[:, cl], ident_f[:]), reads=[r_KhT, r_const], writes=[rb])
                        if d == 0:
                            P.op("pe", lambda e: e.transpose(bk[:, 256:384], vT[:, bs + ci * 128:bs + (ci + 1) * 128], ident_f[:]),
                                 reads=[r_rkv, r_const], writes=[rb])
                            padcopy(Vp, kc * 256, 18 * 256, bk[:, 256:384], r_Vp, rb)
                        padcopy(Bhp[ci][0], 0, 256, bk[:, 0:128], Bhp[ci][1], rb)
                        padcopy(Khp[ci][0], 0, 256, bk[:, 128:256], Khp[ci][1], rb)
                        b1, rb1 = nbank()
                        b2, rb2 = nbank()
                        b3, rb3 = nbank()
                        for q_, (lt_, rl_) in enumerate(((Bm1, r_Bm1), (Bm2, r_Bm2), (Km1, r_Km1), (Km2, r_Km2))):
                            P.op("pe", lambda e: e.matmul(b1[:, q_ * 128:(q_ + 1) * 128], lt_[:, cl], At[:, cl], start=True, stop=True),
                                 reads=[rl_, r_At], writes=[rb1])
                            P.op("pe", lambda e: e.matmul(b2[:, q_ * 128:(q_ + 1) * 128], lt_[:, cl], Rt[:, cl], start=True, stop=True),
                                 reads=[rl_, r_Rt], writes=[rb2])
                        P.op("pe", lambda e: e.matmul(b3[:, 0:128], At[:, cl], Bm1[:, cl], start=True, stop=True), reads=[r_At, r_Bm1], writes=[rb3])
                        P.op("pe", lambda e: e.matmul(b3[:, 128:256], At[:, cl], Bm2[:, cl], start=True, stop=True), reads=[r_At, r_Bm2], writes=[rb3])
                        P.op("dve", lambda e: e.tensor_tensor(out=SC1a[ci][0][:], in0=b1[:, 0:256], in1=mk[:, mo:mo + 256], op=ALU.mult),
                             reads=[rb1, r_w], writes=[SC1a[ci][1]])
                        P.op("dve", lambda e: e.tensor_tensor(out=SC1b[ci][0][:], in0=b1[:, 256:512], in1=mk[:, mo + 256:mo + 512], op=ALU.mult),
                             reads=[rb1, r_w], writes=[SC1b[ci][1]])
                        P.op("dve", lambda e: e.tensor_tensor(out=SC2[ci][0][:], in0=b2[:], in1=mk[:, mo + 512:mo + 1024], op=ALU.mult),
                             reads=[rb2, r_w], writes=[SC2[ci][1]])
                        P.op("dve", lambda e: e.tensor_tensor(out=SA[ci][0][:], in0=b3[:, 0:256], in1=mk[:, mo + 1024:mo + 1280], op=ALU.mult),
                             reads=[rb3, r_w], writes=[SA[ci][1]])
                        P.op("pool", lambda e: e.tensor_tensor(out=TT[ci][0][:], in0=SC1a[ci][0][:], in1=ident2_f[:], op=ALU.add),
                             reads=[SC1a[ci][1], r_const], writes=[TT[ci][1]])
                    for j in range(1, 7):
                        for ci in range(NCH):
                            if j == 1:
                                Xs, rXs, XTs, rXTs, xo, xto = SA[ci][0], SA[ci][1], SC1a[ci][0], SC1a[ci][1], 0, 0
                            else:
                                Xs, rXs = XX[ci][j % 2]
                                XTs, rXTs, xo, xto = Xs, rXs, 0, 256
                            Xn, rXn = XX[ci][(j + 1) % 2]
                            bn, rbn = nbank()
                            for h in range(2):
                                hs = slice(h * 128, (h + 1) * 128)
                                Xh = Xs[:, xo + h * 128:xo + (h + 1) * 128]
                                XTh = XTs[:, xto + h * 128:xto + (h + 1) * 128]
                                P.op("pe", lambda e: e.matmul(bn[:, hs], XTh, Xh, start=True, stop=True), reads=[rXs, rXTs], writes=[rbn])
                                if j < 6:
                                    P.op("pe", lambda e: e.matmul(bn[:, 256 + h * 128:256 + (h + 1) * 128], Xh, XTh, start=True, stop=True),
                                         reads=[rXs, rXTs], writes=[rbn])
                            w_ = 512 if j < 6 else 256
                            P.op("act", lambda e: e.copy(out=Xn[:, 0:w_], in_=bn[:, 0:w_]), reads=[rbn], writes=[rXn])
                        for jj in ([j - 1] if j >= 2 else []) + ([6] if j == 6 else []):
                            for ci in range(NCH):
                                Xp, rXp = XX[ci][(jj + 1) % 2]
                                bt, rbt = nbank()
                                for h in range(2):
                                    hs = slice(h * 128, (h + 1) * 128)
                                    P.op("pe", lambda e: e.matmul(bt[:, hs], Xp[:, hs], TT[ci][0][:, hs], start=True, stop=True),
                                         reads=[rXp, TT[ci][1]], writes=[rbt])
                                P.op("dve", lambda e: e.tensor_tensor(out=TT[ci][0][:], in0=bt[:, 0:256], in1=TT[ci][0][:], op=ALU.add),
                                     reads=[rbt, TT[ci][1]], writes=[TT[ci][1]])
                    cis = list(range(NCH)) if d == 0 else list(range(NCH))[::-1]
                    for ci in cis:
                        cl = slice(ci * 128, (ci + 1) * 128)
                        c0 = bs + ci * 128
                        kc = c0 // 128
                        RH, rRH = RHp[seqi % 2]
                        U_, rU = Up[seqi % 2]
                        seqi += 1
                        Hm, rHm = Hbm[seqi % 2]
                        P.op("dve", lambda e: e.tensor_scalar(out=Hm[:], in0=Hf[:], scalar1=pnm[:, ci:ci + 1], scalar2=None, op0=ALU.mult),
                             reads=[r_Hf, r_pnd], writes=[rHm])
                        q1, rq1 = nbank()
                        P.op("pe", lambda e: e.matmul(q1[:, 0:128], At[:, cl], Hm[:], start=True, stop=False), reads=[r_At, rHm], writes=[rq1])
                        P.op("pe", lambda e: e.matmul(q1[:, 0:128], SC1b[ci][0][:, 0:128], Vp[:, kc, 0:128], start=False, stop=False),
                             reads=[SC1b[ci][1], r_Vp], writes=[rq1])
                        P.op("pe", lambda e: e.matmul(q1[:, 0:128], SC1b[ci][0][:, 128:256], Vp[:, kc, 128:256], start=False, stop=True),
                             reads=[SC1b[ci][1], r_Vp], writes=[rq1])
                        padcopy(RH, 0, 256, q1[:, 0:128], rRH, rq1)
                        q2, rq2 = nbank()
                        P.op("pe", lambda e: e.matmul(q2[:, 0:128], TT[ci][0][:, 0:128], RH[:, 0:128], start=True, stop=False),
                             reads=[TT[ci][1], rRH], writes=[rq2])
                        P.op("pe", lambda e: e.matmul(q2[:, 0:128], TT[ci][0][:, 128:256], RH[:, 128:256], start=False, stop=True),
                             reads=[TT[ci][1], rRH], writes=[rq2])
                        padcopy(U_, 0, 256, q2[:, 0:128], rU, rq2)
                        q4, rq4 = nbank()
                        P.op("pe", lambda e: e.matmul(q4[:, 0:128], Hm[:], Rt[:, cl], start=True, stop=False), reads=[rHm, r_Rt], writes=[rq4])
                        P.op("pe", lambda e: e.matmul(q4[:, 0:128], U_[:, 0:128], SC2[ci][0][:, 0:128], start=False, stop=False),
                             reads=[rU, SC2[ci][1]], writes=[rq4])
                        P.op("pe", lambda e: e.matmul(q4[:, 0:128], U_[:, 128:256], SC2[ci][0][:, 128:256], start=False, stop=False),
                             reads=[rU, SC2[ci][1]], writes=[rq4])
                        P.op("pe", lambda e: e.matmul(q4[:, 0:128], Vp[:, kc, 0:128], SC2[ci][0][:, 256:384], start=False, stop=False),
                             reads=[r_Vp, SC2[ci][1]], writes=[rq4])
                        P.op("pe", lambda e: e.matmul(q4[:, 0:128], Vp[:, kc, 128:256], SC2[ci][0][:, 384:512], start=False, stop=True),
                             reads=[r_Vp, SC2[ci][1]], writes=[rq4])
                        if d == 0:
                            P.op("act", lambda e: e.copy(out=Yacc[:, c0:c0 + 128], in_=q4[:, 0:128]), reads=[rq4], writes=[r_Y])
                        else:
                            P.op("dve", lambda e: e.tensor_tensor(out=Yacc[:, c0:c0 + 128], in0=q4[:, 0:128], in1=Yacc[:, c0:c0 + 128], op=ALU.add),
                                 reads=[rq4, r_Y], writes=[r_Y])
                        q3, rq3 = nbank()
                        P.op("pe", lambda e: e.matmul(q3[:, 0:128], Bhp[ci][0][:, 0:128], U_[:, 0:128], start=True, stop=False),
                             reads=[Bhp[ci][1], rU], writes=[rq3])
                        P.op("pe", lambda e: e.matmul(q3[:, 0:128], Bhp[ci][0][:, 128:256], U_[:, 128:256], start=False, stop=False),
                             reads=[Bhp[ci][1], rU], writes=[rq3])
                        P.op("pe", lambda e: e.matmul(q3[:, 0:128], Khp[ci][0][:, 0:128], Vp[:, kc, 0:128], start=False, stop=False),
                             reads=[Khp[ci][1], r_Vp], writes=[rq3])
                        P.op("pe", lambda e: e.matmul(q3[:, 0:128], Khp[ci][0][:, 128:256], Vp[:, kc, 128:256], start=False, stop=True),
                             reads=[Khp[ci][1], r_Vp], writes=[rq3])
                        P.op("dve", lambda e: e.scalar_tensor_tensor(out=Hf[:], in0=Hf[:], scalar=pnd[:, ci:ci + 1], in1=q3[:, 0:128],
                                                                    op0=ALU.mult, op1=ALU.add), reads=[r_Hf, r_pnd, rq3], writes=[r_Hf])
            if "rwkv" in dbg and l == 0:
                dy = dbg_out["dbg_ysum"] if "dbg_ysum" in dbg_out else dbg_tensor("dbg_ysum", [512, NT], F32)
                P.dma("sp", dy[hp * 128:(hp + 1) * 128, :], Yacc[:], reads=[r_Y])
            lw_, lb_, rk_ = pcol(l, "lnxw", hp), pcol(l, "lnxb", hp), pcol(l, "rk", hp)
            for bs in range(0, NT, RB):
                n = RB
                bk, rb = nbank()
                P.op("pe", lambda e: e.matmul(bk[:, 0:n], onesbd_f[:], Yacc[:, bs:bs + n], start=True, stop=True), reads=[r_Y, r_const], writes=[rb])
                P.op("dve", lambda e: e.scalar_tensor_tensor(out=yc[:], in0=bk[:, 0:n], scalar=-1.0 / 64, in1=Yacc[:, bs:bs + n],
                                                            op0=ALU.mult, op1=ALU.add), reads=[rb, r_Y], writes=[r_yc])
                P.op("act", lambda e: e.activation(out=sq[:], in_=yc[:], func=AF.Square), reads=[r_yc], writes=[r_sq])
                bk, rb = nbank()
                P.op("pe", lambda e: e.matmul(bk[:, 0:n], onesbd_f[:], sq[:], start=True, stop=True), reads=[r_sq, r_const], writes=[rb])
                P.op("act", lambda e: e.activation(out=rsd[:], in_=bk[:, 0:n], func=AF.Sqrt, scale=1.0 / 64, bias=cst[:, 1:2]),
                     reads=[rb, r_cst], writes=[r_rsd])
                P.op("dve", lambda e: e.reciprocal(out=rsd[:], in_=rsd[:]), reads=[r_rsd], writes=[r_rsd])
                P.op("dve", lambda e: e.tensor_tensor(out=yc[:], in0=yc[:], in1=rsd[:], op=ALU.mult), reads=[r_yc, r_rsd], writes=[r_yc])
                P.op("act", lambda e: e.activation(out=yc[:], in_=yc[:], func=AF.Identity, scale=lw_, bias=lb_), reads=[r_yc, r_par], writes=[r_yc])
                for d, (aa, r_aa) in enumerate(((aa0, r_aa0), (aa1, r_aa1))):
                    bk, rb = nbank()
                    P.op("pe", lambda e: e.matmul(bk[:, 0:n], a2b[d][:, hp * 128:(hp + 1) * 128], adT[:, bs:bs + n], start=True, stop=True),
                         reads=[r_w, r_lora], writes=[rb])
                    P.op("act", lambda e: e.activation(out=aa[:], in_=bk[:, 0:n], func=AF.Sigmoid, scale=1.0, bias=pcol(l, f"a0_{d}", hp)),
                         reads=[rb, r_par], writes=[r_aa])
                P.op("pool", lambda e: e.tensor_tensor(out=aa0[:], in0=aa0[:], in1=aa1[:], op=ALU.add), reads=[r_aa0, r_aa1], writes=[r_aa0])
                P.op("dve", lambda e: e.tensor_scalar(out=tt[:], in0=aa0[:], scalar1=0.5, scalar2=ka, op0=ALU.mult, op1=ALU.mult),
                     reads=[r_aa0, r_par], writes=[r_tt])
                P.op("dve", lambda e: e.tensor_scalar(out=tt[:], in0=tt[:], scalar1=omka, scalar2=None, op0=ALU.add), reads=[r_tt, r_dpar], writes=[r_tt])
                P.op("pool", lambda e: e.tensor_tensor(out=keys[:], in0=tt[:], in1=kT[:, bs:bs + n], op=ALU.mult), reads=[r_tt, r_rkv], writes=[r_keys])
                P.op("dve", lambda e: e.scalar_tensor_tensor(out=bb[:], in0=rT[:, bs:bs + n], scalar=rk_, in1=keys[:], op0=ALU.mult, op1=ALU.mult),
                     reads=[r_rkv, r_par, r_keys], writes=[r_bb])
                bk, rb = nbank()
                P.op("pe", lambda e: e.matmul(bk[:, 0:n], onesbd_f[:], bb[:], start=True, stop=True), reads=[r_bb, r_const], writes=[rb])
                P.op("dve", lambda e: e.tensor_tensor(out=sq[:], in0=bk[:, 0:n], in1=vT[:, bs:bs + n], op=ALU.mult), reads=[rb, r_rkv], writes=[r_sq])
                P.op("pool", lambda e: e.tensor_tensor(out=sq[:], in0=sq[:], in1=yc[:], op=ALU.add), reads=[r_sq, r_yc], writes=[r_sq])
                bk, rb = nbank()
                P.op("pe", lambda e: e.matmul(bk[:, 0:n], g2b[:, hp * 128:(hp + 1) * 128], gsT[:, bs:bs + n], start=True, stop=True),
                     reads=[r_w, r_lora], writes=[rb])
                yo_, r_yo_ = yob[(bs // RB) % 2]
                P.op("dve", lambda e: e.tensor_tensor(out=yo_[:], in0=bk[:, 0:n], in1=sq[:], op=ALU.mult), reads=[rb, r_sq], writes=[r_yo_])
                P.dma("sp", YR_d[hp * 128:(hp + 1) * 128, bs:bs + n], yo_[:], reads=[r_yo_], writes=[rYR[hp]])
        P.pop_scope()

    def load_x(jb):
        for c in range(8):
            P.dma("sp", xT[:, c, 0:LAT], xT_d[jb, c * 128:(c + 1) * 128, :], writes=[rx[0], rx[1], rx[2], rx[3]])
            P.dma("sp", xT[:, c, LAT:NT], cxT_d[jb, c * 128:(c + 1) * 128, :], writes=[rx[4]])

    def dump(name, src, shape, dtype, rs):
        d = dbg_tensor(name, shape, dtype)
        P.dma("sp", d, src, reads=rs)

    prologue()
    if "mod" in dbg:
        d = dbg_tensor("dbg_mod", [128, 2 * 48 * 5], F32)
        P.dma("sp", d, modT[:].rearrange("p a b c -> p (a b c)"), reads=[r_mod])
    for jb in range(nb):
        load_x(jb)
        for l in range(nlayers):
            last = l == nlayers - 1
            tbs = [0, 1, 2, 3, 4]
            phase_proj(l, jb, tbs)
            if stop_after == "proj":
                dump("dbg_PR", PR_d, [RC, NT], F32, rPR)
                dump("dbg_PA", PA_d, [768, NT], F32, rPA)
                dump("dbg_PG", PG_d, [2048, NT], BF16, rPG)
                break
            if "noattn" not in dbg:
                phase_attn(l, not last)
            if "norwkv" not in dbg:
                phase_rwkv(l)
            if stop_after == "mix" or ("mix" in dbg and l == 0 and jb == 0):
                dump(f"dbg_YA", YA_d, [512, NT], BF16, rYA)
                dump(f"dbg_YR", YR_d, [512, NT], BF16, rYR)
                if stop_after == "mix":
                    break
            mtbs = tbs if not last else [0, 1, 2, 3]
            phase_merge(l, jb, mtbs)
            if "x" in dbg and jb == 0:
                d_ = dbg_tensor(f"dbg_xmix{l}", [128, 8 * NT], F32)
                P.dma("sp", d_, xT[:].rearrange("p c t -> p (c t)"), reads=rx)
            phase_ffn(l, jb, mtbs, moe=(l % 2 == 1))
            if "x" in dbg and jb == 0:
                d_ = dbg_tensor(f"dbg_xffn{l}", [128, 8 * NT], F32)
                P.dma("sp", d_, xT[:].rearrange("p c t -> p (c t)"), reads=rx)
            if stop_after == f"l{l}":
                break
        else:
            phase_final(jb)
        if stop_after is not None:
            break
    P.barrier()
    ninstr = P.ninstr
    P.close()
    return nc, dbg_out, ninstr


def make_inmaps(inp, ncores=8, nb=4):
    consts = host_consts()
    params = host_params(inp)
    xT = np.ascontiguousarray(np.transpose(inp["x"], (0, 2, 1)))
    cxT = np.ascontiguousarray(np.transpose(inp["ctx"], (0, 2, 1)))
    maps = []
    for i in range(ncores):
        m = {}
        m["xT"] = xT[i * nb:(i + 1) * nb]
        m["cxT"] = cxT[i * nb:(i + 1) * nb]
        c5 = np.concatenate([inp["c"][i * nb:(i + 1) * nb], np.zeros((4 - nb, D), np.float32), inp["c_ctx"][None, :]], axis=0)
        m["cT"] = np.ascontiguousarray(c5.reshape(5, 8, 128).transpose(2, 1, 0))
        m["params"] = params
        m.update(consts)
        for k in ("ada_w", "w_in", "rwkv_w2", "rwkv_a2", "rwkv_g2", "w_pa", "w_pb", "w_o", "ffn_wg", "ffn_wu", "ffn_wd",
                  "router", "moe_wg", "moe_wu", "moe_wd"):
            m[k] = inp[k]
        maps.append(m)
    return maps


def kernel(**inputs):
    inp = {k: np.asarray(v) for k, v in inputs.items()}
    ncores, nb = 8, 4
    nc, _, _ = build(nb=nb, nlayers=2)
    maps = make_inmaps(inp, ncores=ncores, nb=nb)
    res = run_bass_kernel_spmd(nc, maps, core_ids=list(range(ncores)))
    outT = np.concatenate([np.asarray(r["outT"]) for r in res.results], axis=0)
    return np.ascontiguousarray(np.transpose(outT, (0, 2, 1))).astype(np.float32)
```

```python
import contextlib
import numpy as np
import concourse.bass as bass
import concourse.mybir as mybir
from concourse.bass_utils import run_bass_kernel_spmd

F32 = mybir.dt.float32
BF16 = mybir.dt.bfloat16
AF = mybir.ActivationFunctionType
ALU = mybir.AluOpType
AX = mybir.AxisListType

ENGS = ("pe", "act", "dve", "pool", "sp")
EPOCH = 30000
NDMASEM = 48

D = 1024
LAT = 2048
CTX = 256
NT = LAT + CTX
NIN = 4608
RC = 1792
FF = 3584
NE = 8
LW = -0.6065306597126334
TBS = [(0, 512), (512, 512), (1024, 512), (1536, 512), (2048, 256)]
NPL = 130


class Res:
    __slots__ = ("last_w", "readers")

    def __init__(self):
        self.last_w = None
        self.readers = []


class Prog:
    def __init__(self, nc):
        self.nc = nc
        self.es = contextlib.ExitStack()
        self.seq = {e: 0 for e in ENGS}
        self.sems = []
        self.engsem = {}
        self.known = {e: {} for e in ENGS}
        self.dmasem = []
        self.ndma = 0
        self.dma_last = {}
        self.ninstr = 0
        self.eobj = {"pe": nc.tensor, "act": nc.scalar, "dve": nc.vector, "pool": nc.gpsimd, "sp": nc.sync}
        self.scope = [self.es]

    def new_sem(self, name):
        h = self.es.enter_context(self.nc.semaphore(name))
        self.sems.append(h)
        return len(self.sems) - 1

    def push_scope(self):
        st = contextlib.ExitStack()
        self.scope.append(st)

    def pop_scope(self):
        self.barrier()
        self.scope.pop().close()

    def sb(self, name, shape, dt):
        self.ntile = getattr(self, "ntile", 0) + 1
        return self.scope[-1].enter_context(self.nc.sbuf_tensor(f"{name}_{self.ntile}", list(shape), dt))

    def ps(self, name, shape, dt):
        return self.es.enter_context(self.nc.psum_tensor(name, list(shape), dt))

    def _waits_for(self, eng, reads, writes):
        need = {}
        known = self.known[eng]

        def add(key):
            if key is None:
                return
            s, v, ke = key
            if ke == eng and eng in ("pe", "sp"):
                return
            if known.get(s, 0) >= v:
                return
            if need.get(s, 0) < v:
                need[s] = v

        for r in reads:
            add(r.last_w)
        for w in writes:
            add(w.last_w)
            for k in w.readers:
                add(k)
        for s, v in need.items():
            known[s] = v
        return list(need.items())

    def _emit(self, eng, waits, fn, s, inc):
        e = self.eobj[eng]
        for ws, wv in waits:
            e.wait_ge(self.sems[ws], wv)
        if fn is not None:
            fn(e).then_inc(self.sems[s], inc)

    def op(self, eng, fn, reads=(), writes=()):
        waits = self._waits_for(eng, reads, writes)
        self.seq[eng] += 1
        n = self.seq[eng]
        ep = (n - 1) // EPOCH
        if (eng, ep) not in self.engsem:
            self.engsem[(eng, ep)] = self.new_sem(f"s_{eng}_{ep}")
        s = self.engsem[(eng, ep)]
        v = (n - 1) % EPOCH + 1
        key = (s, v, eng)
        self._emit(eng, waits, fn, s, 1)
        for r in reads:
            r.readers.append(key)
        for w in writes:
            w.last_w = key
            w.readers = []
        self.ninstr += 1
        return key

    def dma(self, eng, out_ap, in_ap, reads=(), writes=()):
        if not self.dmasem:
            self.dmasem = [self.new_sem(f"s_dma_{i}") for i in range(NDMASEM)]
        j = self.ndma
        self.ndma += 1
        slot = j % NDMASEM
        s = self.dmasem[slot]
        v = 16 * (j // NDMASEM + 1)
        waits = self._waits_for(eng, reads, writes)
        prev = self.dma_last.get(slot)
        if prev is not None and self.known[eng].get(prev[0], 0) < prev[1]:
            d = dict(waits)
            d[prev[0]] = max(d.get(prev[0], 0), prev[1])
            self.known[eng][prev[0]] = prev[1]
            waits = list(d.items())
        key = (s, v, "dma")
        self.dma_last[slot] = key
        self._emit(eng, waits, lambda e: e.dma_start(out=out_ap, in_=in_ap), s, 16)
        for r in reads:
            r.readers.append(key)
        for w in writes:
            w.last_w = key
            w.readers = []
        self.ninstr += 1
        return key

    def barrier(self):
        keys = []
        for (eng, ep), s in self.engsem.items():
            if self.seq[eng] > 0 and ep == (self.seq[eng] - 1) // EPOCH:
                keys.append((s, (self.seq[eng] - 1) % EPOCH + 1))
        for slot, k in self.dma_last.items():
            keys.append((k[0], k[1]))
        for eng in ENGS:
            for s, v in keys:
                if self.known[eng].get(s, 0) < v:
                    self.known[eng][s] = v
                    self.eobj[eng].wait_ge(self.sems[s], v)

    def close(self):
        while len(self.scope) > 1:
            self.scope.pop().close()
        self.es.close()


def _colz(v):
    return np.ascontiguousarray(np.asarray(v, np.float32).reshape(-1, 128).T)


def host_consts():
    c = {}
    c["ident"] = np.eye(128, dtype=np.float32)
    bd = np.zeros((128, 128), np.float32)
    bd[:64, :64] = 1.0
    bd[64:, 64:] = 1.0
    c["onesbd"] = bd
    p = np.arange(128)[:, None]
    f = np.arange(128)[None, :]
    lt = (p < f).astype(np.float32)
    le = (p <= f).astype(np.float32)
    gt = (p > f).astype(np.float32)
    ge = (p >= f).astype(np.float32)
    c["mask"] = np.concatenate([
        np.tile(lt, (1, 4)), np.tile(le, (1, 4)), np.tile(gt, (1, 2)),
        np.tile(gt, (1, 4)), np.tile(ge, (1, 4)), np.tile(lt, (1, 2)),
    ], axis=1).astype(np.float32)
    R = np.zeros((128, 128), np.float32)
    for i in range(64):
        R[2 * i + 1, 2 * i] = -1.0
        R[2 * i, 2 * i + 1] = 1.0
    c["rot"] = R
    t = np.arange(LAT)
    row = (t // 64).astype(np.float32)
    col = (t % 64).astype(np.float32)
    inv = (10000.0 ** (-np.arange(0, 32, 2, dtype=np.float32) / 32.0)).astype(np.float32)
    ang = np.concatenate([row[:, None] * inv[None, :], col[:, None] * inv[None, :]], axis=1).astype(np.float32)
    cos = np.cos(ang).astype(np.float32)
    sin = np.sin(ang).astype(np.float32)
    cosf = np.repeat(cos, 2, axis=1).T
    sinf = np.repeat(sin, 2, axis=1).T
    c["cos"] = np.ascontiguousarray(np.concatenate([cosf, cosf], axis=0))
    c["sin"] = np.ascontiguousarray(np.concatenate([sinf, sinf], axis=0))
    rm = np.ones((128, 512), np.float32)
    rm[:, ::128] = 0.0
    c["rmask"] = rm
    return c


def host_params(inp):
    cols = []
    for l in range(2):
        cols += [_colz(inp["norm1"][l]), _colz(inp["norm2"][l]), _colz(inp["ada_b"][l]),
                 _colz(inp["shift_mu"][l, 0]), _colz(inp["shift_mu"][l, 1]),
                 _colz(inp["rwkv_w0"][l, 0]), _colz(inp["rwkv_w0"][l, 1]),
                 _colz(inp["rwkv_a0"][l, 0]), _colz(inp["rwkv_a0"][l, 1]),
                 _colz(inp["rwkv_kk"][l]), _colz(inp["rwkv_ka"][l]), _colz(inp["rwkv_rk"][l]),
                 _colz(inp["lnx_w"][l]), _colz(inp["lnx_b"][l]),
                 _colz(np.tile(inp["q_norm"][l], 2)), _colz(np.tile(inp["k_norm"][l], 2))]
    cols.append(_colz(inp["final_norm"]))
    return np.ascontiguousarray(np.concatenate(cols, axis=1))


PO = {}
_o = 0
for _n, _w in [("norm1", 8), ("norm2", 8), ("adab", 48), ("mu0", 14), ("mu1", 14), ("w0_0", 4), ("w0_1", 4),
               ("a0_0", 4), ("a0_1", 4), ("kk", 4), ("ka", 4), ("rk", 4), ("lnxw", 4), ("lnxb", 4), ("qn", 1), ("kn", 1)]:
    PO[_n] = _o
    _o += _w
assert _o == NPL


def build(nb=4, nlayers=2, dbg=None, stop_after=None):
    dbg = dbg or set()
    nc = bass.Bass("TRN2", target_bir_lowering=False)
    P = Prog(nc)
    dt = nc.dram_tensor

    def din(name, shape, dtype=F32):
        return dt(name, list(shape), dtype, kind="ExternalInput").ap()

    def dscr(name, shape, dtype):
        return dt(name, list(shape), dtype, kind="Internal").ap()

    xT_d = din("xT", [nb, D, LAT])
    cxT_d = din("cxT", [nb, D, CTX])
    cT_d = din("cT", [128, 8, 5])
    par_d = din("params", [128, 2 * NPL + 8])
    ident_d = din("ident", [128, 128])
    onesbd_d = din("onesbd", [128, 128])
    mask_d = din("mask", [128, 2560])
    rot_d = din("rot", [128, 128])
    cos_d = din("cos", [128, LAT])
    sin_d = din("sin", [128, LAT])
    rmask_d = din("rmask", [128, 512])
    ada_w_d = din("ada_w", [2, D, 6 * D])
    w_in_d = din("w_in", [2, D, NIN])
    w2_d = din("rwkv_w2", [2, 2, 64, 512])
    a2_d = din("rwkv_a2", [2, 2, 64, 512])
    g2_d = din("rwkv_g2", [2, 128, 512])
    w_pa_d = din("w_pa", [2, 512, D])
    w_pb_d = din("w_pb", [2, 512, D])
    w_o_d = din("w_o", [2, D, D])
    ffn_wg_d = din("ffn_wg", [1, D, FF])
    ffn_wu_d = din("ffn_wu", [1, D, FF])
    ffn_wd_d = din("ffn_wd", [1, FF, D])
    router_d = din("router", [1, D, NE])
    if nlayers > 1:
        moe_wg_d = din("moe_wg", [1, NE, D, FF])
        moe_wu_d = din("moe_wu", [1, NE, D, FF])
        moe_wd_d = din("moe_wd", [1, NE, FF, D])
    outT_d = dt("outT", [nb, D, LAT], F32, kind="ExternalOutput").ap()

    PR_d = dscr("PR", [RC, NT], F32)
    PA_d = dscr("PA", [768, NT], F32)
    PG_d = dscr("PG", [2048, NT], BF16)
    YR_d = dscr("YR", [512, NT], BF16)
    YA_d = dscr("YA", [512, NT], BF16)
    rPR = [Res() for _ in range(14)]
    rPA = [Res() for _ in range(6)]
    rPG = [Res() for _ in range(16)]
    rYR = [Res() for _ in range(4)]
    rYA = [Res() for _ in range(4)]
    dbg_out = {}

    def dbg_tensor(name, shape, dtype=F32):
        dbg_out[name] = dt(name, list(shape), dtype, kind="ExternalOutput").ap()
        return dbg_out[name]

    xT = P.sb("xT_sb", [128, 8, NT], F32)
    rx = [Res() for _ in TBS]
    par = P.sb("par", [128, 2 * NPL + 8], F32)
    r_par = Res()
    dpar = P.sb("dpar", [128, 64], F32)
    r_dpar = Res()
    cst = P.sb("cst", [128, 8], F32)
    r_cst = Res()
    modT = P.sb("modT", [128, 2, 48, 5], F32)
    r_mod = Res()
    mdv = P.sb("mdv", [128, 2, 6, 8, 5], F32)
    r_mdv = Res()
    ident_f = P.sb("ident_f", [128, 128], F32)
    ident_b = P.sb("ident_b", [128, 128], BF16)
    ident2_f = P.sb("ident2_f", [128, 256], F32)
    onesbd_f = P.sb("onesbd_f", [128, 128], F32)
    onesbd_b = P.sb("onesbd_b", [128, 128], BF16)
    ones_f = P.sb("ones_f", [128, 128], F32)
    rot_b = P.sb("rot_b", [128, 128], BF16)
    r_const = Res()

    banks = [P.ps(f"bank{i}", [128, 512], F32) for i in range(8)]
    rbank = [Res() for _ in range(8)]
    bank_ctr = [0]

    def nbank(lo=0, hi=8):
        i = lo + bank_ctr[0] % (hi - lo)
        bank_ctr[0] += 1
        return banks[i], rbank[i]

    def pcol(l, name, j=0):
        o = l * NPL + PO[name] + j
        return par[:, o:o + 1]

    def prologue():
        P.push_scope()
        stg = P.sb("pl_stg", [128, 128], F32)
        r_stg = Res()
        P.dma("sp", par[:], par_d, writes=[r_par])
        P.dma("sp", ident_f[:], ident_d, writes=[r_const])
        P.dma("sp", onesbd_f[:], onesbd_d, writes=[r_const])
        P.dma("sp", stg[:], rot_d, writes=[r_stg])
        P.op("dve", lambda e: e.tensor_copy(out=rot_b[:], in_=stg[:]), reads=[r_stg], writes=[r_const])
        P.op("dve", lambda e: e.tensor_copy(out=ident_b[:], in_=ident_f[:]), reads=[r_const], writes=[r_const])
        P.op("dve", lambda e: e.tensor_copy(out=ident2_f[:, 0:128], in_=ident_f[:]), reads=[r_const], writes=[r_const])
        P.op("dve", lambda e: e.tensor_copy(out=ident2_f[:, 128:256], in_=ident_f[:]), reads=[r_const], writes=[r_const])
        P.op("dve", lambda e: e.tensor_copy(out=onesbd_b[:], in_=onesbd_f[:]), reads=[r_const], writes=[r_const])
        P.op("dve", lambda e: e.memset(ones_f[:], 1.0), writes=[r_const])
        P.op("dve", lambda e: e.memset(cst[:, 0:1], 1e-6), writes=[r_cst])
        P.op("dve", lambda e: e.memset(cst[:, 1:2], 64e-5), writes=[r_cst])
        P.op("dve", lambda e: e.memset(cst[:, 2:3], 1.0), writes=[r_cst])
        P.op("dve", lambda e: e.memset(cst[:, 3:4], 0.0), writes=[r_cst])
        P.op("dve", lambda e: e.memset(cst[:, 4:5], 1e-24), writes=[r_cst])
        for l in range(2):
            b0 = l * 32
            m0 = par[:, l * NPL + PO["mu0"]: l * NPL + PO["mu0"] + 14]
            m1 = par[:, l * NPL + PO["mu1"]: l * NPL + PO["mu1"] + 14]
            P.op("dve", lambda e, m0=m0, m1=m1, b0=b0: e.tensor_tensor(out=dpar[:, b0:b0 + 14], in0=m0, in1=m1, op=ALU.add),
                 reads=[r_par], writes=[r_dpar])
            P.op("dve", lambda e, b0=b0: e.tensor_scalar(out=dpar[:, b0:b0 + 14], in0=dpar[:, b0:b0 + 14], scalar1=-1.0, scalar2=1.0,
                                                        op0=ALU.mult, op1=ALU.add), reads=[r_dpar], writes=[r_dpar])
            ka = par[:, l * NPL + PO["ka"]: l * NPL + PO["ka"] + 4]
            P.op("dve", lambda e, ka=ka, b0=b0: e.tensor_scalar(out=dpar[:, b0 + 14:b0 + 18], in0=ka, scalar1=-1.0, scalar2=1.0,
                                                               op0=ALU.mult, op1=ALU.add), reads=[r_par], writes=[r_dpar])
            qn = pcol(l, "qn")
            P.op("dve", lambda e, qn=qn, b0=b0: e.tensor_scalar(out=dpar[:, b0 + 18:b0 + 19], in0=qn, scalar1=0.125, scalar2=None,
                                                               op0=ALU.mult), reads=[r_par], writes=[r_dpar])
        cact = P.sb("pl_cact", [128, 8, 5], F32)
        r_cact = Res()
        P.dma("sp", cact[:], cT_d, writes=[r_cact])
        P.op("act", lambda e: e.activation(out=cact[:], in_=cact[:], func=AF.Silu), reads=[r_cact], writes=[r_cact])
        aw = [P.sb(f"pl_aw{i}", [128, 8, 512], F32) for i in range(2)]
        r_aw = [Res(), Res()]
        for l in range(nlayers):
            for cb in range(12):
                i = cb % 2
                src = ada_w_d[l].rearrange("(kc p) n -> p kc n", p=128)[:, :, cb * 512:(cb + 1) * 512]
                P.dma("sp", aw[i][:], src, writes=[r_aw[i]])
                for cc in range(4):
                    ch = cb * 4 + cc
                    bk, rb = nbank()
                    for kc in range(8):
                        P.op("pe", lambda e, bk=bk, i=i, kc=kc, cc=cc: e.matmul(
                            bk[:, 0:5], aw[i][:, kc, cc * 128:(cc + 1) * 128], cact[:, kc, :], start=(kc == 0), stop=(kc == 7)),
                            reads=[r_aw[i], r_cact], writes=[rb])
                    bcol = pcol(l, "adab", ch)
                    P.op("dve", lambda e, bk=bk, l=l, ch=ch, bcol=bcol: e.tensor_scalar(
                        out=modT[:, l, ch, :], in0=bk[:, 0:5], scalar1=bcol, scalar2=None, op0=ALU.add),
                        reads=[rb, r_par], writes=[r_mod])
            for half, nm in ((0, "norm1"), (1, "norm2")):
                base = half * 24
                for c in range(8):
                    nrm = pcol(l, nm, c)
                    P.op("dve", lambda e, l=l, half=half, c=c, base=base, nrm=nrm: e.tensor_scalar(
                        out=mdv[:, l, half * 3 + 0, c, :], in0=modT[:, l, base + 8 + c, :], scalar1=1.0, scalar2=nrm,
                        op0=ALU.add, op1=ALU.mult), reads=[r_mod, r_par], writes=[r_mdv])
                    P.op("dve", lambda e, l=l, half=half, c=c, base=base: e.tensor_copy(
                        out=mdv[:, l, half * 3 + 1, c, :], in_=modT[:, l, base + c, :]), reads=[r_mod], writes=[r_mdv])
                    P.op("dve", lambda e, l=l, half=half, c=c, base=base: e.tensor_copy(
                        out=mdv[:, l, half * 3 + 2, c, :], in_=modT[:, l, base + 16 + c, :]), reads=[r_mod], writes=[r_mdv])
        P.pop_scope()

    def norm_block(l, half, jb, tb, hT, rh, sqb, r_sqb, tmpb, r_tmpb, rstd, r_rstd, hT32=None, rh32=None):
        s, n = TBS[tb]
        j = 4 if tb == 4 else jb
        bk, rb = nbank()
        for c in range(8):
            i = c % 2
            P.op("act", lambda e, c=c, i=i: e.activation(out=sqb[i][:, 0:n], in_=xT[:, c, s:s + n], func=AF.Square),
                 reads=[rx[tb]], writes=[r_sqb[i]])
            P.op("pe", lambda e, c=c, i=i, bk=bk: e.matmul(bk[:, 0:n], ones_f[:], sqb[i][:, 0:n], start=(c == 0), stop=(c == 7)),
                 reads=[r_sqb[i], r_const], writes=[rb])
        P.op("act", lambda e, bk=bk: e.activation(out=rstd[:, 0:n], in_=bk[:, 0:n], func=AF.Sqrt, scale=1.0 / D, bias=cst[:, 0:1]),
             reads=[rb, r_cst], writes=[r_rstd])
        P.op("dve", lambda e: e.reciprocal(out=rstd[:, 0:n], in_=rstd[:, 0:n]), reads=[r_rstd], writes=[r_rstd])
        for c in range(8):
            i = c % 2
            g = mdv[:, l, half * 3 + 0, c, j:j + 1]
            sh = mdv[:, l, half * 3 + 1, c, j:j + 1]
            P.op("dve", lambda e, c=c, i=i, g=g: e.scalar_tensor_tensor(out=tmpb[i][:, 0:n], in0=xT[:, c, s:s + n], scalar=g,
                                                                      in1=rstd[:, 0:n], op0=ALU.mult, op1=ALU.mult),
                 reads=[rx[tb], r_mdv, r_rstd], writes=[r_tmpb[i]])
            P.op("act", lambda e, c=c, i=i, sh=sh: e.activation(out=hT[:, c, s:s + n], in_=tmpb[i][:, 0:n], func=AF.Identity,
                                                                scale=1.0, bias=sh),
                 reads=[r_tmpb[i], r_mdv], writes=[rh[tb]])
            if hT32 is not None:
                P.op("pool", lambda e, c=c, i=i, sh=sh: e.tensor_scalar(out=hT32[:, c, 0:n], in0=tmpb[i][:, 0:n], scalar1=sh, scalar2=None,
                                                                       op0=ALU.add),
                     reads=[r_tmpb[i], r_mdv], writes=[rh32])

    def phase_proj(l, jb, tbs):
        P.push_scope()
        hT = P.sb("hT", [128, 8, NT], BF16)
        rh = [Res() for _ in TBS]
        sqb = [P.sb(f"p1_sq{i}", [128, 512], F32) for i in range(2)]
        r_sqb = [Res(), Res()]
        tmpb = [P.sb(f"p1_tmp{i}", [128, 512], F32) for i in range(2)]
        r_tmpb = [Res(), Res()]
        rstd = P.sb("p1_rstd", [128, 512], F32)
        r_rstd = Res()
        for tb in tbs:
            norm_block(l, 0, jb, tb, hT, rh, sqb, r_sqb, tmpb, r_tmpb, rstd, r_rstd)
        if "h" in dbg and l == 0:
            hd = dbg_tensor("dbg_h", [D, NT], BF16)
            for c in range(8):
                P.dma("sp", hd[c * 128:(c + 1) * 128, :], hT[:, c, :], reads=rh)
        wst = [P.sb(f"p2_wst{i}", [128, 8, 256], F32) for i in range(2)]
        r_wst = [Res(), Res()]
        wbf = [P.sb(f"p2_wbf{i}", [128, 8, 256], BF16) for i in range(2)]
        r_wbf = [Res(), Res()]
        pf = P.sb("p2_pf", [128, NT + 3], F32)
        r_pf = Res()
        po = [P.sb(f"p2_po{i}", [128, NT], F32) for i in range(2)]
        r_po = [Res(), Res()]
        pg = [P.sb(f"p2_pg{i}", [128, NT], BF16) for i in range(2)]
        r_pg = [Res(), Res()]
        P.op("pool", lambda e: e.memset(pf[:], 0.0), writes=[r_pf])
        wsrc = w_in_d[l].rearrange("(kc p) n -> p kc n", p=128)
        nctx = 4 in tbs
        for cg in range(18):
            i = cg % 2
            P.dma("sp", wst[i][:], wsrc[:, :, cg * 256:(cg + 1) * 256], writes=[r_wst[i]])
            P.op("pool", lambda e, i=i: e.tensor_copy(out=wbf[i][:], in_=wst[i][:]), reads=[r_wst[i]], writes=[r_wbf[i]])
            for cc in range(2):
                gc = cg * 2 + cc
                o = gc % 2
                for tb in tbs:
                    s, n = TBS[tb]
                    bk, rb = nbank()
                    for kc in range(8):
                        P.op("pe", lambda e, bk=bk, i=i, kc=kc, cc=cc, s=s, n=n: e.matmul(
                            bk[:, 0:n], wbf[i][:, kc, cc * 128:(cc + 1) * 128], hT[:, kc, s:s + n], start=(kc == 0), stop=(kc == 7)),
                            reads=[r_wbf[i], rh[tb]], writes=[rb])
                    if gc < 14:
                        off = 1 + s if tb < 4 else 2 + s
                        P.op("act", lambda e, bk=bk, off=off, n=n: e.copy(out=pf[:, off:off + n], in_=bk[:, 0:n]),
                             reads=[rb], writes=[r_pf])
                    elif gc < 20:
                        P.op("act", lambda e, bk=bk, o=o, s=s, n=n: e.copy(out=po[o][:, s:s + n], in_=bk[:, 0:n]),
                             reads=[rb], writes=[r_po[o]])
                    else:
                        P.op("act", lambda e, bk=bk, o=o, s=s, n=n: e.activation(out=pg[o][:, s:s + n], in_=bk[:, 0:n], func=AF.Sigmoid),
                             reads=[rb], writes=[r_pg[o]])
                if gc < 14:
                    muc = dpar[:, l * 32 + gc: l * 32 + gc + 1]
                    mu0 = pcol(l, "mu0", gc)
                    mu1 = pcol(l, "mu1", gc)
                    segs = [(0, LAT, 1)] + ([(LAT, CTX, 2 + LAT)] if nctx else [])
                    for (os_, n, ps_) in segs:
                        P.op("act", lambda e, o=o, os_=os_, n=n, ps_=ps_, muc=muc: e.activation(
                            out=po[o][:, os_:os_ + n], in_=pf[:, ps_:ps_ + n], func=AF.Identity, scale=muc, bias=cst[:, 3:4]),
                            reads=[r_pf, r_dpar, r_cst], writes=[r_po[o]])
                        P.op("dve", lambda e, o=o, os_=os_, n=n, ps_=ps_, mu0=mu0: e.scalar_tensor_tensor(
                            out=po[o][:, os_:os_ + n], in0=pf[:, ps_ - 1:ps_ - 1 + n], scalar=mu0, in1=po[o][:, os_:os_ + n],
                            op0=ALU.mult, op1=ALU.add), reads=[r_pf, r_par, r_po[o]], writes=[r_po[o]])
                        P.op("dve", lambda e, o=o, os_=os_, n=n, ps_=ps_, mu1=mu1: e.scalar_tensor_tensor(
                            out=po[o][:, os_:os_ + n], in0=pf[:, ps_ + 1:ps_ + 1 + n], scalar=mu1, in1=po[o][:, os_:os_ + n],
                            op0=ALU.mult, op1=ALU.add), reads=[r_pf, r_par, r_po[o]], writes=[r_po[o]])
                    P.dma("sp", PR_d[gc * 128:(gc + 1) * 128, :], po[o][:], reads=[r_po[o]], writes=[rPR[gc]])
                elif gc < 20:
                    P.dma("sp", PA_d[(gc - 14) * 128:(gc - 13) * 128, :], po[o][:], reads=[r_po[o]], writes=[rPA[gc - 14]])
                else:
                    P.dma("sp", PG_d[(gc - 20) * 128:(gc - 19) * 128, :], pg[o][:], reads=[r_pg[o]], writes=[rPG[gc - 20]])
        P.pop_scope()

    KR_d = dscr("KR", [128, NT], BF16)
    rKR = Res()

    def phase_attn(l, do_ctx):
        P.push_scope()
        cos_t = P.sb("at_cos", [128, LAT], F32)
        sin_t = P.sb("at_sin", [128, LAT], F32)
        r_cs = Res()
        P.dma("sp", cos_t[:], cos_d, writes=[r_cs])
        P.dma("sp", sin_t[:], sin_d, writes=[r_cs])
        ones_b = P.sb("at_ones", [128, 64], BF16)
        P.op("pool", lambda e: e.memset(ones_b[:], 1.0), writes=[r_cs])
        raw = P.sb("at_raw", [128, NT], F32)
        r_raw = Res()
        QR = [P.sb(f"at_qr{c}", [128, NT], BF16) for c in range(4)]
        r_QR = [Res() for _ in range(4)]
        krp = P.sb("at_krp", [128, NT], BF16)
        r_krp = Res()
        KX = [[P.sb(f"at_kx{g}{hf}", [128, NT], BF16) for hf in range(2)] for g in range(2)]
        r_KX = [[Res(), Res()], [Res(), Res()]]
        Vt = P.sb("at_vt", [128, 18, 128], BF16)
        r_Vt = Res()
        sq = P.sb("at_sq", [128, 512], BF16)
        r_sq = Res()
        rs = P.sb("at_rs", [128, 512], F32)
        r_rs = Res()
        qn = P.sb("at_qn", [128, 512], BF16)
        r_qn = Res()
        t1 = P.sb("at_t1", [128, 512], F32)
        r_t1 = Res()
        t2 = P.sb("at_t2", [128, 512], F32)
        r_t2 = Res()

        def proc_chunk(src_rows, gain, dst, r_dst):
            P.dma("sp", raw[:], PA_d[src_rows * 128:(src_rows + 1) * 128, :], reads=[rPA[src_rows]], writes=[r_raw])
            for tb in range(5):
                s, n = TBS[tb]
                P.op("act", lambda e: e.activation(out=sq[:, 0:n], in_=raw[:, s:s + n], func=AF.Square), reads=[r_raw], writes=[r_sq])
                bk, rb = nbank()
                P.op("pe", lambda e: e.matmul(bk[:, 0:n], onesbd_b[:], sq[:, 0:n], start=True, stop=True), reads=[r_sq, r_const], writes=[rb])
                P.op("act", lambda e: e.activation(out=rs[:, 0:n], in_=bk[:, 0:n], func=AF.Sqrt, scale=1.0 / 64, bias=cst[:, 0:1]),
                     reads=[rb, r_cst], writes=[r_rs])
                P.op("dve", lambda e: e.reciprocal(out=rs[:, 0:n], in_=rs[:, 0:n]), reads=[r_rs], writes=[r_rs])
                if tb < 4:
                    P.op("dve", lambda e: e.scalar_tensor_tensor(out=qn[:, 0:n], in0=raw[:, s:s + n], scalar=gain, in1=rs[:, 0:n],
                                                                op0=ALU.mult, op1=ALU.mult), reads=[r_raw, r_rs, r_par, r_dpar], writes=[r_qn])
                    bk2, rb2 = nbank()
                    P.op("pe", lambda e: e.matmul(bk2[:, 0:n], rot_b[:], qn[:, 0:n], start=True, stop=True), reads=[r_qn, r_const], writes=[rb2])
                    P.op("pool", lambda e: e.tensor_tensor(out=t1[:, 0:n], in0=qn[:, 0:n], in1=cos_t[:, s:s + n], op=ALU.mult),
                         reads=[r_qn, r_cs], writes=[r_t1])
                    P.op("dve", lambda e: e.tensor_tensor(out=t2[:, 0:n], in0=bk2[:, 0:n], in1=sin_t[:, s:s + n], op=ALU.mult),
                         reads=[rb2, r_cs], writes=[r_t2])
                    P.op("pool", lambda e: e.tensor_tensor(out=dst[:, s:s + n], in0=t1[:, 0:n], in1=t2[:, 0:n], op=ALU.add),
                         reads=[r_t1, r_t2], writes=[r_dst])
                else:
                    P.op("dve", lambda e: e.scalar_tensor_tensor(out=dst[:, s:s + n], in0=raw[:, s:s + n], scalar=gain, in1=rs[:, 0:n],
                                                                op0=ALU.mult, op1=ALU.mult), reads=[r_raw, r_rs, r_par, r_dpar], writes=[r_dst])

        qg = dpar[:, l * 32 + 18:l * 32 + 19]
        for c in range(4):
            proc_chunk(c, qg, QR[c], r_QR[c])
        proc_chunk(4, pcol(l, "kn"), krp, r_krp)
        P.dma("sp", KR_d, krp[:], reads=[r_krp], writes=[rKR])
        for g in range(2):
            for hf in range(2):
                P.op("pool", lambda e, g=g, hf=hf: e.memset(KX[g][hf][:], 0.0), writes=[r_KX[g][hf]])
                P.dma("sp", KX[g][hf][hf * 64:(hf + 1) * 64, :], KR_d[g * 64:(g + 1) * 64, :], reads=[rKR], writes=[r_KX[g][hf]])
        P.dma("sp", raw[:], PA_d[5 * 128:6 * 128, :], reads=[rPA[5]], writes=[r_raw])
        for k4 in range(0, 18, 4):
            nk = min(4, 18 - k4)
            bk, rb = nbank()
            for i in range(nk):
                kc = k4 + i
                P.op("pe", lambda e, i=i, kc=kc: e.transpose(bk[:, i * 128:(i + 1) * 128], raw[:, kc * 128:(kc + 1) * 128], ident_f[:]),
                     reads=[r_raw, r_const], writes=[rb])
            P.op("act", lambda e, k4=k4, nk=nk: e.copy(out=Vt[:, k4:k4 + nk, :].rearrange("p a b -> p (a b)"), in_=bk[:, 0:nk * 128]),
                 reads=[rb], writes=[r_Vt])
        if "attn" in dbg and l == 0:
            dq = dbg_tensor("dbg_qr", [512, NT], BF16)
            for c in range(4):
                P.dma("sp", dq[c * 128:(c + 1) * 128, :], QR[c][:], reads=[r_QR[c]])
            dk = dbg_tensor("dbg_kr", [128, NT], BF16)
            P.dma("sp", dk, krp[:], reads=[r_krp])
        pT = [P.sb(f"at_pT{i}", [128, 512], BF16) for i in range(3)]
        r_pT = [Res() for _ in range(3)]
        rec = P.sb("at_rec", [64, 512], F32)
        r_rec = Res()
        yo = [P.sb(f"at_yo{i}", [64, 512], BF16) for i in range(2)]
        r_yo = [Res(), Res()]
        cnt = 0
        for h in range(8):
            c, hf, g = h // 2, h % 2, h // 4
            for tb in range(5 if do_ctx else 4):
                s, n = TBS[tb]
                kcs = list(range(18)) if tb < 4 else [16, 17]
                io = cnt % 2
                bkO, rbO = banks[io * 2], rbank[io * 2]
                bkD, rbD = banks[io * 2 + 1], rbank[io * 2 + 1]
                for ki, kc in enumerate(kcs):
                    bkS, rbS = nbank(4, 8)
                    ip = (cnt * 18 + ki) % 3
                    P.op("pe", lambda e, bkS=bkS, kc=kc: e.matmul(bkS[:, 0:n], KX[g][hf][:, kc * 128:(kc + 1) * 128], QR[c][:, s:s + n],
                                                               start=True, stop=True), reads=[r_KX[g][hf], r_QR[c]], writes=[rbS])
                    P.op("act", lambda e, bkS=bkS, ip=ip: e.activation(out=pT[ip][:, 0:n], in_=bkS[:, 0:n], func=AF.Exp),
                         reads=[rbS], writes=[r_pT[ip]])
                    P.op("pe", lambda e, kc=kc, ip=ip, ki=ki: e.matmul(bkO[0:64, 0:n], Vt[:, kc, g * 64:(g + 1) * 64], pT[ip][:, 0:n],
                                                                      start=(ki == 0), stop=(ki == len(kcs) - 1)),
                         reads=[r_Vt, r_pT[ip]], writes=[rbO])
                    P.op("pe", lambda e, ip=ip, ki=ki: e.matmul(bkD[0:64, 0:n], ones_b[:], pT[ip][:, 0:n],
                                                               start=(ki == 0), stop=(ki == len(kcs) - 1)),
                         reads=[r_cs, r_pT[ip]], writes=[rbD])
                P.op("dve", lambda e: e.reciprocal(out=rec[:, 0:n], in_=bkD[0:64, 0:n]), reads=[rbD], writes=[r_rec])
                P.op("dve", lambda e, io=io: e.tensor_tensor(out=yo[io][:, 0:n], in0=bkO[0:64, 0:n], in1=rec[:, 0:n], op=ALU.mult),
                     reads=[rbO, r_rec], writes=[r_yo[io]])
                P.dma("sp", YA_d[h * 64:(h + 1) * 64, s:s + n], yo[io][:, 0:n], reads=[r_yo[io]], writes=[rYA[h // 2]])
                cnt += 1
        P.pop_scope()

    def load_cast(dst_ap, src_ap, stg_list, r_stg_list, ctr, r_dst, shape_slice):
        i = ctr[0] % len(stg_list)
        ctr[0] += 1
        st = shape_slice(stg_list[i])
        P.dma("sp", st, src_ap, writes=[r_stg_list[i]])
        P.op("pool", lambda e: e.tensor_copy(out=dst_ap, in_=st), reads=[r_stg_list[i]], writes=[r_dst])

    def phase_merge(l, jb, tbs):
        P.push_scope()
        stg = [P.sb(f"mg_stg{i}", [128, 1024], F32) for i in range(2)]
        r_stg = [Res(), Res()]
        ctr = [0]
        wpa = P.sb("mg_wpa", [128, 4, 1024], BF16)
        wpb = P.sb("mg_wpb", [128, 4, 1024], BF16)
        wo = P.sb("mg_wo", [128, 8, 1024], BF16)
        r_w = Res()
        for kc in range(4):
            load_cast(wpa[:, kc, :], w_pa_d[l, kc * 128:(kc + 1) * 128, :], stg, r_stg, ctr, r_w, lambda t: t[:])
            load_cast(wpb[:, kc, :], w_pb_d[l, kc * 128:(kc + 1) * 128, :], stg, r_stg, ctr, r_w, lambda t: t[:])
        for kc in range(8):
            load_cast(wo[:, kc, :], w_o_d[l, kc * 128:(kc + 1) * 128, :], stg, r_stg, ctr, r_w, lambda t: t[:])
        yr = P.sb("mg_yr", [128, 4, 512], BF16)
        ya = P.sb("mg_ya", [128, 4, 512], BF16)
        ga = P.sb("mg_ga", [128, 8, 512], BF16)
        gb = P.sb("mg_gb", [128, 8, 512], BF16)
        r_in = Res()
        z = P.sb("mg_z", [128, 8, 512], BF16)
        r_z = Res()
        ta = [P.sb(f"mg_ta{i}", [128, 512], F32) for i in range(2)]
        r_ta = [Res(), Res()]
        tb_ = [P.sb(f"mg_tb{i}", [128, 512], F32) for i in range(2)]
        r_tb = [Res(), Res()]
        for tb in tbs:
            s, n = TBS[tb]
            j = 4 if tb == 4 else jb
            P.dma("sp", yr[:, :, 0:n], YR_d.rearrange("(c p) t -> p c t", p=128)[:, :, s:s + n], reads=rYR, writes=[r_in])
            P.dma("sp", ya[:, :, 0:n], YA_d.rearrange("(c p) t -> p c t", p=128)[:, :, s:s + n], reads=rYA, writes=[r_in])
            P.dma("sp", ga[:, :, 0:n], PG_d[0:1024, :].rearrange("(c p) t -> p c t", p=128)[:, :, s:s + n], reads=rPG, writes=[r_in])
            P.dma("sp", gb[:, :, 0:n], PG_d[1024:2048, :].rearrange("(c p) t -> p c t", p=128)[:, :, s:s + n], reads=rPG, writes=[r_in])
            for dc in range(8):
                i = dc % 2
                bkA, rbA = nbank()
                for kc in range(4):
                    P.op("pe", lambda e, kc=kc: e.matmul(bkA[:, 0:n], wpa[:, kc, dc * 128:(dc + 1) * 128], yr[:, kc, 0:n],
                                                        start=(kc == 0), stop=(kc == 3)), reads=[r_w, r_in], writes=[rbA])
                bkB, rbB = nbank()
                for kc in range(4):
                    P.op("pe", lambda e, kc=kc: e.matmul(bkB[:, 0:n], wpb[:, kc, dc * 128:(dc + 1) * 128], ya[:, kc, 0:n],
                                                        start=(kc == 0), stop=(kc == 3)), reads=[r_w, r_in], writes=[rbB])
                P.op("dve", lambda e: e.tensor_tensor(out=ta[i][:, 0:n], in0=bkA[:, 0:n], in1=ga[:, dc, 0:n], op=ALU.mult),
                     reads=[rbA, r_in], writes=[r_ta[i]])
                P.op("dve", lambda e: e.tensor_tensor(out=tb_[i][:, 0:n], in0=bkB[:, 0:n], in1=gb[:, dc, 0:n], op=ALU.mult),
                     reads=[rbB, r_in], writes=[r_tb[i]])
                P.op("pool", lambda e: e.tensor_tensor(out=z[:, dc, 0:n], in0=ta[i][:, 0:n], in1=tb_[i][:, 0:n], op=ALU.add),
                     reads=[r_ta[i], r_tb[i]], writes=[r_z])
            for dc in range(8):
                bkO, rbO = nbank()
                for kc in range(8):
                    P.op("pe", lambda e, kc=kc: e.matmul(bkO[:, 0:n], wo[:, kc, dc * 128:(dc + 1) * 128], z[:, kc, 0:n],
                                                        start=(kc == 0), stop=(kc == 7)), reads=[r_w, r_z], writes=[rbO])
                gt = mdv[:, l, 2, dc, j:j + 1]
                P.op("dve", lambda e, gt=gt: e.scalar_tensor_tensor(out=xT[:, dc, s:s + n], in0=bkO[:, 0:n], scalar=gt, in1=xT[:, dc, s:s + n],
                                                                   op0=ALU.mult, op1=ALU.add), reads=[rbO, r_mdv, rx[tb]], writes=[rx[tb]])
        P.pop_scope()

    def phase_ffn(l, jb, tbs, moe):
        P.push_scope()
        hT = P.sb("hT2", [128, 8, NT], BF16)
        rh = [Res() for _ in TBS]
        if moe:
            wgT = P.sb("f1_wgT", [8, LAT], F32)
            r_wgT = Res()
            sel_all = P.sb("f1_sel", [8, NE, 128], F32)
            r_sel = Res()
        P.push_scope()
        sqb = [P.sb(f"f1_sq{i}", [128, 512], F32) for i in range(2)]
        r_sqb = [Res(), Res()]
        tmpb = [P.sb(f"f1_tmp{i}", [128, 512], F32) for i in range(2)]
        r_tmpb = [Res(), Res()]
        rstd = P.sb("f1_rstd", [128, 512], F32)
        r_rstd = Res()
        if moe:
            h32 = P.sb("f1_h32", [128, 8, 512], F32)
            r_h32 = Res()
            rt_f = P.sb("f1_rt", [128, 8, NE], F32)
            r_rt = Res()
            P.dma("sp", rt_f[:], router_d[0].rearrange("(kc p) e -> p kc e", p=128), writes=[r_rt])
            lgT = P.sb("f1_lgT", [8, 512], F32)
            r_lgT = Res()
            sm = P.sb("f1_sm", [128, 64], F32)
            r_sm = Res()
            for e_ in range(NE):
                P.op("dve", lambda e, e_=e_: e.tensor_copy(out=sel_all[:, e_, :], in_=bass.AP(ident_f, e_, [[128, 8], [0, 128]])),
                     reads=[r_const], writes=[r_sel])
        for tb in tbs:
            s, n = TBS[tb]
            if moe:
                norm_block(l, 1, jb, tb, hT, rh, sqb, r_sqb, tmpb, r_tmpb, rstd, r_rstd, hT32=h32, rh32=r_h32)
                bk, rb = nbank()
                for kc in range(8):
                    P.op("pe", lambda e, kc=kc: e.matmul(bk[0:8, 0:n], rt_f[:, kc, :], h32[:, kc, 0:n], start=(kc == 0), stop=(kc == 7)),
                         reads=[r_rt, r_h32], writes=[rb])
                P.op("act", lambda e: e.copy(out=lgT[:, 0:n], in_=bk[0:8, 0:n]), reads=[rb], writes=[r_lgT])
                bk2, rb2 = nbank()
                for sb_ in range(n // 128):
                    bk1, rb1 = nbank()
                    P.op("pe", lambda e: e.transpose(bk1[:, 0:8], lgT[:, sb_ * 128:(sb_ + 1) * 128], ident_f[0:8, 0:8]),
                         reads=[r_lgT, r_const], writes=[rb1])
                    lg, m1, eq, lg2, m2, sel, ex, den = (sm[:, 0:8], sm[:, 8:9], sm[:, 16:24], sm[:, 24:32], sm[:, 9:10], sm[:, 32:40],
                                                         sm[:, 40:48], sm[:, 10:11])
                    nm1 = sm[:, 11:12]
                    wg_ = sm[:, 48:56]
                    P.op("dve", lambda e: e.tensor_copy(out=lg, in_=bk1[:, 0:8]), reads=[rb1], writes=[r_sm])
                    P.op("dve", lambda e: e.tensor_reduce(out=m1, in_=lg, axis=AX.X, op=ALU.max), reads=[r_sm], writes=[r_sm])
                    P.op("dve", lambda e: e.tensor_scalar(out=eq, in0=lg, scalar1=m1, scalar2=-1e30, op0=ALU.is_equal, op1=ALU.mult),
                         reads=[r_sm], writes=[r_sm])
                    P.op("dve", lambda e: e.tensor_tensor(out=lg2, in0=lg, in1=eq, op=ALU.add), reads=[r_sm], writes=[r_sm])
                    P.op("dve", lambda e: e.tensor_reduce(out=m2, in_=lg2, axis=AX.X, op=ALU.max), reads=[r_sm], writes=[r_sm])
                    P.op("dve", lambda e: e.tensor_scalar(out=sel, in0=lg, scalar1=m2, scalar2=None, op0=ALU.is_ge), reads=[r_sm], writes=[r_sm])
                    P.op("dve", lambda e: e.tensor_scalar(out=nm1, in0=m1, scalar1=-1.0, scalar2=None, op0=ALU.mult), reads=[r_sm], writes=[r_sm])
                    P.op("act", lambda e: e.activation(out=ex, in_=lg, func=AF.Exp, scale=1.0, bias=nm1), reads=[r_sm], writes=[r_sm])
                    P.op("dve", lambda e: e.tensor_tensor(out=ex, in0=ex, in1=sel, op=ALU.mult), reads=[r_sm], writes=[r_sm])
                    P.op("dve", lambda e: e.tensor_reduce(out=den, in_=ex, axis=AX.X, op=ALU.add), reads=[r_sm], writes=[r_sm])
                    P.op("dve", lambda e: e.reciprocal(out=den, in_=den), reads=[r_sm], writes=[r_sm])
                    P.op("dve", lambda e: e.tensor_scalar(out=wg_, in0=ex, scalar1=den, scalar2=None, op0=ALU.mult), reads=[r_sm], writes=[r_sm])
                    P.op("pe", lambda e: e.transpose(bk2[0:8, sb_ * 128:(sb_ + 1) * 128], wg_, ident_f[:]), reads=[r_sm, r_const], writes=[rb2])
                P.op("act", lambda e: e.copy(out=wgT[:, s:s + n], in_=bk2[0:8, 0:n]), reads=[rb2], writes=[r_wgT])
            else:
                norm_block(l, 1, jb, tb, hT, rh, sqb, r_sqb, tmpb, r_tmpb, rstd, r_rstd)
        if "moe" in dbg and moe:
            dw = dbg_tensor("dbg_wgT", [8, LAT], F32)
            P.dma("sp", dw, wgT[:], reads=[r_wgT])
        P.pop_scope()
        NFI = 4
        stg = [P.sb(f"f2_stg{i}", [128, 1024], F32) for i in range(4)]
        r_stg = [Res() for _ in range(4)]
        ctr = [0]
        wgb = [P.sb(f"f2_wgb{i}", [128, 8, 128], BF16) for i in range(3)]
        wub = [P.sb(f"f2_wub{i}", [128, 8, 128], BF16) for i in range(3)]
        r_wgu = [Res(), Res(), Res()]
        wdb = [P.sb(f"f2_wdb{i}", [128, NFI, 1024], BF16) for i in range(2)]
        r_wdb = [Res(), Res()]
        actT = P.sb("f2_act", [128, NFI, NT], BF16)
        r_act = [Res() for _ in TBS]
        sl = [P.sb(f"f2_sl{i}", [128, 512], F32) for i in range(2)]
        r_sl = [Res(), Res()]
        sl2 = [P.sb(f"f2_sl2{i}", [128, 512], F32) for i in range(2)]
        r_sl2 = [Res(), Res()]
        if moe:
            wb = P.sb("f2_wb", [128, LAT], BF16)
            r_wb = Res()
        gi = 0
        ii = 0
        for ex_ in range(NE if moe else 1):
            if moe:
                wgs, wus, wds = moe_wg_d[0, ex_], moe_wu_d[0, ex_], moe_wd_d[0, ex_]
                for tb in tbs:
                    s, n = TBS[tb]
                    bk, rb = nbank()
                    P.op("pe", lambda e: e.matmul(bk[:, 0:n], sel_all[:, ex_, :], wgT[:, s:s + n], start=True, stop=True),
                         reads=[r_sel, r_wgT], writes=[rb])
                    P.op("act", lambda e: e.copy(out=wb[:, s:s + n], in_=bk[:, 0:n]), reads=[rb], writes=[r_wb])
            else:
                wgs, wus, wds = ffn_wg_d[0], ffn_wu_d[0], ffn_wd_d[0]
            wgs_r = wgs.rearrange("(kc p) f -> p kc f", p=128)
            wus_r = wus.rearrange("(kc p) f -> p kc f", p=128)
            for fg in range(FF // 128 // NFI):
                g2 = gi % 2
                gi += 1
                for fi in range(NFI):
                    fc = fg * NFI + fi
                    w2i = ii % 3
                    ii += 1
                    load_cast(wgb[w2i][:], wgs_r[:, :, fc * 128:(fc + 1) * 128], stg, r_stg, ctr, r_wgu[w2i],
                              lambda t: t[:].rearrange("p (a b) -> p a b", a=8))
                    load_cast(wub[w2i][:], wus_r[:, :, fc * 128:(fc + 1) * 128], stg, r_stg, ctr, r_wgu[w2i],
                              lambda t: t[:].rearrange("p (a b) -> p a b", a=8))
                    load_cast(wdb[g2][:, fi, :], wds[fc * 128:(fc + 1) * 128, :], stg, r_stg, ctr, r_wdb[g2], lambda t: t[:])
                    for tb in tbs:
                        s, n = TBS[tb]
                        si = (fi + tb) % 2
                        bkG, rbG = nbank()
                        for kc in range(8):
                            P.op("pe", lambda e, kc=kc: e.matmul(bkG[:, 0:n], wgb[w2i][:, kc, :], hT[:, kc, s:s + n], start=(kc == 0), stop=(kc == 7)),
                                 reads=[r_wgu[w2i], rh[tb]], writes=[rbG])
                        bkU, rbU = nbank()
                        for kc in range(8):
                            P.op("pe", lambda e, kc=kc: e.matmul(bkU[:, 0:n], wub[w2i][:, kc, :], hT[:, kc, s:s + n], start=(kc == 0), stop=(kc == 7)),
                                 reads=[r_wgu[w2i], rh[tb]], writes=[rbU])
                        P.op("act", lambda e: e.activation(out=sl[si][:, 0:n], in_=bkG[:, 0:n], func=AF.Silu), reads=[rbG], writes=[r_sl[si]])
                        if moe:
                            P.op("dve", lambda e: e.tensor_tensor(out=sl2[si][:, 0:n], in0=bkU[:, 0:n], in1=sl[si][:, 0:n], op=ALU.mult),
                                 reads=[rbU, r_sl[si]], writes=[r_sl2[si]])
                            P.op("pool", lambda e: e.tensor_tensor(out=actT[:, fi, s:s + n], in0=sl2[si][:, 0:n], in1=wb[:, s:s + n], op=ALU.mult),
                                 reads=[r_sl2[si], r_wb], writes=[r_act[tb]])
                        else:
                            P.op("dve", lambda e: e.tensor_tensor(out=actT[:, fi, s:s + n], in0=bkU[:, 0:n], in1=sl[si][:, 0:n], op=ALU.mult),
                                 reads=[rbU, r_sl[si]], writes=[r_act[tb]])
                for tb in tbs:
                    s, n = TBS[tb]
                    j = 4 if tb == 4 else jb
                    for dc in range(8):
                        bkO, rbO = nbank()
                        for fi in range(NFI):
                            P.op("pe", lambda e, fi=fi: e.matmul(bkO[:, 0:n], wdb[g2][:, fi, dc * 128:(dc + 1) * 128], actT[:, fi, s:s + n],
                                                                start=(fi == 0), stop=(fi == NFI - 1)), reads=[r_wdb[g2], r_act[tb]], writes=[rbO])
                        gt = mdv[:, l, 5, dc, j:j + 1]
                        P.op("dve", lambda e, gt=gt: e.scalar_tensor_tensor(out=xT[:, dc, s:s + n], in0=bkO[:, 0:n], scalar=gt, in1=xT[:, dc, s:s + n],
                                                                           op0=ALU.mult, op1=ALU.add), reads=[rbO, r_mdv, rx[tb]], writes=[rx[tb]])
        P.pop_scope()

    def phase_final(jb):
        P.push_scope()
        sqb = [P.sb(f"fn_sq{i}", [128, 512], F32) for i in range(2)]
        r_sqb = [Res(), Res()]
        ob = [P.sb(f"fn_o{i}", [128, 512], F32) for i in range(2)]
        r_ob = [Res(), Res()]
        rstd = P.sb("fn_rstd", [128, 512], F32)
        r_rstd = Res()
        for tb in range(4):
            s, n = TBS[tb]
            bk, rb = nbank()
            for c in range(8):
                i = c % 2
                P.op("act", lambda e: e.activation(out=sqb[i][:, 0:n], in_=xT[:, c, s:s + n], func=AF.Square), reads=[rx[tb]], writes=[r_sqb[i]])
                P.op("pe", lambda e: e.matmul(bk[:, 0:n], ones_f[:], sqb[i][:, 0:n], start=(c == 0), stop=(c == 7)),
                     reads=[r_sqb[i], r_const], writes=[rb])
            P.op("act", lambda e: e.activation(out=rstd[:, 0:n], in_=bk[:, 0:n], func=AF.Sqrt, scale=1.0 / D, bias=cst[:, 0:1]),
                 reads=[rb, r_cst], writes=[r_rstd])
            P.op("dve", lambda e: e.reciprocal(out=rstd[:, 0:n], in_=rstd[:, 0:n]), reads=[r_rstd], writes=[r_rstd])
            for c in range(8):
                i = c % 2
                fnc = par[:, 2 * NPL + c:2 * NPL + c + 1]
                P.op("dve", lambda e: e.scalar_tensor_tensor(out=ob[i][:, 0:n], in0=xT[:, c, s:s + n], scalar=fnc, in1=rstd[:, 0:n],
                                                            op0=ALU.mult, op1=ALU.mult), reads=[rx[tb], r_par, r_rstd], writes=[r_ob[i]])
                P.dma("sp", outT_d[jb, c * 128:(c + 1) * 128, s:s + n], ob[i][:, 0:n], reads=[r_ob[i]])
        P.pop_scope()

    RB = 256

    def phase_rwkv(l):
        P.push_scope()
        twT = P.sb("rw_tw", [64, NT], BF16)
        adT = P.sb("rw_ad", [64, NT], BF16)
        gsT = P.sb("rw_gs", [128, NT], BF16)
        r_lora = Res()
        w2b = [P.sb(f"rw_w2b{d}", [64, 512], BF16) for d in range(2)]
        a2b = [P.sb(f"rw_a2b{d}", [64, 512], BF16) for d in range(2)]
        g2b = P.sb("rw_g2b", [128, 512], BF16)
        mk = P.sb("rw_mk", [128, 2560], BF16)
        rmask = P.sb("rw_rmask", [128, 512], F32)
        r_w = Res()
        P.push_scope()
        stg = P.sb("rw_stg", [128, NT], F32)
        r_stg = Res()
        P.dma("sp", stg[0:64, :], PR_d[1536:1600, :], reads=[rPR[12]], writes=[r_stg])
        P.op("act", lambda e: e.activation(out=twT[:], in_=stg[0:64, :], func=AF.Tanh), reads=[r_stg], writes=[r_lora])
        P.dma("sp", stg[0:64, :], PR_d[1600:1664, :], reads=[rPR[12]], writes=[r_stg])
        P.op("act", lambda e: e.copy(out=adT[:], in_=stg[0:64, :]), reads=[r_stg], writes=[r_lora])
        P.dma("sp", stg[:], PR_d[1664:1792, :], reads=[rPR[13]], writes=[r_stg])
        P.op("act", lambda e: e.activation(out=gsT[:], in_=stg[:], func=AF.Sigmoid), reads=[r_stg], writes=[r_lora])
        for d in range(2):
            P.dma("sp", stg[0:64, 0:512], w2_d[l, d], writes=[r_stg])
            P.op("dve", lambda e: e.tensor_copy(out=w2b[d][:], in_=stg[0:64, 0:512]), reads=[r_stg], writes=[r_w])
            P.dma("sp", stg[0:64, 0:512], a2_d[l, d], writes=[r_stg])
            P.op("dve", lambda e: e.tensor_copy(out=a2b[d][:], in_=stg[0:64, 0:512]), reads=[r_stg], writes=[r_w])
        P.dma("sp", stg[:, 0:512], g2_d[l], writes=[r_stg])
        P.op("dve", lambda e: e.tensor_copy(out=g2b[:], in_=stg[:, 0:512]), reads=[r_stg], writes=[r_w])
        for i in range(5):
            P.dma("sp", stg[:, 0:512], mask_d[:, i * 512:(i + 1) * 512], writes=[r_stg])
            P.op("dve", lambda e: e.tensor_copy(out=mk[:, i * 512:(i + 1) * 512], in_=stg[:, 0:512]), reads=[r_stg], writes=[r_w])
        P.dma("sp", rmask[:], rmask_d, writes=[r_w])
        P.pop_scope()

        rT = P.sb("rw_r", [128, NT], F32)
        kT = P.sb("rw_k", [128, NT], F32)
        vT = P.sb("rw_v", [128, NT], F32)
        kkT = P.sb("rw_kk", [128, NT], F32)
        r_rkv = Res()
        r_kk = Res()
        Yacc = P.sb("rw_yacc", [128, NT], F32)
        r_Y = Res()
        yob = [(P.sb(f"rw_yob{i}", [128, RB], BF16), Res()) for i in range(2)]
        Vp = P.sb("rw_vp", [128, 18, 256], BF16)
        r_Vp = Res()
        P.op("pool", lambda e: e.memset(Vp[:], 0.0), writes=[r_Vp])

        def f32t(nm, w=RB):
            return P.sb(nm, [128, w], F32), Res()

        def b16t(nm, w=RB):
            return P.sb(nm, [128, w], BF16), Res()
        sg, r_sg = f32t("rw_sg")
        a_, r_a = f32t("rw_a")
        cs, r_cs_ = f32t("rw_cs")
        s1, r_s1 = f32t("rw_s1")
        s0, r_s0 = f32t("rw_s0")
        e0, r_e0 = f32t("rw_e0")
        e1, r_e1 = f32t("rw_e1")
        e2, r_e2 = f32t("rw_e2")
        e3, r_e3 = f32t("rw_e3")
        tt, r_tt = f32t("rw_tt")
        keys, r_keys = f32t("rw_keys")
        bb, r_bb = f32t("rw_bb")
        BhT, r_BhT = f32t("rw_BhT")
        KhT, r_KhT = f32t("rw_KhT")
        At, r_At = b16t("rw_At")
        Rt, r_Rt = b16t("rw_Rt")
        Bm1, r_Bm1 = b16t("rw_Bm1")
        Bm2, r_Bm2 = b16t("rw_Bm2")
        Km1, r_Km1 = b16t("rw_Km1")
        Km2, r_Km2 = b16t("rw_Km2")
        for t_, r_ in ((Bm1, r_Bm1), (Bm2, r_Bm2), (Km1, r_Km1), (Km2, r_Km2)):
            P.op("pool", lambda e, t_=t_: e.memset(t_[:], 0.0), writes=[r_])
        bsm = P.sb("rw_bsm", [128, 8], F32)
        r_bsm = Res()
        pnd = P.sb("rw_pnd", [128, 2], F32)
        pnm = P.sb("rw_pnm", [128, 2], F32)
        Hbm = [b16t(f"rw_Hbm{i}", 128) for i in range(2)]
        r_pnd = Res()
        NCH = RB // 128
        SC1a = [f32t(f"rw_SC1a_{i}", 256) for i in range(NCH)]
        SC1b = [b16t(f"rw_SC1b_{i}", 256) for i in range(NCH)]
        SC2 = [b16t(f"rw_SC2_{i}", 512) for i in range(NCH)]
        SA = [f32t(f"rw_SA_{i}", 256) for i in range(NCH)]
        TT = [f32t(f"rw_TT_{i}", 256) for i in range(NCH)]
        XX = [[f32t(f"rw_XX_{i}_{j}", 512) for j in range(2)] for i in range(NCH)]
        Bhp = [b16t(f"rw_Bhp_{i}", 256) for i in range(NCH)]
        Khp = [b16t(f"rw_Khp_{i}", 256) for i in range(NCH)]
        RHp = [f32t(f"rw_RHp_{i}", 256) for i in range(2)]
        Up = [b16t(f"rw_Up_{i}", 256) for i in range(2)]
        for lst in (Bhp, Khp, RHp, Up):
            for t_, r_ in lst:
                P.op("pool", lambda e, t_=t_: e.memset(t_[:], 0.0), writes=[r_])
        Hf = P.sb("rw_Hf", [128, 128], F32)
        Hbf = P.sb("rw_Hbf", [128, 128], BF16)
        r_Hf = Res()
        r_Hbf = Res()
        yc, r_yc = s0, r_s0
        sq, r_sq = e0, r_e0
        rsd, r_rsd = e1, r_e1
        aa0, r_aa0 = e2, r_e2
        aa1, r_aa1 = e3, r_e3

        def padcopy(dst_t, dst_off, pstride, src_ap, r_dst, r_src):
            out_ap = bass.AP(dst_t, dst_off, [[pstride, 128], [192, 2], [1, 64]])
            P.op("dve", lambda e: e.tensor_copy(out=out_ap, in_=src_ap.rearrange("p (h j) -> p h j", h=2)), reads=[r_src], writes=[r_dst])

        kkc = lambda j: pcol(l, "kk", j)
        for hp in range(4):
            P.dma("sp", rT[:], PR_d[hp * 128:(hp + 1) * 128, :], reads=[rPR[hp]], writes=[r_rkv])
            P.dma("sp", kT[:], PR_d[512 + hp * 128:512 + (hp + 1) * 128, :], reads=[rPR[4 + hp]], writes=[r_rkv])
            P.dma("sp", vT[:], PR_d[1024 + hp * 128:1024 + (hp + 1) * 128, :], reads=[rPR[8 + hp]], writes=[r_rkv])
            ka = pcol(l, "ka", hp)
            omka = dpar[:, l * 32 + 14 + hp:l * 32 + 15 + hp]
            for bs in range(0, NT, RB):
                n = RB
                P.op("dve", lambda e: e.tensor_scalar(out=tt[:], in0=kT[:, bs:bs + n], scalar1=kkc(hp), scalar2=None, op0=ALU.mult),
                     reads=[r_rkv, r_par], writes=[r_tt])
                P.op("act", lambda e: e.activation(out=sq[:], in_=tt[:], func=AF.Square), reads=[r_tt], writes=[r_sq])
                bk, rb = nbank()
                P.op("pe", lambda e: e.matmul(bk[:, 0:n], onesbd_f[:], sq[:], start=True, stop=True), reads=[r_sq, r_const], writes=[rb])
                P.op("act", lambda e: e.activation(out=rsd[:], in_=bk[:, 0:n], func=AF.Sqrt, scale=1.0, bias=cst[:, 4:5]),
                     reads=[rb, r_cst], writes=[r_rsd])
                P.op("dve", lambda e: e.reciprocal(out=rsd[:], in_=rsd[:]), reads=[r_rsd], writes=[r_rsd])
                P.op("dve", lambda e: e.tensor_tensor(out=kkT[:, bs:bs + n], in0=tt[:], in1=rsd[:], op=ALU.mult),
                     reads=[r_tt, r_rsd], writes=[r_kk])
            for d in range(2):
                P.op("pool", lambda e: e.memset(Hf[:], 0.0), writes=[r_Hf])
                P.op("pool", lambda e: e.memset(Hbf[:], 0.0), writes=[r_Hbf])
                lat = list(range(0, LAT, RB))
                order = [LAT] + (lat if d == 0 else lat[::-1])
                mo = d * 1280
                w0c = pcol(l, f"w0_{d}", hp)
                a0c = pcol(l, f"a0_{d}", hp)
                seqi = 0
                for bs in order:
                    n = RB
                    bk, rb = nbank()
                    P.op("pe", lambda e: e.matmul(bk[:, 0:n], w2b[d][:, hp * 128:(hp + 1) * 128], twT[:, bs:bs + n], start=True, stop=True),
                         reads=[r_w, r_lora], writes=[rb])
                    P.op("act", lambda e: e.activation(out=sg[:], in_=bk[:, 0:n], func=AF.Sigmoid, scale=1.0, bias=w0c),
                         reads=[rb, r_par], writes=[r_sg])
                    bk, rb = nbank()
                    P.op("pe", lambda e: e.matmul(bk[:, 0:n], a2b[d][:, hp * 128:(hp + 1) * 128], adT[:, bs:bs + n], start=True, stop=True),
                         reads=[r_w, r_lora], writes=[rb])
                    P.op("act", lambda e: e.activation(out=a_[:], in_=bk[:, 0:n], func=AF.Sigmoid, scale=1.0, bias=a0c),
                         reads=[rb, r_par], writes=[r_a])
                    P.op("dve", lambda e: e.tensor_tensor_scan(out=cs[:], data0=rmask[:, 0:n], data1=sg[:], initial=0.0, op0=ALU.mult, op1=ALU.add),
                         reads=[r_w, r_sg], writes=[r_cs_])
                    if d == 0:
                        sS, r_sS = cs, r_cs_
                        P.op("dve", lambda e: e.tensor_tensor(out=s0[:], in0=cs[:], in1=sg[:], op=ALU.subtract), reads=[r_cs_, r_sg], writes=[r_s0])
                    else:
                        for ci in range(NCH):
                            tot = cs[:, ci * 128 + 127:ci * 128 + 128]
                            P.op("dve", lambda e: e.tensor_scalar(out=s0[:, ci * 128:(ci + 1) * 128], in0=cs[:, ci * 128:(ci + 1) * 128],
                                                                 scalar1=-1.0, scalar2=tot, op0=ALU.mult, op1=ALU.add),
                                 reads=[r_cs_], writes=[r_s0])
                        P.op("dve", lambda e: e.tensor_tensor(out=s1[:], in0=s0[:], in1=sg[:], op=ALU.add), reads=[r_s0, r_sg], writes=[r_s1])
                        sS, r_sS = s1, r_s1
                    for ci in range(NCH):
                        tot = cs[:, ci * 128 + 127:ci * 128 + 128]
                        for q_, fac in enumerate((LW / 2, -LW / 2, LW)):
                            P.op("dve", lambda e: e.tensor_scalar(out=bsm[:, ci * 4 + q_:ci * 4 + q_ + 1], in0=tot, scalar1=fac, scalar2=None, op0=ALU.mult),
                                 reads=[r_cs_], writes=[r_bsm])
                        P.op("act", lambda e: e.activation(out=pnd[:, ci:ci + 1], in_=tot, func=AF.Exp, scale=LW), reads=[r_cs_], writes=[r_pnd])
                        P.op("act", lambda e: e.activation(out=pnm[:, ci:ci + 1], in_=tot, func=AF.Exp, scale=LW / 2), reads=[r_cs_], writes=[r_pnd])
                        cl = slice(ci * 128, (ci + 1) * 128)
                        mpos, mneg, cb_ = (bsm[:, ci * 4 + q_:ci * 4 + q_ + 1] for q_ in range(3))
                        P.op("act", lambda e: e.activation(out=e1[:, cl], in_=sS[:, cl], func=AF.Exp, scale=LW, bias=mneg), reads=[r_sS, r_bsm], writes=[r_e1])
                        P.op("act", lambda e: e.activation(out=e0[:, cl], in_=s0[:, cl], func=AF.Exp, scale=LW, bias=mneg), reads=[r_s0, r_bsm], writes=[r_e0])
                        P.op("act", lambda e: e.activation(out=e2[:, cl], in_=sS[:, cl], func=AF.Exp, scale=-LW, bias=mpos), reads=[r_sS, r_bsm], writes=[r_e2])
                        P.op("act", lambda e: e.activation(out=e3[:, cl], in_=sS[:, cl], func=AF.Exp, scale=-LW, bias=cb_), reads=[r_sS, r_bsm], writes=[r_e3])
                    P.op("dve", lambda e: e.tensor_scalar(out=tt[:], in0=a_[:], scalar1=ka, scalar2=omka, op0=ALU.mult, op1=ALU.add),
                         reads=[r_a, r_par, r_dpar], writes=[r_tt])
                    P.op("pool", lambda e: e.tensor_tensor(out=keys[:], in0=tt[:], in1=kT[:, bs:bs + n], op=ALU.mult), reads=[r_tt, r_rkv], writes=[r_keys])
                    P.op("pool", lambda e: e.tensor_tensor(out=bb[:], in0=kkT[:, bs:bs + n], in1=a_[:], op=ALU.mult), reads=[r_kk, r_a], writes=[r_bb])
                    P.op("dve", lambda e: e.scalar_tensor_tensor(out=At[:], in0=kkT[:, bs:bs + n], scalar=-1.0, in1=e0[:], op0=ALU.mult, op1=ALU.mult),
                         reads=[r_kk, r_e0], writes=[r_At])
                    P.op("pool", lambda e: e.tensor_tensor(out=Rt[:], in0=rT[:, bs:bs + n], in1=e1[:], op=ALU.mult), reads=[r_rkv, r_e1], writes=[r_Rt])
                    P.op("dve", lambda e: e.tensor_tensor(out=Bm1[0:64, :], in0=bb[0:64, :], in1=e2[0:64, :], op=ALU.mult), reads=[r_bb, r_e2], writes=[r_Bm1])
                    P.op("dve", lambda e: e.tensor_tensor(out=Bm2[64:128, :], in0=bb[64:128, :], in1=e2[64:128, :], op=ALU.mult), reads=[r_bb, r_e2], writes=[r_Bm2])
                    P.op("pool", lambda e: e.tensor_tensor(out=Km1[0:64, :], in0=keys[0:64, :], in1=e2[0:64, :], op=ALU.mult), reads=[r_keys, r_e2], writes=[r_Km1])
                    P.op("pool", lambda e: e.tensor_tensor(out=Km2[64:128, :], in0=keys[64:128, :], in1=e2[64:128, :], op=ALU.mult), reads=[r_keys, r_e2], writes=[r_Km2])
                    P.op("dve", lambda e: e.tensor_tensor(out=BhT[:], in0=bb[:], in1=e3[:], op=ALU.mult), reads=[r_bb, r_e3], writes=[r_BhT])
                    P.op("pool", lambda e: e.tensor_tensor(out=KhT[:], in0=keys[:], in1=e3[:], op=ALU.mult), reads=[r_keys, r_e3], writes=[r_KhT])
                    for ci in range(NCH):
                        cl = slice(ci * 128, (ci + 1) * 128)
                        kc = (bs + ci * 128) // 128
                        bk, rb = nbank()
                        P.op("pe", lambda e: e.transpose(bk[:, 0:128], BhT[:, cl], ident_f[:]), reads=[r_BhT, r_const], writes=[rb])
                        P.op("pe", lambda e: e.transpose(bk[:, 128:256], KhT[:, cl], ident_f[:]), reads=[r_KhT, r_const], writes=[rb])
                        if d == 0:
                            P.op("pe", lambda e: e.transpose(bk[:, 256:384], vT[:, bs + ci * 128:bs + (ci + 1) * 128], ident_f[:]),
                                 reads=[r_rkv, r_const], writes=[rb])
                            padcopy(Vp, kc * 256, 18 * 256, bk[:, 256:384], r_Vp, rb)
                        padcopy(Bhp[ci][0], 0, 256, bk[:, 0:128], Bhp[ci][1], rb)
                        padcopy(Khp[ci][0], 0, 256, bk[:, 128:256], Khp[ci][1], rb)
                        b1, rb1 = nbank()
                        b2, rb2 = nbank()
                        b3, rb3 = nbank()
                        for q_, (lt_, rl_) in enumerate(((Bm1, r_Bm1), (Bm2, r_Bm2), (Km1, r_Km1), (Km2, r_Km2))):
                            P.op("pe", lambda e: e.matmul(b1[:, q_ * 128:(q_ + 1) * 128], lt_[:, cl], At[:, cl], start=True, stop=True),
                                 reads=[rl_, r_At], writes=[rb1])
                            P.op("pe", lambda e: e.matmul(b2[:, q_ * 128:(q_ + 1) * 128], lt_[:, cl], Rt[:, cl], start=True, stop=True),
                                 reads=[rl_, r_Rt], writes=[rb2])
                        P.op("pe", lambda e: e.matmul(b3[:, 0:128], At[:, cl], Bm1[:, cl], start=True, stop=True), reads=[r_At, r_Bm1], writes=[rb3])
                        P.op("pe", lambda e: e.matmul(b3[:, 128:256], At[:, cl], Bm2[:, cl], start=True, stop=True), reads=[r_At, r_Bm2], writes=[rb3])
                        P.op("dve", lambda e: e.tensor_tensor(out=SC1a[ci][0][:], in0=b1[:, 0:256], in1=mk[:, mo:mo + 256], op=ALU.mult),
                             reads=[rb1, r_w], writes=[SC1a[ci][1]])
                        P.op("dve", lambda e: e.tensor_tensor(out=SC1b[ci][0][:], in0=b1[:, 256:512], in1=mk[:, mo + 256:mo + 512], op=ALU.mult),
                             reads=[rb1, r_w], writes=[SC1b[ci][1]])
                        P.op("dve", lambda e: e.tensor_tensor(out=SC2[ci][0][:], in0=b2[:], in1=mk[:, mo + 512:mo + 1024], op=ALU.mult),
                             reads=[rb2, r_w], writes=[SC2[ci][1]])
                        P.op("dve", lambda e: e.tensor_tensor(out=SA[ci][0][:], in0=b3[:, 0:256], in1=mk[:, mo + 1024:mo + 1280], op=ALU.mult),
                             reads=[rb3, r_w], writes=[SA[ci][1]])
                        P.op("pool", lambda e: e.tensor_tensor(out=TT[ci][0][:], in0=SC1a[ci][0][:], in1=ident2_f[:], op=ALU.add),
                             reads=[SC1a[ci][1], r_const], writes=[TT[ci][1]])
                    for j in range(1, 7):
                        for ci in range(NCH):
                            if j == 1:
                                Xs, rXs, XTs, rXTs, xo, xto = SA[ci][0], SA[ci][1], SC1a[ci][0], SC1a[ci][1], 0, 0
                            else:
                                Xs, rXs = XX[ci][j % 2]
                                XTs, rXTs, xo, xto = Xs, rXs, 0, 256
                            Xn, rXn = XX[ci][(j + 1) % 2]
                            bn, rbn = nbank()
                            for h in range(2):
                                hs = slice(h * 128, (h + 1) * 128)
                                Xh = Xs[:, xo + h * 128:xo + (h + 1) * 128]
                                XTh = XTs[:, xto + h * 128:xto + (h + 1) * 128]
                                P.op("pe", lambda e: e.matmul(bn[:, hs], XTh, Xh, start=True, stop=True), reads=[rXs, rXTs], writes=[rbn])
                                if j < 6:
                                    P.op("pe", lambda e: e.matmul(bn[:, 256 + h * 128:256 + (h + 1) * 128], Xh, XTh, start=True, stop=True),
                                         reads=[rXs, rXTs], writes=[rbn])
                            w_ = 512 if j < 6 else 256
                            P.op("act", lambda e: e.copy(out=Xn[:, 0:w_], in_=bn[:, 0:w_]), reads=[rbn], writes=[rXn])
                        for jj in ([j - 1] if j >= 2 else []) + ([6] if j == 6 else []):
                            for ci in range(NCH):
                                Xp, rXp = XX[ci][(jj + 1) % 2]
                                bt, rbt = nbank()
                                for h in range(2):
                                    hs = slice(h * 128, (h + 1) * 128)
                                    P.op("pe", lambda e: e.matmul(bt[:, hs], Xp[:, hs], TT[ci][0][:, hs], start=True, stop=True),
                                         reads=[rXp, TT[ci][1]], writes=[rbt])
                                P.op("dve", lambda e: e.tensor_tensor(out=TT[ci][0][:], in0=bt[:, 0:256], in1=TT[ci][0][:], op=ALU.add),
                                     reads=[rbt, TT[ci][1]], writes=[TT[ci][1]])
                    cis = list(range(NCH)) if d == 0 else list(range(NCH))[::-1]
                    for ci in cis:
                        cl = slice(ci * 128, (ci + 1) * 128)
                        c0 = bs + ci * 128
                        kc = c0 // 128
                        RH, rRH = RHp[seqi % 2]
                        U_, rU = Up[seqi % 2]
                        seqi += 1
                        Hm, rHm = Hbm[seqi % 2]
                        P.op("dve", lambda e: e.tensor_scalar(out=Hm[:], in0=Hf[:], scalar1=pnm[:, ci:ci + 1], scalar2=None, op0=ALU.mult),
                             reads=[r_Hf, r_pnd], writes=[rHm])
                        q1, rq1 = nbank()
                        P.op("pe", lambda e: e.matmul(q1[:, 0:128], At[:, cl], Hm[:], start=True, stop=False), reads=[r_At, rHm], writes=[rq1])
                        P.op("pe", lambda e: e.matmul(q1[:, 0:128], SC1b[ci][0][:, 0:128], Vp[:, kc, 0:128], start=False, stop=False),
                             reads=[SC1b[ci][1], r_Vp], writes=[rq1])
                        P.op("pe", lambda e: e.matmul(q1[:, 0:128], SC1b[ci][0][:, 128:256], Vp[:, kc, 128:256], start=False, stop=True),
                             reads=[SC1b[ci][1], r_Vp], writes=[rq1])
                        padcopy(RH, 0, 256, q1[:, 0:128], rRH, rq1)
                        q2, rq2 = nbank()
                        P.op("pe", lambda e: e.matmul(q2[:, 0:128], TT[ci][0][:, 0:128], RH[:, 0:128], start=True, stop=False),
                             reads=[TT[ci][1], rRH], writes=[rq2])
                        P.op("pe", lambda e: e.matmul(q2[:, 0:128], TT[ci][0][:, 128:256], RH[:, 128:256], start=False, stop=True),
                             reads=[TT[ci][1], rRH], writes=[rq2])
                        padcopy(U_, 0, 256, q2[:, 0:128], rU, rq2)
                        q4, rq4 = nbank()
                        P.op("pe", lambda e: e.matmul(q4[:, 0:128], Hm[:], Rt[:, cl], start=True, stop=False), reads=[rHm, r_Rt], writes=[rq4])
                        P.op("pe", lambda e: e.matmul(q4[:, 0:128], U_[:, 0:128], SC2[ci][0][:, 0:128], start=False, stop=False),
                             reads=[rU, SC2[ci][1]], writes=[rq4])
                        P.op("pe", lambda e: e.matmul(q4[:, 0:128], U_[:, 128:256], SC2[ci][0][:, 128:256], start=False, stop=False),
                             reads=[rU, SC2[ci][1]], writes=[rq4])
                        P.op("pe", lambda e: e.matmul(q4[:, 0:128], Vp[:, kc, 0:128], SC2[ci][0][:, 256:384], start=False, stop=False),
                             reads=[r_Vp, SC2[ci][1]], writes=[rq4])
                        P.op("pe", lambda e: e.matmul(q4[:, 0:128], Vp[:, kc, 128:256], SC2[ci][0][:, 384:512], start=False, stop=True),
                             reads=[r_Vp, SC2[ci][1]], writes=[rq4])
                        if d == 0:
                            P.op("act", lambda e: e.copy(out=Yacc[:, c0:c0 + 128], in_=q4[:, 0:128]), reads=[rq4], writes=[r_Y])
                        else:
                            P.op("dve", lambda e: e.tensor_tensor(out=Yacc[:, c0:c0 + 128], in0=q4[:, 0:128], in1=Yacc[:, c0:c0 + 128], op=ALU.add),
                                 reads=[rq4, r_Y], writes=[r_Y])
                        q3, rq3 = nbank()
                        P.op("pe", lambda e: e.matmul(q3[:, 0:128], Bhp[ci][0][:, 0:128], U_[:, 0:128], start=True, stop=False),
                             reads=[Bhp[ci][1], rU], writes=[rq3])
                        P.op("pe", lambda e: e.matmul(q3[:, 0:128], Bhp[ci][0][:, 128:256], U_[:, 128:256], start=False, stop=False),
                             reads=[Bhp[ci][1], rU], writes=[rq3])
                        P.op("pe", lambda e: e.matmul(q3[:, 0:128], Khp[ci][0][:, 0:128], Vp[:, kc, 0:128], start=False, stop=False),
                             reads=[Khp[ci][1], r_Vp], writes=[rq3])
                        P.op("pe", lambda e: e.matmul(q3[:, 0:128], Khp[ci][0][:, 128:256], Vp[:, kc, 128:256], start=False, stop=True),
                             reads=[Khp[ci][1], r_Vp], writes=[rq3])
                        P.op("dve", lambda e: e.scalar_tensor_tensor(out=Hf[:], in0=Hf[:], scalar=pnd[:, ci:ci + 1], in1=q3[:, 0:128],
                                                                    op0=ALU.mult, op1=ALU.add), reads=[r_Hf, r_pnd, rq3], writes=[r_Hf])
            if "rwkv" in dbg and l == 0:
                dy = dbg_out["dbg_ysum"] if "dbg_ysum" in dbg_out else dbg_tensor("dbg_ysum", [512, NT], F32)
                P.dma("sp", dy[hp * 128:(hp + 1) * 128, :], Yacc[:], reads=[r_Y])
            lw_, lb_, rk_ = pcol(l, "lnxw", hp), pcol(l, "lnxb", hp), pcol(l, "rk", hp)
            for bs in range(0, NT, RB):
                n = RB
                bk, rb = nbank()
                P.op("pe", lambda e: e.matmul(bk[:, 0:n], onesbd_f[:], Yacc[:, bs:bs + n], start=True, stop=True), reads=[r_Y, r_const], writes=[rb])
                P.op("dve", lambda e: e.scalar_tensor_tensor(out=yc[:], in0=bk[:, 0:n], scalar=-1.0 / 64, in1=Yacc[:, bs:bs + n],
                                                            op0=ALU.mult, op1=ALU.add), reads=[rb, r_Y], writes=[r_yc])
                P.op("act", lambda e: e.activation(out=sq[:], in_=yc[:], func=AF.Square), reads=[r_yc], writes=[r_sq])
                bk, rb = nbank()
                P.op("pe", lambda e: e.matmul(bk[:, 0:n], onesbd_f[:], sq[:], start=True, stop=True), reads=[r_sq, r_const], writes=[rb])
                P.op("act", lambda e: e.activation(out=rsd[:], in_=bk[:, 0:n], func=AF.Sqrt, scale=1.0 / 64, bias=cst[:, 1:2]),
                     reads=[rb, r_cst], writes=[r_rsd])
                P.op("dve", lambda e: e.reciprocal(out=rsd[:], in_=rsd[:]), reads=[r_rsd], writes=[r_rsd])
                P.op("dve", lambda e: e.tensor_tensor(out=yc[:], in0=yc[:], in1=rsd[:], op=ALU.mult), reads=[r_yc, r_rsd], writes=[r_yc])
                P.op("act", lambda e: e.activation(out=yc[:], in_=yc[:], func=AF.Identity, scale=lw_, bias=lb_), reads=[r_yc, r_par], writes=[r_yc])
                for d, (aa, r_aa) in enumerate(((aa0, r_aa0), (aa1, r_aa1))):
                    bk, rb = nbank()
                    P.op("pe", lambda e: e.matmul(bk[:, 0:n], a2b[d][:, hp * 128:(hp + 1) * 128], adT[:, bs:bs + n], start=True, stop=True),
                         reads=[r_w, r_lora], writes=[rb])
                    P.op("act", lambda e: e.activation(out=aa[:], in_=bk[:, 0:n], func=AF.Sigmoid, scale=1.0, bias=pcol(l, f"a0_{d}", hp)),
                         reads=[rb, r_par], writes=[r_aa])
                P.op("pool", lambda e: e.tensor_tensor(out=aa0[:], in0=aa0[:], in1=aa1[:], op=ALU.add), reads=[r_aa0, r_aa1], writes=[r_aa0])
                P.op("dve", lambda e: e.tensor_scalar(out=tt[:], in0=aa0[:], scalar1=0.5, scalar2=ka, op0=ALU.mult, op1=ALU.mult),
                     reads=[r_aa0, r_par], writes=[r_tt])
                P.op("dve", lambda e: e.tensor_scalar(out=tt[:], in0=tt[:], scalar1=omka, scalar2=None, op0=ALU.add), reads=[r_tt, r_dpar], writes=[r_tt])
                P.op("pool", lambda e: e.tensor_tensor(out=keys[:], in0=tt[:], in1=kT[:, bs:bs + n], op=ALU.mult), reads=[r_tt, r_rkv], writes=[r_keys])
                P.op("dve", lambda e: e.scalar_tensor_tensor(out=bb[:], in0=rT[:, bs:bs + n], scalar=rk_, in1=keys[:], op0=ALU.mult, op1=ALU.mult),
                     reads=[r_rkv, r_par, r_keys], writes=[r_bb])
                bk, rb = nbank()
                P.op("pe", lambda e: e.matmul(bk[:, 0:n], onesbd_f[:], bb[:], start=True, stop=True), reads=[r_bb, r_const], writes=[rb])
                P.op("dve", lambda e: e.tensor_tensor(out=sq[:], in0=bk[:, 0:n], in1=vT[:, bs:bs + n], op=ALU.mult), reads=[rb, r_rkv], writes=[r_sq])
                P.op("pool", lambda e: e.tensor_tensor(out=sq[:], in0=sq[:], in1=yc[:], op=ALU.add), reads=[r_sq, r_yc], writes=[r_sq])
                bk, rb = nbank()
                P.op("pe", lambda e: e.matmul(bk[:, 0:n], g2b[:, hp * 128:(hp + 1) * 128], gsT[:, bs:bs + n], start=True, stop=True),
                     reads=[r_w, r_lora], writes=[rb])
                yo_, r_yo_ = yob[(bs // RB) % 2]
                P.op("dve", lambda e: e.tensor_tensor(out=yo_[:], in0=bk[:, 0:n], in1=sq[:], op=ALU.mult), reads=[rb, r_sq], writes=[r_yo_])
                P.dma("sp", YR_d[hp * 128:(hp + 1) * 128, bs:bs + n], yo_[:], reads=[r_yo_], writes=[rYR[hp]])
        P.pop_scope()

    def load_x(jb):
        for c in range(8):
            P.dma("sp", xT[:, c, 0:LAT], xT_d[jb, c * 128:(c + 1) * 128, :], writes=[rx[0], rx[1], rx[2], rx[3]])
            P.dma("sp", xT[:, c, LAT:NT], cxT_d[jb, c * 128:(c + 1) * 128, :], writes=[rx[4]])

    def dump(name, src, shape, dtype, rs):
        d = dbg_tensor(name, shape, dtype)
        P.dma("sp", d, src, reads=rs)

    prologue()
    if "mod" in dbg:
        d = dbg_tensor("dbg_mod", [128, 2 * 48 * 5], F32)
        P.dma("sp", d, modT[:].rearrange("p a b c -> p (a b c)"), reads=[r_mod])
    for jb in range(nb):
        load_x(jb)
        for l in range(nlayers):
            last = l == nlayers - 1
            tbs = [0, 1, 2, 3, 4]
            phase_proj(l, jb, tbs)
            if stop_after == "proj":
                dump("dbg_PR", PR_d, [RC, NT], F32, rPR)
                dump("dbg_PA", PA_d, [768, NT], F32, rPA)
                dump("dbg_PG", PG_d, [2048, NT], BF16, rPG)
                break
            if "noattn" not in dbg:
                phase_attn(l, not last)
            if "norwkv" not in dbg:
                phase_rwkv(l)
            if stop_after == "mix" or ("mix" in dbg and l == 0 and jb == 0):
                dump(f"dbg_YA", YA_d, [512, NT], BF16, rYA)
                dump(f"dbg_YR", YR_d, [512, NT], BF16, rYR)
                if stop_after == "mix":
                    break
            mtbs = tbs if not last else [0, 1, 2, 3]
            phase_merge(l, jb, mtbs)
            if "x" in dbg and jb == 0:
                d_ = dbg_tensor(f"dbg_xmix{l}", [128, 8 * NT], F32)
                P.dma("sp", d_, xT[:].rearrange("p c t -> p (c t)"), reads=rx)
            phase_ffn(l, jb, mtbs, moe=(l % 2 == 1))
            if "x" in dbg and jb == 0:
                d_ = dbg_tensor(f"dbg_xffn{l}", [128, 8 * NT], F32)
                P.dma("sp", d_, xT[:].rearrange("p c t -> p (c t)"), reads=rx)
            if stop_after == f"l{l}":
                break
        else:
            phase_final(jb)
        if stop_after is not None:
            break
    P.barrier()
    ninstr = P.ninstr
    P.close()
    return nc, dbg_out, ninstr


def make_inmaps(inp, ncores=8, nb=4):
    consts = host_consts()
    params = host_params(inp)
    xT = np.ascontiguousarray(np.transpose(inp["x"], (0, 2, 1)))
    cxT = np.ascontiguousarray(np.transpose(inp["ctx"], (0, 2, 1)))
    maps = []
    for i in range(ncores):
        m = {}
        m["xT"] = xT[i * nb:(i + 1) * nb]
        m["cxT"] = cxT[i * nb:(i + 1) * nb]
        c5 = np.concatenate([inp["c"][i * nb:(i + 1) * nb], np.zeros((4 - nb, D), np.float32), inp["c_ctx"][None, :]], axis=0)
        m["cT"] = np.ascontiguousarray(c5.reshape(5, 8, 128).transpose(2, 1, 0))
        m["params"] = params
        m.update(consts)
        for k in ("ada_w", "w_in", "rwkv_w2", "rwkv_a2", "rwkv_g2", "w_pa", "w_pb", "w_o", "ffn_wg", "ffn_wu", "ffn_wd",
                  "router", "moe_wg", "moe_wu", "moe_wd"):
            m[k] = inp[k]
        maps.append(m)
    return maps


def kernel(**inputs):
    inp = {k: np.asarray(v) for k, v in inputs.items()}
    ncores, nb = 8, 4
    nc, _, _ = build(nb=nb, nlayers=2)
    maps = make_inmaps(inp, ncores=ncores, nb=nb)
    res = run_bass_kernel_spmd(nc, maps, core_ids=list(range(ncores)))
    outT = np.concatenate([np.asarray(r["outT"]) for r in res.results], axis=0)
    return np.ascontiguousarray(np.transpose(outT, (0, 2, 1))).astype(np.float32)
```

```python
import contextlib
import numpy as np
import concourse.bass as bass
import concourse.mybir as mybir
from concourse.bass_utils import run_bass_kernel_spmd

F32 = mybir.dt.float32
BF16 = mybir.dt.bfloat16
AF = mybir.ActivationFunctionType
ALU = mybir.AluOpType
AX = mybir.AxisListType

ENGS = ("pe", "act", "dve", "pool", "sp")
EPOCH = 30000
NDMASEM = 48

D = 1024
LAT = 2048
CTX = 256
NT = LAT + CTX
NIN = 4608
RC = 1792
FF = 3584
NE = 8
LW = -0.6065306597126334
TBS = [(0, 512), (512, 512), (1024, 512), (1536, 512), (2048, 256)]
NPL = 130


class Res:
    __slots__ = ("last_w", "readers")

    def __init__(self):
        self.last_w = None
        self.readers = []


class Prog:
    def __init__(self, nc):
        self.nc = nc
        self.es = contextlib.ExitStack()
        self.seq = {e: 0 for e in ENGS}
        self.sems = []
        self.engsem = {}
        self.known = {e: {} for e in ENGS}
        self.dmasem = []
        self.ndma = 0
        self.dma_last = {}
        self.ninstr = 0
        self.eobj = {"pe": nc.tensor, "act": nc.scalar, "dve": nc.vector, "pool": nc.gpsimd, "sp": nc.sync}
        self.scope = [self.es]

    def new_sem(self, name):
        h = self.es.enter_context(self.nc.semaphore(name))
        self.sems.append(h)
        return len(self.sems) - 1

    def push_scope(self):
        st = contextlib.ExitStack()
        self.scope.append(st)

    def pop_scope(self):
        self.barrier()
        self.scope.pop().close()

    def sb(self, name, shape, dt):
        self.ntile = getattr(self, "ntile", 0) + 1
        return self.scope[-1].enter_context(self.nc.sbuf_tensor(f"{name}_{self.ntile}", list(shape), dt))

    def ps(self, name, shape, dt):
        return self.es.enter_context(self.nc.psum_tensor(name, list(shape), dt))

    def _waits_for(self, eng, reads, writes):
        need = {}
        known = self.known[eng]

        def add(key):
            if key is None:
                return
            s, v, ke = key
            if ke == eng and eng in ("pe", "sp"):
                return
            if known.get(s, 0) >= v:
                return
            if need.get(s, 0) < v:
                need[s] = v

        for r in reads:
            add(r.last_w)
        for w in writes:
            add(w.last_w)
            for k in w.readers:
                add(k)
        for s, v in need.items():
            known[s] = v
        return list(need.items())

    def _emit(self, eng, waits, fn, s, inc):
        e = self.eobj[eng]
        for ws, wv in waits:
            e.wait_ge(self.sems[ws], wv)
        if fn is not None:
            fn(e).then_inc(self.sems[s], inc)

    def op(self, eng, fn, reads=(), writes=()):
        waits = self._waits_for(eng, reads, writes)
        self.seq[eng] += 1
        n = self.seq[eng]
        ep = (n - 1) // EPOCH
        if (eng, ep) not in self.engsem:
            self.engsem[(eng, ep)] = self.new_sem(f"s_{eng}_{ep}")
        s = self.engsem[(eng, ep)]
        v = (n - 1) % EPOCH + 1
        key = (s, v, eng)
        self._emit(eng, waits, fn, s, 1)
        for r in reads:
            r.readers.append(key)
        for w in writes:
            w.last_w = key
            w.readers = []
        self.ninstr += 1
        return key

    def dma(self, eng, out_ap, in_ap, reads=(), writes=()):
        if not self.dmasem:
            self.dmasem = [self.new_sem(f"s_dma_{i}") for i in range(NDMASEM)]
        j = self.ndma
        self.ndma += 1
        slot = j % NDMASEM
        s = self.dmasem[slot]
        v = 16 * (j // NDMASEM + 1)
        waits = self._waits_for(eng, reads, writes)
        prev = self.dma_last.get(slot)
        if prev is not None and self.known[eng].get(prev[0], 0) < prev[1]:
            d = dict(waits)
            d[prev[0]] = max(d.get(prev[0], 0), prev[1])
            self.known[eng][prev[0]] = prev[1]
            waits = list(d.items())
        key = (s, v, "dma")
        self.dma_last[slot] = key
        self._emit(eng, waits, lambda e: e.dma_start(out=out_ap, in_=in_ap), s, 16)
        for r in reads:
            r.readers.append(key)
        for w in writes:
            w.last_w = key
            w.readers = []
        self.ninstr += 1
        return key

    def barrier(self):
        keys = []
        for (eng, ep), s in self.engsem.items():
            if self.seq[eng] > 0 and ep == (self.seq[eng] - 1) // EPOCH:
                keys.append((s, (self.seq[eng] - 1) % EPOCH + 1))
        for slot, k in self.dma_last.items():
            keys.append((k[0], k[1]))
        for eng in ENGS:
            for s, v in keys:
                if self.known[eng].get(s, 0) < v:
                    self.known[eng][s] = v
                    self.eobj[eng].wait_ge(self.sems[s], v)

    def close(self):
        while len(self.scope) > 1:
            self.scope.pop().close()
        self.es.close()


def _colz(v):
    return np.ascontiguousarray(np.asarray(v, np.float32).reshape(-1, 128).T)


def host_consts():
    c = {}
    c["ident"] = np.eye(128, dtype=np.float32)
    bd = np.zeros((128, 128), np.float32)
    bd[:64, :64] = 1.0
    bd[64:, 64:] = 1.0
    c["onesbd"] = bd
    p = np.arange(128)[:, None]
    f = np.arange(128)[None, :]
    lt = (p < f).astype(np.float32)
    le = (p <= f).astype(np.float32)
    gt = (p > f).astype(np.float32)
    ge = (p >= f).astype(np.float32)
    c["mask"] = np.concatenate([
        np.tile(lt, (1, 4)), np.tile(le, (1, 4)), np.tile(gt, (1, 2)),
        np.tile(gt, (1, 4)), np.tile(ge, (1, 4)), np.tile(lt, (1, 2)),
    ], axis=1).astype(np.float32)
    R = np.zeros((128, 128), np.float32)
    for i in range(64):
        R[2 * i + 1, 2 * i] = -1.0
        R[2 * i, 2 * i + 1] = 1.0
    c["rot"] = R
    t = np.arange(LAT)
    row = (t // 64).astype(np.float32)
    col = (t % 64).astype(np.float32)
    inv = (10000.0 ** (-np.arange(0, 32, 2, dtype=np.float32) / 32.0)).astype(np.float32)
    ang = np.concatenate([row[:, None] * inv[None, :], col[:, None] * inv[None, :]], axis=1).astype(np.float32)
    cos = np.cos(ang).astype(np.float32)
    sin = np.sin(ang).astype(np.float32)
    cosf = np.repeat(cos, 2, axis=1).T
    sinf = np.repeat(sin, 2, axis=1).T
    c["cos"] = np.ascontiguousarray(np.concatenate([cosf, cosf], axis=0))
    c["sin"] = np.ascontiguousarray(np.concatenate([sinf, sinf], axis=0))
    rm = np.ones((128, 512), np.float32)
    rm[:, ::128] = 0.0
    c["rmask"] = rm
    return c


def host_params(inp):
    cols = []
    for l in range(2):
        cols += [_colz(inp["norm1"][l]), _colz(inp["norm2"][l]), _colz(inp["ada_b"][l]),
                 _colz(inp["shift_mu"][l, 0]), _colz(inp["shift_mu"][l, 1]),
                 _colz(inp["rwkv_w0"][l, 0]), _colz(inp["rwkv_w0"][l, 1]),
                 _colz(inp["rwkv_a0"][l, 0]), _colz(inp["rwkv_a0"][l, 1]),
                 _colz(inp["rwkv_kk"][l]), _colz(inp["rwkv_ka"][l]), _colz(inp["rwkv_rk"][l]),
                 _colz(inp["lnx_w"][l]), _colz(inp["lnx_b"][l]),
                 _colz(np.tile(inp["q_norm"][l], 2)), _colz(np.tile(inp["k_norm"][l], 2))]
    cols.append(_colz(inp["final_norm"]))
    return np.ascontiguousarray(np.concatenate(cols, axis=1))


PO = {}
_o = 0
for _n, _w in [("norm1", 8), ("norm2", 8), ("adab", 48), ("mu0", 14), ("mu1", 14), ("w0_0", 4), ("w0_1", 4),
               ("a0_0", 4), ("a0_1", 4), ("kk", 4), ("ka", 4), ("rk", 4), ("lnxw", 4), ("lnxb", 4), ("qn", 1), ("kn", 1)]:
    PO[_n] = _o
    _o += _w
assert _o == NPL


def build(nb=4, nlayers=2, dbg=None, stop_after=None):
    dbg = dbg or set()
    nc = bass.Bass("TRN2", target_bir_lowering=False)
    P = Prog(nc)
    dt = nc.dram_tensor

    def din(name, shape, dtype=F32):
        return dt(name, list(shape), dtype, kind="ExternalInput").ap()

    def dscr(name, shape, dtype):
        return dt(name, list(shape), dtype, kind="Internal").ap()

    xT_d = din("xT", [nb, D, LAT])
    cxT_d = din("cxT", [nb, D, CTX])
    cT_d = din("cT", [128, 8, 5])
    par_d = din("params", [128, 2 * NPL + 8])
    ident_d = din("ident", [128, 128])
    onesbd_d = din("onesbd", [128, 128])
    mask_d = din("mask", [128, 2560])
    rot_d = din("rot", [128, 128])
    cos_d = din("cos", [128, LAT])
    sin_d = din("sin", [128, LAT])
    rmask_d = din("rmask", [128, 512])
    ada_w_d = din("ada_w", [2, D, 6 * D])
    w_in_d = din("w_in", [2, D, NIN])
    w2_d = din("rwkv_w2", [2, 2, 64, 512])
    a2_d = din("rwkv_a2", [2, 2, 64, 512])
    g2_d = din("rwkv_g2", [2, 128, 512])
    w_pa_d = din("w_pa", [2, 512, D])
    w_pb_d = din("w_pb", [2, 512, D])
    w_o_d = din("w_o", [2, D, D])
    ffn_wg_d = din("ffn_wg", [1, D, FF])
    ffn_wu_d = din("ffn_wu", [1, D, FF])
    ffn_wd_d = din("ffn_wd", [1, FF, D])
    router_d = din("router", [1, D, NE])
    if nlayers > 1:
        moe_wg_d = din("moe_wg", [1, NE, D, FF])
        moe_wu_d = din("moe_wu", [1, NE, D, FF])
        moe_wd_d = din("moe_wd", [1, NE, FF, D])
    outT_d = dt("outT", [nb, D, LAT], F32, kind="ExternalOutput").ap()

    PR_d = dscr("PR", [RC, NT], F32)
    PA_d = dscr("PA", [768, NT], F32)
    PG_d = dscr("PG", [2048, NT], BF16)
    YR_d = dscr("YR", [512, NT], BF16)
    YA_d = dscr("YA", [512, NT], BF16)
    rPR = [Res() for _ in range(14)]
    rPA = [Res() for _ in range(6)]
    rPG = [Res() for _ in range(16)]
    rYR = [Res() for _ in range(4)]
    rYA = [Res() for _ in range(4)]
    dbg_out = {}

    def dbg_tensor(name, shape, dtype=F32):
        dbg_out[name] = dt(name, list(shape), dtype, kind="ExternalOutput").ap()
        return dbg_out[name]

    xT = P.sb("xT_sb", [128, 8, NT], F32)
    rx = [Res() for _ in TBS]
    par = P.sb("par", [128, 2 * NPL + 8], F32)
    r_par = Res()
    dpar = P.sb("dpar", [128, 64], F32)
    r_dpar = Res()
    cst = P.sb("cst", [128, 8], F32)
    r_cst = Res()
    modT = P.sb("modT", [128, 2, 48, 5], F32)
    r_mod = Res()
    mdv = P.sb("mdv", [128, 2, 6, 8, 5], F32)
    r_mdv = Res()
    ident_f = P.sb("ident_f", [128, 128], F32)
    ident_b = P.sb("ident_b", [128, 128], BF16)
    ident2_f = P.sb("ident2_f", [128, 256], F32)
    onesbd_f = P.sb("onesbd_f", [128, 128], F32)
    onesbd_b = P.sb("onesbd_b", [128, 128], BF16)
    ones_f = P.sb("ones_f", [128, 128], F32)
    rot_b = P.sb("rot_b", [128, 128], BF16)
    r_const = Res()

    banks = [P.ps(f"bank{i}", [128, 512], F32) for i in range(8)]
    rbank = [Res() for _ in range(8)]
    bank_ctr = [0]

    def nbank(lo=0, hi=8):
        i = lo + bank_ctr[0] % (hi - lo)
        bank_ctr[0] += 1
        return banks[i], rbank[i]

    def pcol(l, name, j=0):
        o = l * NPL + PO[name] + j
        return par[:, o:o + 1]

    def prologue():
        P.push_scope()
        stg = P.sb("pl_stg", [128, 128], F32)
        r_stg = Res()
        P.dma("sp", par[:], par_d, writes=[r_par])
        P.dma("sp", ident_f[:], ident_d, writes=[r_const])
        P.dma("sp", onesbd_f[:], onesbd_d, writes=[r_const])
        P.dma("sp", stg[:], rot_d, writes=[r_stg])
        P.op("dve", lambda e: e.tensor_copy(out=rot_b[:], in_=stg[:]), reads=[r_stg], writes=[r_const])
        P.op("dve", lambda e: e.tensor_copy(out=ident_b[:], in_=ident_f[:]), reads=[r_const], writes=[r_const])
        P.op("dve", lambda e: e.tensor_copy(out=ident2_f[:, 0:128], in_=ident_f[:]), reads=[r_const], writes=[r_const])
        P.op("dve", lambda e: e.tensor_copy(out=ident2_f[:, 128:256], in_=ident_f[:]), reads=[r_const], writes=[r_const])
        P.op("dve", lambda e: e.tensor_copy(out=onesbd_b[:], in_=onesbd_f[:]), reads=[r_const], writes=[r_const])
        P.op("dve", lambda e: e.memset(ones_f[:], 1.0), writes=[r_const])
        P.op("dve", lambda e: e.memset(cst[:, 0:1], 1e-6), writes=[r_cst])
        P.op("dve", lambda e: e.memset(cst[:, 1:2], 64e-5), writes=[r_cst])
        P.op("dve", lambda e: e.memset(cst[:, 2:3], 1.0), writes=[r_cst])
        P.op("dve", lambda e: e.memset(cst[:, 3:4], 0.0), writes=[r_cst])
        P.op("dve", lambda e: e.memset(cst[:, 4:5], 1e-24), writes=[r_cst])
        for l in range(2):
            b0 = l * 32
            m0 = par[:, l * NPL + PO["mu0"]: l * NPL + PO["mu0"] + 14]
            m1 = par[:, l * NPL + PO["mu1"]: l * NPL + PO["mu1"] + 14]
            P.op("dve", lambda e, m0=m0, m1=m1, b0=b0: e.tensor_tensor(out=dpar[:, b0:b0 + 14], in0=m0, in1=m1, op=ALU.add),
                 reads=[r_par], writes=[r_dpar])
            P.op("dve", lambda e, b0=b0: e.tensor_scalar(out=dpar[:, b0:b0 + 14], in0=dpar[:, b0:b0 + 14], scalar1=-1.0, scalar2=1.0,
                                                        op0=ALU.mult, op1=ALU.add), reads=[r_dpar], writes=[r_dpar])
            ka = par[:, l * NPL + PO["ka"]: l * NPL + PO["ka"] + 4]
            P.op("dve", lambda e, ka=ka, b0=b0: e.tensor_scalar(out=dpar[:, b0 + 14:b0 + 18], in0=ka, scalar1=-1.0, scalar2=1.0,
                                                               op0=ALU.mult, op1=ALU.add), reads=[r_par], writes=[r_dpar])
            qn = pcol(l, "qn")
            P.op("dve", lambda e, qn=qn, b0=b0: e.tensor_scalar(out=dpar[:, b0 + 18:b0 + 19], in0=qn, scalar1=0.125, scalar2=None,
                                                               op0=ALU.mult), reads=[r_par], writes=[r_dpar])
        cact = P.sb("pl_cact", [128, 8, 5], F32)
        r_cact = Res()
        P.dma("sp", cact[:], cT_d, writes=[r_cact])
        P.op("act", lambda e: e.activation(out=cact[:], in_=cact[:], func=AF.Silu), reads=[r_cact], writes=[r_cact])
        aw = [P.sb(f"pl_aw{i}", [128, 8, 512], F32) for i in range(2)]
        r_aw = [Res(), Res()]
        for l in range(nlayers):
            for cb in range(12):
                i = cb % 2
                src = ada_w_d[l].rearrange("(kc p) n -> p kc n", p=128)[:, :, cb * 512:(cb + 1) * 512]
                P.dma("sp", aw[i][:], src, writes=[r_aw[i]])
                for cc in range(4):
                    ch = cb * 4 + cc
                    bk, rb = nbank()
                    for kc in range(8):
                        P.op("pe", lambda e, bk=bk, i=i, kc=kc, cc=cc: e.matmul(
                            bk[:, 0:5], aw[i][:, kc, cc * 128:(cc + 1) * 128], cact[:, kc, :], start=(kc == 0), stop=(kc == 7)),
                            reads=[r_aw[i], r_cact], writes=[rb])
                    bcol = pcol(l, "adab", ch)
                    P.op("dve", lambda e, bk=bk, l=l, ch=ch, bcol=bcol: e.tensor_scalar(
                        out=modT[:, l, ch, :], in0=bk[:, 0:5], scalar1=bcol, scalar2=None, op0=ALU.add),
                        reads=[rb, r_par], writes=[r_mod])
            for half, nm in ((0, "norm1"), (1, "norm2")):
                base = half * 24
                for c in range(8):
                    nrm = pcol(l, nm, c)
                    P.op("dve", lambda e, l=l, half=half, c=c, base=base, nrm=nrm: e.tensor_scalar(
                        out=mdv[:, l, half * 3 + 0, c, :], in0=modT[:, l, base + 8 + c, :], scalar1=1.0, scalar2=nrm,
                        op0=ALU.add, op1=ALU.mult), reads=[r_mod, r_par], writes=[r_mdv])
                    P.op("dve", lambda e, l=l, half=half, c=c, base=base: e.tensor_copy(
                        out=mdv[:, l, half * 3 + 1, c, :], in_=modT[:, l, base + c, :]), reads=[r_mod], writes=[r_mdv])
                    P.op("dve", lambda e, l=l, half=half, c=c, base=base: e.tensor_copy(
                        out=mdv[:, l, half * 3 + 2, c, :], in_=modT[:, l, base + 16 + c, :]), reads=[r_mod], writes=[r_mdv])
        P.pop_scope()

    def norm_block(l, half, jb, tb, hT, rh, sqb, r_sqb, tmpb, r_tmpb, rstd, r_rstd, hT32=None, rh32=None):
        s, n = TBS[tb]
        j = 4 if tb == 4 else jb
        bk, rb = nbank()
        for c in range(8):
            i = c % 2
            P.op("act", lambda e, c=c, i=i: e.activation(out=sqb[i][:, 0:n], in_=xT[:, c, s:s + n], func=AF.Square),
                 reads=[rx[tb]], writes=[r_sqb[i]])
            P.op("pe", lambda e, c=c, i=i, bk=bk: e.matmul(bk[:, 0:n], ones_f[:], sqb[i][:, 0:n], start=(c == 0), stop=(c == 7)),
                 reads=[r_sqb[i], r_const], writes=[rb])
        P.op("act", lambda e, bk=bk: e.activation(out=rstd[:, 0:n], in_=bk[:, 0:n], func=AF.Sqrt, scale=1.0 / D, bias=cst[:, 0:1]),
             reads=[rb, r_cst], writes=[r_rstd])
        P.op("dve", lambda e: e.reciprocal(out=rstd[:, 0:n], in_=rstd[:, 0:n]), reads=[r_rstd], writes=[r_rstd])
        for c in range(8):
            i = c % 2
            g = mdv[:, l, half * 3 + 0, c, j:j + 1]
            sh = mdv[:, l, half * 3 + 1, c, j:j + 1]
            P.op("dve", lambda e, c=c, i=i, g=g: e.scalar_tensor_tensor(out=tmpb[i][:, 0:n], in0=xT[:, c, s:s + n], scalar=g,
                                                                      in1=rstd[:, 0:n], op0=ALU.mult, op1=ALU.mult),
                 reads=[rx[tb], r_mdv, r_rstd], writes=[r_tmpb[i]])
            P.op("act", lambda e, c=c, i=i, sh=sh: e.activation(out=hT[:, c, s:s + n], in_=tmpb[i][:, 0:n], func=AF.Identity,
                                                                scale=1.0, bias=sh),
                 reads=[r_tmpb[i], r_mdv], writes=[rh[tb]])
            if hT32 is not None:
                P.op("pool", lambda e, c=c, i=i, sh=sh: e.tensor_scalar(out=hT32[:, c, 0:n], in0=tmpb[i][:, 0:n], scalar1=sh, scalar2=None,
                                                                       op0=ALU.add),
                     reads=[r_tmpb[i], r_mdv], writes=[rh32])

    def phase_proj(l, jb, tbs):
        P.push_scope()
        hT = P.sb("hT", [128, 8, NT], BF16)
        rh = [Res() for _ in TBS]
        sqb = [P.sb(f"p1_sq{i}", [128, 512], F32) for i in range(2)]
        r_sqb = [Res(), Res()]
        tmpb = [P.sb(f"p1_tmp{i}", [128, 512], F32) for i in range(2)]
        r_tmpb = [Res(), Res()]
        rstd = P.sb("p1_rstd", [128, 512], F32)
        r_rstd = Res()
        for tb in tbs:
            norm_block(l, 0, jb, tb, hT, rh, sqb, r_sqb, tmpb, r_tmpb, rstd, r_rstd)
        if "h" in dbg and l == 0:
            hd = dbg_tensor("dbg_h", [D, NT], BF16)
            for c in range(8):
                P.dma("sp", hd[c * 128:(c + 1) * 128, :], hT[:, c, :], reads=rh)
        wst = [P.sb(f"p2_wst{i}", [128, 8, 256], F32) for i in range(2)]
        r_wst = [Res(), Res()]
        wbf = [P.sb(f"p2_wbf{i}", [128, 8, 256], BF16) for i in range(2)]
        r_wbf = [Res(), Res()]
        pf = P.sb("p2_pf", [128, NT + 3], F32)
        r_pf = Res()
        po = [P.sb(f"p2_po{i}", [128, NT], F32) for i in range(2)]
        r_po = [Res(), Res()]
        pg = [P.sb(f"p2_pg{i}", [128, NT], BF16) for i in range(2)]
        r_pg = [Res(), Res()]
        P.op("pool", lambda e: e.memset(pf[:], 0.0), writes=[r_pf])
        wsrc = w_in_d[l].rearrange("(kc p) n -> p kc n", p=128)
        nctx = 4 in tbs
        def issue_w(cg_):
            i_ = cg_ % 2
            P.dma("sp", wst[i_][:], wsrc[:, :, cg_ * 256:(cg_ + 1) * 256], writes=[r_wst[i_]])
            P.op("pool", lambda e: e.tensor_copy(out=wbf[i_][:], in_=wst[i_][:]), reads=[r_wst[i_]], writes=[r_wbf[i_]])
        issue_w(0)
        for cg in range(18):
            i = cg % 2
            if cg + 1 < 18:
                issue_w(cg + 1)
            for cc in range(2):
                gc = cg * 2 + cc
                o = gc % 2
                for tb in tbs:
                    s, n = TBS[tb]
                    bk, rb = nbank()
                    for kc in range(8):
                        P.op("pe", lambda e, bk=bk, i=i, kc=kc, cc=cc, s=s, n=n: e.matmul(
                            bk[:, 0:n], wbf[i][:, kc, cc * 128:(cc + 1) * 128], hT[:, kc, s:s + n], start=(kc == 0), stop=(kc == 7)),
                            reads=[r_wbf[i], rh[tb]], writes=[rb])
                    if gc < 14:
                        off = 1 + s if tb < 4 else 2 + s
                        P.op("act", lambda e, bk=bk, off=off, n=n: e.copy(out=pf[:, off:off + n], in_=bk[:, 0:n]),
                             reads=[rb], writes=[r_pf])
                    elif gc < 20:
                        P.op("act", lambda e, bk=bk, o=o, s=s, n=n: e.copy(out=po[o][:, s:s + n], in_=bk[:, 0:n]),
                             reads=[rb], writes=[r_po[o]])
                    else:
                        P.op("act", lambda e, bk=bk, o=o, s=s, n=n: e.activation(out=pg[o][:, s:s + n], in_=bk[:, 0:n], func=AF.Sigmoid),
                             reads=[rb], writes=[r_pg[o]])
                if gc < 14:
                    muc = dpar[:, l * 32 + gc: l * 32 + gc + 1]
                    mu0 = pcol(l, "mu0", gc)
                    mu1 = pcol(l, "mu1", gc)
                    segs = [(0, LAT, 1)] + ([(LAT, CTX, 2 + LAT)] if nctx else [])
                    for (os_, n, ps_) in segs:
                        P.op("act", lambda e, o=o, os_=os_, n=n, ps_=ps_, muc=muc: e.activation(
                            out=po[o][:, os_:os_ + n], in_=pf[:, ps_:ps_ + n], func=AF.Identity, scale=muc, bias=cst[:, 3:4]),
                            reads=[r_pf, r_dpar, r_cst], writes=[r_po[o]])
                        P.op("dve", lambda e, o=o, os_=os_, n=n, ps_=ps_, mu0=mu0: e.scalar_tensor_tensor(
                            out=po[o][:, os_:os_ + n], in0=pf[:, ps_ - 1:ps_ - 1 + n], scalar=mu0, in1=po[o][:, os_:os_ + n],
                            op0=ALU.mult, op1=ALU.add), reads=[r_pf, r_par, r_po[o]], writes=[r_po[o]])
                        P.op("dve", lambda e, o=o, os_=os_, n=n, ps_=ps_, mu1=mu1: e.scalar_tensor_tensor(
                            out=po[o][:, os_:os_ + n], in0=pf[:, ps_ + 1:ps_ + 1 + n], scalar=mu1, in1=po[o][:, os_:os_ + n],
                            op0=ALU.mult, op1=ALU.add), reads=[r_pf, r_par, r_po[o]], writes=[r_po[o]])
                    P.dma("sp", PR_d[gc * 128:(gc + 1) * 128, :], po[o][:], reads=[r_po[o]], writes=[rPR[gc]])
                elif gc < 20:
                    P.dma("sp", PA_d[(gc - 14) * 128:(gc - 13) * 128, :], po[o][:], reads=[r_po[o]], writes=[rPA[gc - 14]])
                else:
                    P.dma("sp", PG_d[(gc - 20) * 128:(gc - 19) * 128, :], pg[o][:], reads=[r_pg[o]], writes=[rPG[gc - 20]])
        P.pop_scope()

    KR_d = dscr("KR", [128, NT], BF16)
    rKR = Res()

    def phase_attn(l, do_ctx):
        P.push_scope()
        cos_t = P.sb("at_cos", [128, LAT], F32)
        sin_t = P.sb("at_sin", [128, LAT], F32)
        r_cs = Res()
        P.dma("sp", cos_t[:], cos_d, writes=[r_cs])
        P.dma("sp", sin_t[:], sin_d, writes=[r_cs])
        ones_b = P.sb("at_ones", [128, 64], BF16)
        P.op("pool", lambda e: e.memset(ones_b[:], 1.0), writes=[r_cs])
        raw = P.sb("at_raw", [128, NT], F32)
        r_raw = Res()
        QR = [P.sb(f"at_qr{c}", [128, NT], BF16) for c in range(4)]
        r_QR = [Res() for _ in range(4)]
        krp = P.sb("at_krp", [128, NT], BF16)
        r_krp = Res()
        KX = [[P.sb(f"at_kx{g}{hf}", [128, NT], BF16) for hf in range(2)] for g in range(2)]
        r_KX = [[Res(), Res()], [Res(), Res()]]
        Vt = P.sb("at_vt", [128, 18, 128], BF16)
        r_Vt = Res()
        sq = P.sb("at_sq", [128, 512], BF16)
        r_sq = Res()
        rs = P.sb("at_rs", [128, 512], F32)
        r_rs = Res()
        qn = P.sb("at_qn", [128, 512], BF16)
        r_qn = Res()
        t1 = P.sb("at_t1", [128, 512], F32)
        r_t1 = Res()
        t2 = P.sb("at_t2", [128, 512], F32)
        r_t2 = Res()

        def proc_chunk(src_rows, gain, dst, r_dst):
            P.dma("sp", raw[:], PA_d[src_rows * 128:(src_rows + 1) * 128, :], reads=[rPA[src_rows]], writes=[r_raw])
            for tb in range(5):
                s, n = TBS[tb]
                P.op("act", lambda e: e.activation(out=sq[:, 0:n], in_=raw[:, s:s + n], func=AF.Square), reads=[r_raw], writes=[r_sq])
                bk, rb = nbank()
                P.op("pe", lambda e: e.matmul(bk[:, 0:n], onesbd_b[:], sq[:, 0:n], start=True, stop=True), reads=[r_sq, r_const], writes=[rb])
                P.op("act", lambda e: e.activation(out=rs[:, 0:n], in_=bk[:, 0:n], func=AF.Sqrt, scale=1.0 / 64, bias=cst[:, 0:1]),
                     reads=[rb, r_cst], writes=[r_rs])
                P.op("dve", lambda e: e.reciprocal(out=rs[:, 0:n], in_=rs[:, 0:n]), reads=[r_rs], writes=[r_rs])
                if tb < 4:
                    P.op("dve", lambda e: e.scalar_tensor_tensor(out=qn[:, 0:n], in0=raw[:, s:s + n], scalar=gain, in1=rs[:, 0:n],
                                                                op0=ALU.mult, op1=ALU.mult), reads=[r_raw, r_rs, r_par, r_dpar], writes=[r_qn])
                    bk2, rb2 = nbank()
                    P.op("pe", lambda e: e.matmul(bk2[:, 0:n], rot_b[:], qn[:, 0:n], start=True, stop=True), reads=[r_qn, r_const], writes=[rb2])
                    P.op("pool", lambda e: e.tensor_tensor(out=t1[:, 0:n], in0=qn[:, 0:n], in1=cos_t[:, s:s + n], op=ALU.mult),
                         reads=[r_qn, r_cs], writes=[r_t1])
                    P.op("dve", lambda e: e.tensor_tensor(out=t2[:, 0:n], in0=bk2[:, 0:n], in1=sin_t[:, s:s + n], op=ALU.mult),
                         reads=[rb2, r_cs], writes=[r_t2])
                    P.op("pool", lambda e: e.tensor_tensor(out=dst[:, s:s + n], in0=t1[:, 0:n], in1=t2[:, 0:n], op=ALU.add),
                         reads=[r_t1, r_t2], writes=[r_dst])
                else:
                    P.op("dve", lambda e: e.scalar_tensor_tensor(out=dst[:, s:s + n], in0=raw[:, s:s + n], scalar=gain, in1=rs[:, 0:n],
                                                                op0=ALU.mult, op1=ALU.mult), reads=[r_raw, r_rs, r_par, r_dpar], writes=[r_dst])

        qg = dpar[:, l * 32 + 18:l * 32 + 19]
        for c in range(4):
            proc_chunk(c, qg, QR[c], r_QR[c])
        proc_chunk(4, pcol(l, "kn"), krp, r_krp)
        P.dma("sp", KR_d, krp[:], reads=[r_krp], writes=[rKR])
        for g in range(2):
            for hf in range(2):
                P.op("pool", lambda e, g=g, hf=hf: e.memset(KX[g][hf][:], 0.0), writes=[r_KX[g][hf]])
                P.dma("sp", KX[g][hf][hf * 64:(hf + 1) * 64, :], KR_d[g * 64:(g + 1) * 64, :], reads=[rKR], writes=[r_KX[g][hf]])
        P.dma("sp", raw[:], PA_d[5 * 128:6 * 128, :], reads=[rPA[5]], writes=[r_raw])
        for k4 in range(0, 18, 4):
            nk = min(4, 18 - k4)
            bk, rb = nbank()
            for i in range(nk):
                kc = k4 + i
                P.op("pe", lambda e, i=i, kc=kc: e.transpose(bk[:, i * 128:(i + 1) * 128], raw[:, kc * 128:(kc + 1) * 128], ident_f[:]),
                     reads=[r_raw, r_const], writes=[rb])
            P.op("act", lambda e, k4=k4, nk=nk: e.copy(out=Vt[:, k4:k4 + nk, :].rearrange("p a b -> p (a b)"), in_=bk[:, 0:nk * 128]),
                 reads=[rb], writes=[r_Vt])
        if "attn" in dbg and l == 0:
            dq = dbg_tensor("dbg_qr", [512, NT], BF16)
            for c in range(4):
                P.dma("sp", dq[c * 128:(c + 1) * 128, :], QR[c][:], reads=[r_QR[c]])
            dk = dbg_tensor("dbg_kr", [128, NT], BF16)
            P.dma("sp", dk, krp[:], reads=[r_krp])
        pT = [P.sb(f"at_pT{i}", [128, 512], BF16) for i in range(3)]
        r_pT = [Res() for _ in range(3)]
        rec = P.sb("at_rec", [64, 512], F32)
        r_rec = Res()
        yo = [P.sb(f"at_yo{i}", [64, 512], BF16) for i in range(2)]
        r_yo = [Res(), Res()]
        cnt = 0
        for h in range(8):
            c, hf, g = h // 2, h % 2, h // 4
            for tb in range(5 if do_ctx else 4):
                s, n = TBS[tb]
                kcs = list(range(18)) if tb < 4 else [16, 17]
                io = cnt % 2
                bkO, rbO = banks[io * 2], rbank[io * 2]
                bkD, rbD = banks[io * 2 + 1], rbank[io * 2 + 1]
                for ki, kc in enumerate(kcs):
                    bkS, rbS = nbank(4, 8)
                    ip = (cnt * 18 + ki) % 3
                    P.op("pe", lambda e, bkS=bkS, kc=kc: e.matmul(bkS[:, 0:n], KX[g][hf][:, kc * 128:(kc + 1) * 128], QR[c][:, s:s + n],
                                                               start=True, stop=True), reads=[r_KX[g][hf], r_QR[c]], writes=[rbS])
                    P.op("act", lambda e, bkS=bkS, ip=ip: e.activation(out=pT[ip][:, 0:n], in_=bkS[:, 0:n], func=AF.Exp),
                         reads=[rbS], writes=[r_pT[ip]])
                    P.op("pe", lambda e, kc=kc, ip=ip, ki=ki: e.matmul(bkO[0:64, 0:n], Vt[:, kc, g * 64:(g + 1) * 64], pT[ip][:, 0:n],
                                                                      start=(ki == 0), stop=(ki == len(kcs) - 1)),
                         reads=[r_Vt, r_pT[ip]], writes=[rbO])
                    P.op("pe", lambda e, ip=ip, ki=ki: e.matmul(bkD[0:64, 0:n], ones_b[:], pT[ip][:, 0:n],
                                                               start=(ki == 0), stop=(ki == len(kcs) - 1)),
                         reads=[r_cs, r_pT[ip]], writes=[rbD])
                P.op("dve", lambda e: e.reciprocal(out=rec[:, 0:n], in_=bkD[0:64, 0:n]), reads=[rbD], writes=[r_rec])
                P.op("dve", lambda e, io=io: e.tensor_tensor(out=yo[io][:, 0:n], in0=bkO[0:64, 0:n], in1=rec[:, 0:n], op=ALU.mult),
                     reads=[rbO, r_rec], writes=[r_yo[io]])
                P.dma("sp", YA_d[h * 64:(h + 1) * 64, s:s + n], yo[io][:, 0:n], reads=[r_yo[io]], writes=[rYA[h // 2]])
                cnt += 1
        P.pop_scope()

    def load_cast(dst_ap, src_ap, stg_list, r_stg_list, ctr, r_dst, shape_slice):
        i = ctr[0] % len(stg_list)
        ctr[0] += 1
        st = shape_slice(stg_list[i])
        P.dma("sp", st, src_ap, writes=[r_stg_list[i]])
        P.op("pool", lambda e: e.tensor_copy(out=dst_ap, in_=st), reads=[r_stg_list[i]], writes=[r_dst])

    def phase_merge(l, jb, tbs):
        P.push_scope()
        stg = [P.sb(f"mg_stg{i}", [128, 1024], F32) for i in range(2)]
        r_stg = [Res(), Res()]
        ctr = [0]
        wpa = P.sb("mg_wpa", [128, 4, 1024], BF16)
        wpb = P.sb("mg_wpb", [128, 4, 1024], BF16)
        wo = P.sb("mg_wo", [128, 8, 1024], BF16)
        r_w = Res()
        for kc in range(4):
            load_cast(wpa[:, kc, :], w_pa_d[l, kc * 128:(kc + 1) * 128, :], stg, r_stg, ctr, r_w, lambda t: t[:])
            load_cast(wpb[:, kc, :], w_pb_d[l, kc * 128:(kc + 1) * 128, :], stg, r_stg, ctr, r_w, lambda t: t[:])
        for kc in range(8):
            load_cast(wo[:, kc, :], w_o_d[l, kc * 128:(kc + 1) * 128, :], stg, r_stg, ctr, r_w, lambda t: t[:])
        yr = P.sb("mg_yr", [128, 4, 512], BF16)
        ya = P.sb("mg_ya", [128, 4, 512], BF16)
        ga = P.sb("mg_ga", [128, 8, 512], BF16)
        gb = P.sb("mg_gb", [128, 8, 512], BF16)
        r_in = Res()
        z = P.sb("mg_z", [128, 8, 512], BF16)
        r_z = Res()
        ta = [P.sb(f"mg_ta{i}", [128, 512], F32) for i in range(2)]
        r_ta = [Res(), Res()]
        tb_ = [P.sb(f"mg_tb{i}", [128, 512], F32) for i in range(2)]
        r_tb = [Res(), Res()]
        for tb in tbs:
            s, n = TBS[tb]
            j = 4 if tb == 4 else jb
            P.dma("sp", yr[:, :, 0:n], YR_d.rearrange("(c p) t -> p c t", p=128)[:, :, s:s + n], reads=rYR, writes=[r_in])
            P.dma("sp", ya[:, :, 0:n], YA_d.rearrange("(c p) t -> p c t", p=128)[:, :, s:s + n], reads=rYA, writes=[r_in])
            P.dma("sp", ga[:, :, 0:n], PG_d[0:1024, :].rearrange("(c p) t -> p c t", p=128)[:, :, s:s + n], reads=rPG, writes=[r_in])
            P.dma("sp", gb[:, :, 0:n], PG_d[1024:2048, :].rearrange("(c p) t -> p c t", p=128)[:, :, s:s + n], reads=rPG, writes=[r_in])
            for dc in range(8):
                i = dc % 2
                bkA, rbA = nbank()
                for kc in range(4):
                    P.op("pe", lambda e, kc=kc: e.matmul(bkA[:, 0:n], wpa[:, kc, dc * 128:(dc + 1) * 128], yr[:, kc, 0:n],
                                                        start=(kc == 0), stop=(kc == 3)), reads=[r_w, r_in], writes=[rbA])
                bkB, rbB = nbank()
                for kc in range(4):
                    P.op("pe", lambda e, kc=kc: e.matmul(bkB[:, 0:n], wpb[:, kc, dc * 128:(dc + 1) * 128], ya[:, kc, 0:n],
                                                        start=(kc == 0), stop=(kc == 3)), reads=[r_w, r_in], writes=[rbB])
                P.op("dve", lambda e: e.tensor_tensor(out=ta[i][:, 0:n], in0=bkA[:, 0:n], in1=ga[:, dc, 0:n], op=ALU.mult),
                     reads=[rbA, r_in], writes=[r_ta[i]])
                P.op("dve", lambda e: e.tensor_tensor(out=tb_[i][:, 0:n], in0=bkB[:, 0:n], in1=gb[:, dc, 0:n], op=ALU.mult),
                     reads=[rbB, r_in], writes=[r_tb[i]])
                P.op("pool", lambda e: e.tensor_tensor(out=z[:, dc, 0:n], in0=ta[i][:, 0:n], in1=tb_[i][:, 0:n], op=ALU.add),
                     reads=[r_ta[i], r_tb[i]], writes=[r_z])
            for dc in range(8):
                bkO, rbO = nbank()
                for kc in range(8):
                    P.op("pe", lambda e, kc=kc: e.matmul(bkO[:, 0:n], wo[:, kc, dc * 128:(dc + 1) * 128], z[:, kc, 0:n],
                                                        start=(kc == 0), stop=(kc == 7)), reads=[r_w, r_z], writes=[rbO])
                gt = mdv[:, l, 2, dc, j:j + 1]
                P.op("dve", lambda e, gt=gt: e.scalar_tensor_tensor(out=xT[:, dc, s:s + n], in0=bkO[:, 0:n], scalar=gt, in1=xT[:, dc, s:s + n],
                                                                   op0=ALU.mult, op1=ALU.add), reads=[rbO, r_mdv, rx[tb]], writes=[rx[tb]])
        P.pop_scope()

    def phase_ffn(l, jb, tbs, moe):
        P.push_scope()
        hT = P.sb("hT2", [128, 8, NT], BF16)
        rh = [Res() for _ in TBS]
        if moe:
            wgT = P.sb("f1_wgT", [8, LAT], F32)
            r_wgT = Res()
            sel_all = P.sb("f1_sel", [8, NE, 128], F32)
            r_sel = Res()
        P.push_scope()
        sqb = [P.sb(f"f1_sq{i}", [128, 512], F32) for i in range(2)]
        r_sqb = [Res(), Res()]
        tmpb = [P.sb(f"f1_tmp{i}", [128, 512], F32) for i in range(2)]
        r_tmpb = [Res(), Res()]
        rstd = P.sb("f1_rstd", [128, 512], F32)
        r_rstd = Res()
        if moe:
            h32 = P.sb("f1_h32", [128, 8, 512], F32)
            r_h32 = Res()
            rt_f = P.sb("f1_rt", [128, 8, NE], F32)
            r_rt = Res()
            P.dma("sp", rt_f[:], router_d[0].rearrange("(kc p) e -> p kc e", p=128), writes=[r_rt])
            lgT = P.sb("f1_lgT", [8, 512], F32)
            r_lgT = Res()
            sm = P.sb("f1_sm", [128, 64], F32)
            r_sm = Res()
            for e_ in range(NE):
                P.op("dve", lambda e, e_=e_: e.tensor_copy(out=sel_all[:, e_, :], in_=bass.AP(ident_f, e_, [[128, 8], [0, 128]])),
                     reads=[r_const], writes=[r_sel])
        for tb in tbs:
            s, n = TBS[tb]
            if moe:
                norm_block(l, 1, jb, tb, hT, rh, sqb, r_sqb, tmpb, r_tmpb, rstd, r_rstd, hT32=h32, rh32=r_h32)
                bk, rb = nbank()
                for kc in range(8):
                    P.op("pe", lambda e, kc=kc: e.matmul(bk[0:8, 0:n], rt_f[:, kc, :], h32[:, kc, 0:n], start=(kc == 0), stop=(kc == 7)),
                         reads=[r_rt, r_h32], writes=[rb])
                P.op("act", lambda e: e.copy(out=lgT[:, 0:n], in_=bk[0:8, 0:n]), reads=[rb], writes=[r_lgT])
                bk2, rb2 = nbank()
                for sb_ in range(n // 128):
                    bk1, rb1 = nbank()
                    P.op("pe", lambda e: e.transpose(bk1[:, 0:8], lgT[:, sb_ * 128:(sb_ + 1) * 128], ident_f[0:8, 0:8]),
                         reads=[r_lgT, r_const], writes=[rb1])
                    lg, m1, eq, lg2, m2, sel, ex, den = (sm[:, 0:8], sm[:, 8:9], sm[:, 16:24], sm[:, 24:32], sm[:, 9:10], sm[:, 32:40],
                                                         sm[:, 40:48], sm[:, 10:11])
                    nm1 = sm[:, 11:12]
                    wg_ = sm[:, 48:56]
                    P.op("dve", lambda e: e.tensor_copy(out=lg, in_=bk1[:, 0:8]), reads=[rb1], writes=[r_sm])
                    P.op("dve", lambda e: e.tensor_reduce(out=m1, in_=lg, axis=AX.X, op=ALU.max), reads=[r_sm], writes=[r_sm])
                    P.op("dve", lambda e: e.tensor_scalar(out=eq, in0=lg, scalar1=m1, scalar2=-1e30, op0=ALU.is_equal, op1=ALU.mult),
                         reads=[r_sm], writes=[r_sm])
                    P.op("dve", lambda e: e.tensor_tensor(out=lg2, in0=lg, in1=eq, op=ALU.add), reads=[r_sm], writes=[r_sm])
                    P.op("dve", lambda e: e.tensor_reduce(out=m2, in_=lg2, axis=AX.X, op=ALU.max), reads=[r_sm], writes=[r_sm])
                    P.op("dve", lambda e: e.tensor_scalar(out=sel, in0=lg, scalar1=m2, scalar2=None, op0=ALU.is_ge), reads=[r_sm], writes=[r_sm])
                    P.op("dve", lambda e: e.tensor_scalar(out=nm1, in0=m1, scalar1=-1.0, scalar2=None, op0=ALU.mult), reads=[r_sm], writes=[r_sm])
                    P.op("act", lambda e: e.activation(out=ex, in_=lg, func=AF.Exp, scale=1.0, bias=nm1), reads=[r_sm], writes=[r_sm])
                    P.op("dve", lambda e: e.tensor_tensor(out=ex, in0=ex, in1=sel, op=ALU.mult), reads=[r_sm], writes=[r_sm])
                    P.op("dve", lambda e: e.tensor_reduce(out=den, in_=ex, axis=AX.X, op=ALU.add), reads=[r_sm], writes=[r_sm])
                    P.op("dve", lambda e: e.reciprocal(out=den, in_=den), reads=[r_sm], writes=[r_sm])
                    P.op("dve", lambda e: e.tensor_scalar(out=wg_, in0=ex, scalar1=den, scalar2=None, op0=ALU.mult), reads=[r_sm], writes=[r_sm])
                    P.op("pe", lambda e: e.transpose(bk2[0:8, sb_ * 128:(sb_ + 1) * 128], wg_, ident_f[:]), reads=[r_sm, r_const], writes=[rb2])
                P.op("act", lambda e: e.copy(out=wgT[:, s:s + n], in_=bk2[0:8, 0:n]), reads=[rb2], writes=[r_wgT])
            else:
                norm_block(l, 1, jb, tb, hT, rh, sqb, r_sqb, tmpb, r_tmpb, rstd, r_rstd)
        if "moe" in dbg and moe:
            dw = dbg_tensor("dbg_wgT", [8, LAT], F32)
            P.dma("sp", dw, wgT[:], reads=[r_wgT])
        P.pop_scope()
        NFI = 4
        stg = [P.sb(f"f2_stg{i}", [128, 1024], F32) for i in range(4)]
        r_stg = [Res() for _ in range(4)]
        ctr = [0]
        wgb = [P.sb(f"f2_wgb{i}", [128, 8, 128], BF16) for i in range(3)]
        wub = [P.sb(f"f2_wub{i}", [128, 8, 128], BF16) for i in range(3)]
        r_wgu = [Res(), Res(), Res()]
        wdb = [P.sb(f"f2_wdb{i}", [128, NFI, 1024], BF16) for i in range(2)]
        r_wdb = [Res(), Res()]
        actT = P.sb("f2_act", [128, NFI, NT], BF16)
        r_act = [Res() for _ in TBS]
        sl = [P.sb(f"f2_sl{i}", [128, 512], F32) for i in range(2)]
        r_sl = [Res(), Res()]
        sl2 = [P.sb(f"f2_sl2{i}", [128, 512], F32) for i in range(2)]
        r_sl2 = [Res(), Res()]
        if moe:
            wb = P.sb("f2_wb", [128, LAT], BF16)
            r_wb = Res()
        gi = 0
        ii = 0
        for ex_ in range(NE if moe else 1):
            if moe:
                wgs, wus, wds = moe_wg_d[0, ex_], moe_wu_d[0, ex_], moe_wd_d[0, ex_]
                for tb in tbs:
                    s, n = TBS[tb]
                    bk, rb = nbank()
                    P.op("pe", lambda e: e.matmul(bk[:, 0:n], sel_all[:, ex_, :], wgT[:, s:s + n], start=True, stop=True),
                         reads=[r_sel, r_wgT], writes=[rb])
                    P.op("act", lambda e: e.copy(out=wb[:, s:s + n], in_=bk[:, 0:n]), reads=[rb], writes=[r_wb])
            else:
                wgs, wus, wds = ffn_wg_d[0], ffn_wu_d[0], ffn_wd_d[0]
            wgs_r = wgs.rearrange("(kc p) f -> p kc f", p=128)
            wus_r = wus.rearrange("(kc p) f -> p kc f", p=128)
            for fg in range(FF // 128 // NFI):
                g2 = gi % 2
                gi += 1
                for fi in range(NFI):
                    fc = fg * NFI + fi
                    w2i = ii % 3
                    ii += 1
                    load_cast(wgb[w2i][:], wgs_r[:, :, fc * 128:(fc + 1) * 128], stg, r_stg, ctr, r_wgu[w2i],
                              lambda t: t[:].rearrange("p (a b) -> p a b", a=8))
                    load_cast(wub[w2i][:], wus_r[:, :, fc * 128:(fc + 1) * 128], stg, r_stg, ctr, r_wgu[w2i],
                              lambda t: t[:].rearrange("p (a b) -> p a b", a=8))
                    load_cast(wdb[g2][:, fi, :], wds[fc * 128:(fc + 1) * 128, :], stg, r_stg, ctr, r_wdb[g2], lambda t: t[:])
                    for tb in tbs:
                        s, n = TBS[tb]
                        si = (fi + tb) % 2
                        bkG, rbG = nbank()
                        for kc in range(8):
                            P.op("pe", lambda e, kc=kc: e.matmul(bkG[:, 0:n], wgb[w2i][:, kc, :], hT[:, kc, s:s + n], start=(kc == 0), stop=(kc == 7)),
                                 reads=[r_wgu[w2i], rh[tb]], writes=[rbG])
                        bkU, rbU = nbank()
                        for kc in range(8):
                            P.op("pe", lambda e, kc=kc: e.matmul(bkU[:, 0:n], wub[w2i][:, kc, :], hT[:, kc, s:s + n], start=(kc == 0), stop=(kc == 7)),
                                 reads=[r_wgu[w2i], rh[tb]], writes=[rbU])
                        P.op("act", lambda e: e.activation(out=sl[si][:, 0:n], in_=bkG[:, 0:n], func=AF.Silu), reads=[rbG], writes=[r_sl[si]])
                        if moe:
                            P.op("dve", lambda e: e.tensor_tensor(out=sl2[si][:, 0:n], in0=bkU[:, 0:n], in1=sl[si][:, 0:n], op=ALU.mult),
                                 reads=[rbU, r_sl[si]], writes=[r_sl2[si]])
                            P.op("pool", lambda e: e.tensor_tensor(out=actT[:, fi, s:s + n], in0=sl2[si][:, 0:n], in1=wb[:, s:s + n], op=ALU.mult),
                                 reads=[r_sl2[si], r_wb], writes=[r_act[tb]])
                        else:
                            P.op("dve", lambda e: e.tensor_tensor(out=actT[:, fi, s:s + n], in0=bkU[:, 0:n], in1=sl[si][:, 0:n], op=ALU.mult),
                                 reads=[rbU, r_sl[si]], writes=[r_act[tb]])
                for tb in tbs:
                    s, n = TBS[tb]
                    j = 4 if tb == 4 else jb
                    for dc in range(8):
                        bkO, rbO = nbank()
                        for fi in range(NFI):
                            P.op("pe", lambda e, fi=fi: e.matmul(bkO[:, 0:n], wdb[g2][:, fi, dc * 128:(dc + 1) * 128], actT[:, fi, s:s + n],
                                                                start=(fi == 0), stop=(fi == NFI - 1)), reads=[r_wdb[g2], r_act[tb]], writes=[rbO])
                        gt = mdv[:, l, 5, dc, j:j + 1]
                        P.op("dve", lambda e, gt=gt: e.scalar_tensor_tensor(out=xT[:, dc, s:s + n], in0=bkO[:, 0:n], scalar=gt, in1=xT[:, dc, s:s + n],
                                                                           op0=ALU.mult, op1=ALU.add), reads=[rbO, r_mdv, rx[tb]], writes=[rx[tb]])
        P.pop_scope()

    def phase_final(jb):
        P.push_scope()
        sqb = [P.sb(f"fn_sq{i}", [128, 512], F32) for i in range(2)]
        r_sqb = [Res(), Res()]
        ob = [P.sb(f"fn_o{i}", [128, 512], F32) for i in range(2)]
        r_ob = [Res(), Res()]
        rstd = P.sb("fn_rstd", [128, 512], F32)
        r_rstd = Res()
        for tb in range(4):
            s, n = TBS[tb]
            bk, rb = nbank()
            for c in range(8):
                i = c % 2
                P.op("act", lambda e: e.activation(out=sqb[i][:, 0:n], in_=xT[:, c, s:s + n], func=AF.Square), reads=[rx[tb]], writes=[r_sqb[i]])
                P.op("pe", lambda e: e.matmul(bk[:, 0:n], ones_f[:], sqb[i][:, 0:n], start=(c == 0), stop=(c == 7)),
                     reads=[r_sqb[i], r_const], writes=[rb])
            P.op("act", lambda e: e.activation(out=rstd[:, 0:n], in_=bk[:, 0:n], func=AF.Sqrt, scale=1.0 / D, bias=cst[:, 0:1]),
                 reads=[rb, r_cst], writes=[r_rstd])
            P.op("dve", lambda e: e.reciprocal(out=rstd[:, 0:n], in_=rstd[:, 0:n]), reads=[r_rstd], writes=[r_rstd])
            for c in range(8):
                i = c % 2
                fnc = par[:, 2 * NPL + c:2 * NPL + c + 1]
                P.op("dve", lambda e: e.scalar_tensor_tensor(out=ob[i][:, 0:n], in0=xT[:, c, s:s + n], scalar=fnc, in1=rstd[:, 0:n],
                                                            op0=ALU.mult, op1=ALU.mult), reads=[rx[tb], r_par, r_rstd], writes=[r_ob[i]])
                P.dma("sp", outT_d[jb, c * 128:(c + 1) * 128, s:s + n], ob[i][:, 0:n], reads=[r_ob[i]])
        P.pop_scope()

    RB = 256

    def phase_rwkv(l):
        P.push_scope()
        twT = P.sb("rw_tw", [64, NT], BF16)
        adT = P.sb("rw_ad", [64, NT], BF16)
        gsT = P.sb("rw_gs", [128, NT], BF16)
        r_lora = Res()
        w2b = [P.sb(f"rw_w2b{d}", [64, 512], BF16) for d in range(2)]
        a2b = [P.sb(f"rw_a2b{d}", [64, 512], BF16) for d in range(2)]
        g2b = P.sb("rw_g2b", [128, 512], BF16)
        mk = P.sb("rw_mk", [128, 2560], BF16)
        rmask = P.sb("rw_rmask", [128, 512], F32)
        r_w = Res()
        P.push_scope()
        stg = P.sb("rw_stg", [128, NT], F32)
        r_stg = Res()
        P.dma("sp", stg[0:64, :], PR_d[1536:1600, :], reads=[rPR[12]], writes=[r_stg])
        P.op("act", lambda e: e.activation(out=twT[:], in_=stg[0:64, :], func=AF.Tanh), reads=[r_stg], writes=[r_lora])
        P.dma("sp", stg[0:64, :], PR_d[1600:1664, :], reads=[rPR[12]], writes=[r_stg])
        P.op("act", lambda e: e.copy(out=adT[:], in_=stg[0:64, :]), reads=[r_stg], writes=[r_lora])
        P.dma("sp", stg[:], PR_d[1664:1792, :], reads=[rPR[13]], writes=[r_stg])
        P.op("act", lambda e: e.activation(out=gsT[:], in_=stg[:], func=AF.Sigmoid), reads=[r_stg], writes=[r_lora])
        for d in range(2):
            P.dma("sp", stg[0:64, 0:512], w2_d[l, d], writes=[r_stg])
            P.op("dve", lambda e: e.tensor_copy(out=w2b[d][:], in_=stg[0:64, 0:512]), reads=[r_stg], writes=[r_w])
            P.dma("sp", stg[0:64, 0:512], a2_d[l, d], writes=[r_stg])
            P.op("dve", lambda e: e.tensor_copy(out=a2b[d][:], in_=stg[0:64, 0:512]), reads=[r_stg], writes=[r_w])
        P.dma("sp", stg[:, 0:512], g2_d[l], writes=[r_stg])
        P.op("dve", lambda e: e.tensor_copy(out=g2b[:], in_=stg[:, 0:512]), reads=[r_stg], writes=[r_w])
        for i in range(5):
            P.dma("sp", stg[:, 0:512], mask_d[:, i * 512:(i + 1) * 512], writes=[r_stg])
            P.op("dve", lambda e: e.tensor_copy(out=mk[:, i * 512:(i + 1) * 512], in_=stg[:, 0:512]), reads=[r_stg], writes=[r_w])
        P.dma("sp", rmask[:], rmask_d, writes=[r_w])
        P.pop_scope()

        rT = P.sb("rw_r", [128, NT], F32)
        kT = P.sb("rw_k", [128, NT], F32)
        vT = P.sb("rw_v", [128, NT], F32)
        kkT = P.sb("rw_kk", [128, NT], F32)
        r_rkv = Res()
        r_kk = Res()
        Yacc = P.sb("rw_yacc", [128, NT], F32)
        r_Y = Res()
        yob = [(P.sb(f"rw_yob{i}", [128, RB], BF16), Res()) for i in range(2)]
        Vp = P.sb("rw_vp", [128, 18, 256], BF16)
        r_Vp = Res()
        P.op("pool", lambda e: e.memset(Vp[:], 0.0), writes=[r_Vp])

        def f32t(nm, w=RB):
            return P.sb(nm, [128, w], F32), Res()

        def b16t(nm, w=RB):
            return P.sb(nm, [128, w], BF16), Res()
        sg, r_sg = f32t("rw_sg")
        a_, r_a = f32t("rw_a")
        cs, r_cs_ = f32t("rw_cs")
        s1, r_s1 = f32t("rw_s1")
        s0, r_s0 = f32t("rw_s0")
        e0, r_e0 = f32t("rw_e0")
        e1, r_e1 = f32t("rw_e1")
        e2, r_e2 = f32t("rw_e2")
        e3, r_e3 = f32t("rw_e3")
        tt, r_tt = f32t("rw_tt")
        keys, r_keys = f32t("rw_keys")
        bb, r_bb = f32t("rw_bb")
        BhT, r_BhT = f32t("rw_BhT")
        KhT, r_KhT = f32t("rw_KhT")
        At, r_At = b16t("rw_At")
        Rt, r_Rt = b16t("rw_Rt")
        Bm1, r_Bm1 = b16t("rw_Bm1")
        Bm2, r_Bm2 = b16t("rw_Bm2")
        Km1, r_Km1 = b16t("rw_Km1")
        Km2, r_Km2 = b16t("rw_Km2")
        for t_, r_ in ((Bm1, r_Bm1), (Bm2, r_Bm2), (Km1, r_Km1), (Km2, r_Km2)):
            P.op("pool", lambda e, t_=t_: e.memset(t_[:], 0.0), writes=[r_])
        bsm = P.sb("rw_bsm", [128, 8], F32)
        r_bsm = Res()
        pnd = P.sb("rw_pnd", [128, 2], F32)
        pnm = P.sb("rw_pnm", [128, 2], F32)
        Hbm = [b16t(f"rw_Hbm{i}", 128) for i in range(2)]
        r_pnd = Res()
        NCH = RB // 128
        SC1a = [f32t(f"rw_SC1a_{i}", 256) for i in range(NCH)]
        SC1b = [b16t(f"rw_SC1b_{i}", 256) for i in range(NCH)]
        SC2 = [b16t(f"rw_SC2_{i}", 512) for i in range(NCH)]
        SA = [f32t(f"rw_SA_{i}", 256) for i in range(NCH)]
        TT = [f32t(f"rw_TT_{i}", 256) for i in range(NCH)]
        XX = [[f32t(f"rw_XX_{i}_{j}", 512) for j in range(2)] for i in range(NCH)]
        Bhp = [b16t(f"rw_Bhp_{i}", 256) for i in range(NCH)]
        Khp = [b16t(f"rw_Khp_{i}", 256) for i in range(NCH)]
        RHp = [f32t(f"rw_RHp_{i}", 256) for i in range(2)]
        Up = [b16t(f"rw_Up_{i}", 256) for i in range(2)]
        for lst in (Bhp, Khp, RHp, Up):
            for t_, r_ in lst:
                P.op("pool", lambda e, t_=t_: e.memset(t_[:], 0.0), writes=[r_])
        Hf = P.sb("rw_Hf", [128, 128], F32)
        Hbf = P.sb("rw_Hbf", [128, 128], BF16)
        r_Hf = Res()
        r_Hbf = Res()
        yc, r_yc = s0, r_s0
        sq, r_sq = e0, r_e0
        rsd, r_rsd = e1, r_e1
        aa0, r_aa0 = e2, r_e2
        aa1, r_aa1 = e3, r_e3

        def padcopy(dst_t, dst_off, pstride, src_ap, r_dst, r_src):
            out_ap = bass.AP(dst_t, dst_off, [[pstride, 128], [192, 2], [1, 64]])
            P.op("dve", lambda e: e.tensor_copy(out=out_ap, in_=src_ap.rearrange("p (h j) -> p h j", h=2)), reads=[r_src], writes=[r_dst])

        kkc = lambda j: pcol(l, "kk", j)
        for hp in range(4):
            P.dma("sp", rT[:], PR_d[hp * 128:(hp + 1) * 128, :], reads=[rPR[hp]], writes=[r_rkv])
            P.dma("sp", kT[:], PR_d[512 + hp * 128:512 + (hp + 1) * 128, :], reads=[rPR[4 + hp]], writes=[r_rkv])
            P.dma("sp", vT[:], PR_d[1024 + hp * 128:1024 + (hp + 1) * 128, :], reads=[rPR[8 + hp]], writes=[r_rkv])
            ka = pcol(l, "ka", hp)
            omka = dpar[:, l * 32 + 14 + hp:l * 32 + 15 + hp]
            for bs in range(0, NT, RB):
                n = RB
                P.op("dve", lambda e: e.tensor_scalar(out=tt[:], in0=kT[:, bs:bs + n], scalar1=kkc(hp), scalar2=None, op0=ALU.mult),
                     reads=[r_rkv, r_par], writes=[r_tt])
                P.op("act", lambda e: e.activation(out=sq[:], in_=tt[:], func=AF.Square), reads=[r_tt], writes=[r_sq])
                bk, rb = nbank()
                P.op("pe", lambda e: e.matmul(bk[:, 0:n], onesbd_f[:], sq[:], start=True, stop=True), reads=[r_sq, r_const], writes=[rb])
                P.op("act", lambda e: e.activation(out=rsd[:], in_=bk[:, 0:n], func=AF.Sqrt, scale=1.0, bias=cst[:, 4:5]),
                     reads=[rb, r_cst], writes=[r_rsd])
                P.op("dve", lambda e: e.reciprocal(out=rsd[:], in_=rsd[:]), reads=[r_rsd], writes=[r_rsd])
                P.op("dve", lambda e: e.tensor_tensor(out=kkT[:, bs:bs + n], in0=tt[:], in1=rsd[:], op=ALU.mult),
                     reads=[r_tt, r_rsd], writes=[r_kk])
            for d in range(2):
                P.op("pool", lambda e: e.memset(Hf[:], 0.0), writes=[r_Hf])
                P.op("pool", lambda e: e.memset(Hbf[:], 0.0), writes=[r_Hbf])
                lat = list(range(0, LAT, RB))
                order = [LAT] + (lat if d == 0 else lat[::-1])
                mo = d * 1280
                w0c = pcol(l, f"w0_{d}", hp)
                a0c = pcol(l, f"a0_{d}", hp)
                seqi = 0
                for bs in order:
                    n = RB
                    bk, rb = nbank()
                    P.op("pe", lambda e: e.matmul(bk[:, 0:n], w2b[d][:, hp * 128:(hp + 1) * 128], twT[:, bs:bs + n], start=True, stop=True),
                         reads=[r_w, r_lora], writes=[rb])
                    P.op("act", lambda e: e.activation(out=sg[:], in_=bk[:, 0:n], func=AF.Sigmoid, scale=1.0, bias=w0c),
                         reads=[rb, r_par], writes=[r_sg])
                    bk, rb = nbank()
                    P.op("pe", lambda e: e.matmul(bk[:, 0:n], a2b[d][:, hp * 128:(hp + 1) * 128], adT[:, bs:bs + n], start=True, stop=True),
                         reads=[r_w, r_lora], writes=[rb])
                    P.op("act", lambda e: e.activation(out=a_[:], in_=bk[:, 0:n], func=AF.Sigmoid, scale=1.0, bias=a0c),
                         reads=[rb, r_par], writes=[r_a])
                    P.op("dve", lambda e: e.tensor_tensor_scan(out=cs[:], data0=rmask[:, 0:n], data1=sg[:], initial=0.0, op0=ALU.mult, op1=ALU.add),
                         reads=[r_w, r_sg], writes=[r_cs_])
                    if d == 0:
                        sS, r_sS = cs, r_cs_
                        P.op("dve", lambda e: e.tensor_tensor(out=s0[:], in0=cs[:], in1=sg[:], op=ALU.subtract), reads=[r_cs_, r_sg], writes=[r_s0])
                    else:
                        for ci in range(NCH):
                            tot = cs[:, ci * 128 + 127:ci * 128 + 128]
                            P.op("dve", lambda e: e.tensor_scalar(out=s0[:, ci * 128:(ci + 1) * 128], in0=cs[:, ci * 128:(ci + 1) * 128],
                                                                 scalar1=-1.0, scalar2=tot, op0=ALU.mult, op1=ALU.add),
                                 reads=[r_cs_], writes=[r_s0])
                        P.op("dve", lambda e: e.tensor_tensor(out=s1[:], in0=s0[:], in1=sg[:], op=ALU.add), reads=[r_s0, r_sg], writes=[r_s1])
                        sS, r_sS = s1, r_s1
                    for ci in range(NCH):
                        tot = cs[:, ci * 128 + 127:ci * 128 + 128]
                        for q_, fac in enumerate((LW / 2, -LW / 2, LW)):
                            P.op("dve", lambda e: e.tensor_scalar(out=bsm[:, ci * 4 + q_:ci * 4 + q_ + 1], in0=tot, scalar1=fac, scalar2=None, op0=ALU.mult),
                                 reads=[r_cs_], writes=[r_bsm])
                        P.op("act", lambda e: e.activation(out=pnd[:, ci:ci + 1], in_=tot, func=AF.Exp, scale=LW), reads=[r_cs_], writes=[r_pnd])
                        P.op("act", lambda e: e.activation(out=pnm[:, ci:ci + 1], in_=tot, func=AF.Exp, scale=LW / 2), reads=[r_cs_], writes=[r_pnd])
                        cl = slice(ci * 128, (ci + 1) * 128)
                        mpos, mneg, cb_ = (bsm[:, ci * 4 + q_:ci * 4 + q_ + 1] for q_ in range(3))
                        P.op("act", lambda e: e.activation(out=e1[:, cl], in_=sS[:, cl], func=AF.Exp, scale=LW, bias=mneg), reads=[r_sS, r_bsm], writes=[r_e1])
                        P.op("act", lambda e: e.activation(out=e0[:, cl], in_=s0[:, cl], func=AF.Exp, scale=LW, bias=mneg), reads=[r_s0, r_bsm], writes=[r_e0])
                        P.op("act", lambda e: e.activation(out=e2[:, cl], in_=sS[:, cl], func=AF.Exp, scale=-LW, bias=mpos), reads=[r_sS, r_bsm], writes=[r_e2])
                        P.op("act", lambda e: e.activation(out=e3[:, cl], in_=sS[:, cl], func=AF.Exp, scale=-LW, bias=cb_), reads=[r_sS, r_bsm], writes=[r_e3])
                    P.op("dve", lambda e: e.tensor_scalar(out=tt[:], in0=a_[:], scalar1=ka, scalar2=omka, op0=ALU.mult, op1=ALU.add),
                         reads=[r_a, r_par, r_dpar], writes=[r_tt])
                    P.op("pool", lambda e: e.tensor_tensor(out=keys[:], in0=tt[:], in1=kT[:, bs:bs + n], op=ALU.mult), reads=[r_tt, r_rkv], writes=[r_keys])
                    P.op("pool", lambda e: e.tensor_tensor(out=bb[:], in0=kkT[:, bs:bs + n], in1=a_[:], op=ALU.mult), reads=[r_kk, r_a], writes=[r_bb])
                    P.op("dve", lambda e: e.scalar_tensor_tensor(out=At[:], in0=kkT[:, bs:bs + n], scalar=-1.0, in1=e0[:], op0=ALU.mult, op1=ALU.mult),
                         reads=[r_kk, r_e0], writes=[r_At])
                    P.op("pool", lambda e: e.tensor_tensor(out=Rt[:], in0=rT[:, bs:bs + n], in1=e1[:], op=ALU.mult), reads=[r_rkv, r_e1], writes=[r_Rt])
                    P.op("dve", lambda e: e.tensor_tensor(out=Bm1[0:64, :], in0=bb[0:64, :], in1=e2[0:64, :], op=ALU.mult), reads=[r_bb, r_e2], writes=[r_Bm1])
                    P.op("dve", lambda e: e.tensor_tensor(out=Bm2[64:128, :], in0=bb[64:128, :], in1=e2[64:128, :], op=ALU.mult), reads=[r_bb, r_e2], writes=[r_Bm2])
                    P.op("pool", lambda e: e.tensor_tensor(out=Km1[0:64, :], in0=keys[0:64, :], in1=e2[0:64, :], op=ALU.mult), reads=[r_keys, r_e2], writes=[r_Km1])
                    P.op("pool", lambda e: e.tensor_tensor(out=Km2[64:128, :], in0=keys[64:128, :], in1=e2[64:128, :], op=ALU.mult), reads=[r_keys, r_e2], writes=[r_Km2])
                    P.op("dve", lambda e: e.tensor_tensor(out=BhT[:], in0=bb[:], in1=e3[:], op=ALU.mult), reads=[r_bb, r_e3], writes=[r_BhT])
                    P.op("pool", lambda e: e.tensor_tensor(out=KhT[:], in0=keys[:], in1=e3[:], op=ALU.mult), reads=[r_keys, r_e3], writes=[r_KhT])
                    for ci in range(NCH):
                        cl = slice(ci * 128, (ci + 1) * 128)
                        kc = (bs + ci * 128) // 128
                        bk, rb = nbank()
                        P.op("pe", lambda e: e.transpose(bk[:, 0:128], BhT[:, cl], ident_f[:]), reads=[r_BhT, r_const], writes=[rb])
                        P.op("pe", lambda e: e.transpose(bk[:, 128:256], KhT[:, cl], ident_f[:]), reads=[r_KhT, r_const], writes=[rb])
                        if d == 0:
                            P.op("pe", lambda e: e.transpose(bk[:, 256:384], vT[:, bs + ci * 128:bs + (ci + 1) * 128], ident_f[:]),
                                 reads=[r_rkv, r_const], writes=[rb])
                            padcopy(Vp, kc * 256, 18 * 256, bk[:, 256:384], r_Vp, rb)
                        padcopy(Bhp[ci][0], 0, 256, bk[:, 0:128], Bhp[ci][1], rb)
                        padcopy(Khp[ci][0], 0, 256, bk[:, 128:256], Khp[ci][1], rb)
                        b1, rb1 = nbank()
                        b2, rb2 = nbank()
                        b3, rb3 = nbank()
                        for q_, (lt_, rl_) in enumerate(((Bm1, r_Bm1), (Bm2, r_Bm2), (Km1, r_Km1), (Km2, r_Km2))):
                            P.op("pe", lambda e: e.matmul(b1[:, q_ * 128:(q_ + 1) * 128], lt_[:, cl], At[:, cl], start=True, stop=True),
                                 reads=[rl_, r_At], writes=[rb1])
                            P.op("pe", lambda e: e.matmul(b2[:, q_ * 128:(q_ + 1) * 128], lt_[:, cl], Rt[:, cl], start=True, stop=True),
                                 reads=[rl_, r_Rt], writes=[rb2])
                        P.op("pe", lambda e: e.matmul(b3[:, 0:128], At[:, cl], Bm1[:, cl], start=True, stop=True), reads=[r_At, r_Bm1], writes=[rb3])
                        P.op("pe", lambda e: e.matmul(b3[:, 128:256], At[:, cl], Bm2[:, cl], start=True, stop=True), reads=[r_At, r_Bm2], writes=[rb3])
                        P.op("dve", lambda e: e.tensor_tensor(out=SC1a[ci][0][:], in0=b1[:, 0:256], in1=mk[:, mo:mo + 256], op=ALU.mult),
                             reads=[rb1, r_w], writes=[SC1a[ci][1]])
                        P.op("dve", lambda e: e.tensor_tensor(out=SC1b[ci][0][:], in0=b1[:, 256:512], in1=mk[:, mo + 256:mo + 512], op=ALU.mult),
                             reads=[rb1, r_w], writes=[SC1b[ci][1]])
                        P.op("dve", lambda e: e.tensor_tensor(out=SC2[ci][0][:], in0=b2[:], in1=mk[:, mo + 512:mo + 1024], op=ALU.mult),
                             reads=[rb2, r_w], writes=[SC2[ci][1]])
                        P.op("dve", lambda e: e.tensor_tensor(out=SA[ci][0][:], in0=b3[:, 0:256], in1=mk[:, mo + 1024:mo + 1280], op=ALU.mult),
                             reads=[rb3, r_w], writes=[SA[ci][1]])
                        P.op("pool", lambda e: e.tensor_tensor(out=TT[ci][0][:], in0=SC1a[ci][0][:], in1=ident2_f[:], op=ALU.add),
                             reads=[SC1a[ci][1], r_const], writes=[TT[ci][1]])
                    for j in range(1, 7):
                        for ci in range(NCH):
                            if j == 1:
                                Xs, rXs, XTs, rXTs, xo, xto = SA[ci][0], SA[ci][1], SC1a[ci][0], SC1a[ci][1], 0, 0
                            else:
                                Xs, rXs = XX[ci][j % 2]
                                XTs, rXTs, xo, xto = Xs, rXs, 0, 256
                            Xn, rXn = XX[ci][(j + 1) % 2]
                            bn, rbn = nbank()
                            for h in range(2):
                                hs = slice(h * 128, (h + 1) * 128)
                                Xh = Xs[:, xo + h * 128:xo + (h + 1) * 128]
                                XTh = XTs[:, xto + h * 128:xto + (h + 1) * 128]
                                P.op("pe", lambda e: e.matmul(bn[:, hs], XTh, Xh, start=True, stop=True), reads=[rXs, rXTs], writes=[rbn])
                                if j < 6:
                                    P.op("pe", lambda e: e.matmul(bn[:, 256 + h * 128:256 + (h + 1) * 128], Xh, XTh, start=True, stop=True),
                                         reads=[rXs, rXTs], writes=[rbn])
                            w_ = 512 if j < 6 else 256
                            P.op("act", lambda e: e.copy(out=Xn[:, 0:w_], in_=bn[:, 0:w_]), reads=[rbn], writes=[rXn])
                        for jj in ([j - 1] if j >= 2 else []) + ([6] if j == 6 else []):
                            for ci in range(NCH):
                                Xp, rXp = XX[ci][(jj + 1) % 2]
                                bt, rbt = nbank()
                                for h in range(2):
                                    hs = slice(h * 128, (h + 1) * 128)
                                    P.op("pe", lambda e: e.matmul(bt[:, hs], Xp[:, hs], TT[ci][0][:, hs], start=True, stop=True),
                                         reads=[rXp, TT[ci][1]], writes=[rbt])
                                P.op("dve", lambda e: e.tensor_tensor(out=TT[ci][0][:], in0=bt[:, 0:256], in1=TT[ci][0][:], op=ALU.add),
                                     reads=[rbt, TT[ci][1]], writes=[TT[ci][1]])
                    cis = list(range(NCH)) if d == 0 else list(range(NCH))[::-1]
                    for ci in cis:
                        cl = slice(ci * 128, (ci + 1) * 128)
                        c0 = bs + ci * 128
                        kc = c0 // 128
                        RH, rRH = RHp[seqi % 2]
                        U_, rU = Up[seqi % 2]
                        seqi += 1
                        Hm, rHm = Hbm[seqi % 2]
                        P.op("dve", lambda e: e.tensor_scalar(out=Hm[:], in0=Hf[:], scalar1=pnm[:, ci:ci + 1], scalar2=None, op0=ALU.mult),
                             reads=[r_Hf, r_pnd], writes=[rHm])
                        q1, rq1 = nbank()
                        P.op("pe", lambda e: e.matmul(q1[:, 0:128], At[:, cl], Hm[:], start=True, stop=False), reads=[r_At, rHm], writes=[rq1])
                        P.op("pe", lambda e: e.matmul(q1[:, 0:128], SC1b[ci][0][:, 0:128], Vp[:, kc, 0:128], start=False, stop=False),
                             reads=[SC1b[ci][1], r_Vp], writes=[rq1])
                        P.op("pe", lambda e: e.matmul(q1[:, 0:128], SC1b[ci][0][:, 128:256], Vp[:, kc, 128:256], start=False, stop=True),
                             reads=[SC1b[ci][1], r_Vp], writes=[rq1])
                        padcopy(RH, 0, 256, q1[:, 0:128], rRH, rq1)
                        q2, rq2 = nbank()
                        P.op("pe", lambda e: e.matmul(q2[:, 0:128], TT[ci][0][:, 0:128], RH[:, 0:128], start=True, stop=False),
                             reads=[TT[ci][1], rRH], writes=[rq2])
                        P.op("pe", lambda e: e.matmul(q2[:, 0:128], TT[ci][0][:, 128:256], RH[:, 128:256], start=False, stop=True),
                             reads=[TT[ci][1], rRH], writes=[rq2])
                        padcopy(U_, 0, 256, q2[:, 0:128], rU, rq2)
                        q4, rq4 = nbank()
                        P.op("pe", lambda e: e.matmul(q4[:, 0:128], Hm[:], Rt[:, cl], start=True, stop=False), reads=[rHm, r_Rt], writes=[rq4])
                        P.op("pe", lambda e: e.matmul(q4[:, 0:128], U_[:, 0:128], SC2[ci][0][:, 0:128], start=False, stop=False),
                             reads=[rU, SC2[ci][1]], writes=[rq4])
                        P.op("pe", lambda e: e.matmul(q4[:, 0:128], U_[:, 128:256], SC2[ci][0][:, 128:256], start=False, stop=False),
                             reads=[rU, SC2[ci][1]], writes=[rq4])
                        P.op("pe", lambda e: e.matmul(q4[:, 0:128], Vp[:, kc, 0:128], SC2[ci][0][:, 256:384], start=False, stop=False),
                             reads=[r_Vp, SC2[ci][1]], writes=[rq4])
                        P.op("pe", lambda e: e.matmul(q4[:, 0:128], Vp[:, kc, 128:256], SC2[ci][0][:, 384:512], start=False, stop=True),
                             reads=[r_Vp, SC2[ci][1]], writes=[rq4])
                        if d == 0:
                            P.op("act", lambda e: e.copy(out=Yacc[:, c0:c0 + 128], in_=q4[:, 0:128]), reads=[rq4], writes=[r_Y])
                        else:
                            P.op("dve", lambda e: e.tensor_tensor(out=Yacc[:, c0:c0 + 128], in0=q4[:, 0:128], in1=Yacc[:, c0:c0 + 128], op=ALU.add),
                                 reads=[rq4, r_Y], writes=[r_Y])
                        q3, rq3 = nbank()
                        P.op("pe", lambda e: e.matmul(q3[:, 0:128], Bhp[ci][0][:, 0:128], U_[:, 0:128], start=True, stop=False),
                             reads=[Bhp[ci][1], rU], writes=[rq3])
                        P.op("pe", lambda e: e.matmul(q3[:, 0:128], Bhp[ci][0][:, 128:256], U_[:, 128:256], start=False, stop=False),
                             reads=[Bhp[ci][1], rU], writes=[rq3])
                        P.op("pe", lambda e: e.matmul(q3[:, 0:128], Khp[ci][0][:, 0:128], Vp[:, kc, 0:128], start=False, stop=False),
                             reads=[Khp[ci][1], r_Vp], writes=[rq3])
                        P.op("pe", lambda e: e.matmul(q3[:, 0:128], Khp[ci][0][:, 128:256], Vp[:, kc, 128:256], start=False, stop=True),
                             reads=[Khp[ci][1], r_Vp], writes=[rq3])
                        P.op("dve", lambda e: e.scalar_tensor_tensor(out=Hf[:], in0=Hf[:], scalar=pnd[:, ci:ci + 1], in1=q3[:, 0:128],
                                                                    op0=ALU.mult, op1=ALU.add), reads=[r_Hf, r_pnd, rq3], writes=[r_Hf])
            if "rwkv" in dbg and l == 0:
                dy = dbg_out["dbg_ysum"] if "dbg_ysum" in dbg_out else dbg_tensor("dbg_ysum", [512, NT], F32)
                P.dma("sp", dy[hp * 128:(hp + 1) * 128, :], Yacc[:], reads=[r_Y])
            lw_, lb_, rk_ = pcol(l, "lnxw", hp), pcol(l, "lnxb", hp), pcol(l, "rk", hp)
            for bs in range(0, NT, RB):
                n = RB
                bk, rb = nbank()
                P.op("pe", lambda e: e.matmul(bk[:, 0:n], onesbd_f[:], Yacc[:, bs:bs + n], start=True, stop=True), reads=[r_Y, r_const], writes=[rb])
                P.op("dve", lambda e: e.scalar_tensor_tensor(out=yc[:], in0=bk[:, 0:n], scalar=-1.0 / 64, in1=Yacc[:, bs:bs + n],
                                                            op0=ALU.mult, op1=ALU.add), reads=[rb, r_Y], writes=[r_yc])
                P.op("act", lambda e: e.activation(out=sq[:], in_=yc[:], func=AF.Square), reads=[r_yc], writes=[r_sq])
                bk, rb = nbank()
                P.op("pe", lambda e: e.matmul(bk[:, 0:n], onesbd_f[:], sq[:], start=True, stop=True), reads=[r_sq, r_const], writes=[rb])
                P.op("act", lambda e: e.activation(out=rsd[:], in_=bk[:, 0:n], func=AF.Sqrt, scale=1.0 / 64, bias=cst[:, 1:2]),
                     reads=[rb, r_cst], writes=[r_rsd])
                P.op("dve", lambda e: e.reciprocal(out=rsd[:], in_=rsd[:]), reads=[r_rsd], writes=[r_rsd])
                P.op("dve", lambda e: e.tensor_tensor(out=yc[:], in0=yc[:], in1=rsd[:], op=ALU.mult), reads=[r_yc, r_rsd], writes=[r_yc])
                P.op("act", lambda e: e.activation(out=yc[:], in_=yc[:], func=AF.Identity, scale=lw_, bias=lb_), reads=[r_yc, r_par], writes=[r_yc])
                for d, (aa, r_aa) in enumerate(((aa0, r_aa0), (aa1, r_aa1))):
                    bk, rb = nbank()
                    P.op("pe", lambda e: e.matmul(bk[:, 0:n], a2b[d][:, hp * 128:(hp + 1) * 128], adT[:, bs:bs + n], start=True, stop=True),
                         reads=[r_w, r_lora], writes=[rb])
                    P.op("act", lambda e: e.activation(out=aa[:], in_=bk[:, 0:n], func=AF.Sigmoid, scale=1.0, bias=pcol(l, f"a0_{d}", hp)),
                         reads=[rb, r_par], writes=[r_aa])
                P.op("pool", lambda e: e.tensor_tensor(out=aa0[:], in0=aa0[:], in1=aa1[:], op=ALU.add), reads=[r_aa0, r_aa1], writes=[r_aa0])
                P.op("dve", lambda e: e.tensor_scalar(out=tt[:], in0=aa0[:], scalar1=0.5, scalar2=ka, op0=ALU.mult, op1=ALU.mult),
                     reads=[r_aa0, r_par], writes=[r_tt])
                P.op("dve", lambda e: e.tensor_scalar(out=tt[:], in0=tt[:], scalar1=omka, scalar2=None, op0=ALU.add), reads=[r_tt, r_dpar], writes=[r_tt])
                P.op("pool", lambda e: e.tensor_tensor(out=keys[:], in0=tt[:], in1=kT[:, bs:bs + n], op=ALU.mult), reads=[r_tt, r_rkv], writes=[r_keys])
                P.op("dve", lambda e: e.scalar_tensor_tensor(out=bb[:], in0=rT[:, bs:bs + n], scalar=rk_, in1=keys[:], op0=ALU.mult, op1=ALU.mult),
                     reads=[r_rkv, r_par, r_keys], writes=[r_bb])
                bk, rb = nbank()
                P.op("pe", lambda e: e.matmul(bk[:, 0:n], onesbd_f[:], bb[:], start=True, stop=True), reads=[r_bb, r_const], writes=[rb])
                P.op("dve", lambda e: e.tensor_tensor(out=sq[:], in0=bk[:, 0:n], in1=vT[:, bs:bs + n], op=ALU.mult), reads=[rb, r_rkv], writes=[r_sq])
                P.op("pool", lambda e: e.tensor_tensor(out=sq[:], in0=sq[:], in1=yc[:], op=ALU.add), reads=[r_sq, r_yc], writes=[r_sq])
                bk, rb = nbank()
                P.op("pe", lambda e: e.matmul(bk[:, 0:n], g2b[:, hp * 128:(hp + 1) * 128], gsT[:, bs:bs + n], start=True, stop=True),
                     reads=[r_w, r_lora], writes=[rb])
                yo_, r_yo_ = yob[(bs // RB) % 2]
                P.op("dve", lambda e: e.tensor_tensor(out=yo_[:], in0=bk[:, 0:n], in1=sq[:], op=ALU.mult), reads=[rb, r_sq], writes=[r_yo_])
                P.dma("sp", YR_d[hp * 128:(hp + 1) * 128, bs:bs + n], yo_[:], reads=[r_yo_], writes=[rYR[hp]])
        P.pop_scope()

    def load_x(jb):
        for c in range(8):
            P.dma("sp", xT[:, c, 0:LAT], xT_d[jb, c * 128:(c + 1) * 128, :], writes=[rx[0], rx[1], rx[2], rx[3]])
            P.dma("sp", xT[:, c, LAT:NT], cxT_d[jb, c * 128:(c + 1) * 128, :], writes=[rx[4]])

    def dump(name, src, shape, dtype, rs):
        d = dbg_tensor(name, shape, dtype)
        P.dma("sp", d, src, reads=rs)

    prologue()
    if "mod" in dbg:
        d = dbg_tensor("dbg_mod", [128, 2 * 48 * 5], F32)
        P.dma("sp", d, modT[:].rearrange("p a b c -> p (a b c)"), reads=[r_mod])
    for jb in range(nb):
        load_x(jb)
        for l in range(nlayers):
            last = l == nlayers - 1
            tbs = [0, 1, 2, 3, 4]
            phase_proj(l, jb, tbs)
            if stop_after == "proj":
                dump("dbg_PR", PR_d, [RC, NT], F32, rPR)
                dump("dbg_PA", PA_d, [768, NT], F32, rPA)
                dump("dbg_PG", PG_d, [2048, NT], BF16, rPG)
                break
            if "noattn" not in dbg:
                phase_attn(l, not last)
            if "norwkv" not in dbg:
                phase_rwkv(l)
            if stop_after == "mix" or ("mix" in dbg and l == 0 and jb == 0):
                dump(f"dbg_YA", YA_d, [512, NT], BF16, rYA)
                dump(f"dbg_YR", YR_d, [512, NT], BF16, rYR)
                if stop_after == "mix":
                    break
            mtbs = tbs if not last else [0, 1, 2, 3]
            phase_merge(l, jb, mtbs)
            if "x" in dbg and jb == 0:
                d_ = dbg_tensor(f"dbg_xmix{l}", [128, 8 * NT], F32)
                P.dma("sp", d_, xT[:].rearrange("p c t -> p (c t)"), reads=rx)
            phase_ffn(l, jb, mtbs, moe=(l % 2 == 1))
            if "x" in dbg and jb == 0:
                d_ = dbg_tensor(f"dbg_xffn{l}", [128, 8 * NT], F32)
                P.dma("sp", d_, xT[:].rearrange("p c t -> p (c t)"), reads=rx)
            if stop_after == f"l{l}":
                break
        else:
            phase_final(jb)
        if stop_after is not None:
            break
    P.barrier()
    ninstr = P.ninstr
    P.close()
    return nc, dbg_out, ninstr


def make_inmaps(inp, ncores=8, nb=4):
    consts = host_consts()
    params = host_params(inp)
    xT = np.ascontiguousarray(np.transpose(inp["x"], (0, 2, 1)))
    cxT = np.ascontiguousarray(np.transpose(inp["ctx"], (0, 2, 1)))
    maps = []
    for i in range(ncores):
        m = {}
        m["xT"] = xT[i * nb:(i + 1) * nb]
        m["cxT"] = cxT[i * nb:(i + 1) * nb]
        c5 = np.concatenate([inp["c"][i * nb:(i + 1) * nb], np.zeros((4 - nb, D), np.float32), inp["c_ctx"][None, :]], axis=0)
        m["cT"] = np.ascontiguousarray(c5.reshape(5, 8, 128).transpose(2, 1, 0))
        m["params"] = params
        m.update(consts)
        for k in ("ada_w", "w_in", "rwkv_w2", "rwkv_a2", "rwkv_g2", "w_pa", "w_pb", "w_o", "ffn_wg", "ffn_wu", "ffn_wd",
                  "router", "moe_wg", "moe_wu", "moe_wd"):
            m[k] = inp[k]
        maps.append(m)
    return maps


def kernel(**inputs):
    inp = {k: np.asarray(v) for k, v in inputs.items()}
    ncores, nb = 8, 4
    nc, _, _ = build(nb=nb, nlayers=2)
    maps = make_inmaps(inp, ncores=ncores, nb=nb)
    res = run_bass_kernel_spmd(nc, maps, core_ids=list(range(ncores)))
    outT = np.concatenate([np.asarray(r["outT"]) for r in res.results], axis=0)
    return np.ascontiguousarray(np.transpose(outT, (0, 2, 1))).astype(np.float32)
```
